# Optimizing a Trainium2 kernel written in Bass

```python
import jax, jax.numpy as jnp
from jax import lax
import numpy as np

D_MODEL = 1024
BATCH = 2
SEQ = 8192
DEPTH = 1

N_META = 16
BLOCK = 128
PAD = BLOCK - N_META
MLA_HEADS = 8
MLA_NOPE = 64
MLA_ROPE = 32
MLA_V = 64
Q_LORA = 512
KV_LORA = 256
ROPE_THETA = 10000.0
FOX_HEADS = 8
FOX_HD = 64
N_GROUPS = 4
EXPERTS_PER_GROUP = 8
TOP_K = 2
D_EXPERT = 256
EPS = 1e-6
NEG = -1e30
IN_SPLITS = (Q_LORA, KV_LORA, MLA_ROPE, FOX_HEADS * FOX_HD, FOX_HEADS * FOX_HD, FOX_HEADS * FOX_HD, FOX_HEADS, D_MODEL, D_MODEL)
D_IN = sum(IN_SPLITS)

kernel_name = "hybrid_mla_fox_hmoe_block"


def rms_norm(x, g):
    xf = x.astype(jnp.float32)
    y = xf * lax.rsqrt(jnp.mean(xf * xf, axis=-1, keepdims=True) + EPS)
    return (y * g.astype(jnp.float32)).astype(x.dtype)


def rope_tables(length):
    pos = jnp.arange(length, dtype=jnp.float32)
    inv = ROPE_THETA ** (-jnp.arange(0, MLA_ROPE, 2, dtype=jnp.float32) / MLA_ROPE)
    ang = pos[:, None] * inv[None, :]
    return jnp.cos(ang), jnp.sin(ang)


def apply_rope(x, cos, sin):
    xf = x.astype(jnp.float32)
    x1, x2 = xf[..., : MLA_ROPE // 2], xf[..., MLA_ROPE // 2:]
    c, s = cos[None, :, None, :], sin[None, :, None, :]
    return jnp.concatenate([x1 * c - x2 * s, x1 * s + x2 * c], axis=-1).astype(x.dtype)


def blocked_causal_attention(q, k, v, decay, scale):
    B, L, H, _ = q.shape
    dv = v.shape[-1]
    lp = L + PAD
    nb = lp // BLOCK

    def prep(t):
        return jnp.pad(t, ((0, 0), (PAD, 0)) + ((0, 0),) * (t.ndim - 2)).swapaxes(1, 2)

    qp = prep(q.astype(jnp.float32)) * scale
    kp = prep(k.astype(jnp.float32))
    vp = prep(v)
    q_blocks = qp.reshape(B, H, nb, BLOCK, -1).transpose(2, 0, 1, 3, 4)
    if decay is None:
        cp, c_blocks = None, None
    else:
        cp = prep(decay.astype(jnp.float32))
        c_blocks = cp.reshape(B, H, nb, BLOCK).transpose(2, 0, 1, 3)
    k_pos = jnp.arange(lp)
    k_valid = k_pos >= PAD

    def attend(args):
        i, qb, cb = args
        s = jnp.einsum('bhqd,bhkd->bhqk', qb, kp)
        if cp is not None:
            s = s + cb[..., :, None] - cp[:, :, None, :]
        q_pos = i * BLOCK + jnp.arange(BLOCK)
        mask = (k_pos[None, :] <= q_pos[:, None]) & k_valid[None, :]
        s = jnp.where(mask[None, None], s, NEG)
        p = jax.nn.softmax(s, axis=-1)
        return jnp.einsum('bhqk,bhkd->bhqd', p.astype(vp.dtype), vp)

    out = lax.map(attend, (jnp.arange(nb), q_blocks, c_blocks))
    out = out.transpose(1, 0, 3, 2, 4).reshape(B, lp, H * dv)
    return out[:, PAD:]


def mixer_block(u, cos, sin, w_in, b_forget, q_a_norm, w_q_up, kv_a_norm, w_kv_up,
                w_mla_out, w_fox_out, w_out):
    B, L, _ = u.shape
    proj = u @ w_in
    offsets = [int(o) for o in np.cumsum(IN_SPLITS)[:-1]]
    c_q, c_kv, k_r, fq, fk, fv, f_logit, g_a, g_b = jnp.split(proj, offsets, axis=-1)

    q = (rms_norm(c_q, q_a_norm) @ w_q_up).reshape(B, L, MLA_HEADS, MLA_NOPE + MLA_ROPE)
    q_mla = jnp.concatenate([q[..., :MLA_NOPE], apply_rope(q[..., MLA_NOPE:], cos, sin)], axis=-1)
    kv = (rms_norm(c_kv, kv_a_norm) @ w_kv_up).reshape(B, L, MLA_HEADS, MLA_NOPE + MLA_V)
    k_rope = jnp.broadcast_to(apply_rope(k_r[:, :, None, :], cos, sin), (B, L, MLA_HEADS, MLA_ROPE))
    k_mla = jnp.concatenate([kv[..., :MLA_NOPE], k_rope], axis=-1)
    y_a = blocked_causal_attention(q_mla, k_mla, kv[..., MLA_NOPE:], None,
                                   (MLA_NOPE + MLA_ROPE) ** -0.5)

    log_f = jax.nn.log_sigmoid(f_logit.astype(jnp.float32) + b_forget.astype(jnp.float32))
    decay = jnp.cumsum(log_f, axis=1)
    y_b = blocked_causal_attention(fq.reshape(B, L, FOX_HEADS, FOX_HD),
                                   fk.reshape(B, L, FOX_HEADS, FOX_HD),
                                   fv.reshape(B, L, FOX_HEADS, FOX_HD), decay, FOX_HD ** -0.5)

    merged = jax.nn.sigmoid(g_a) * (y_a @ w_mla_out) + jax.nn.sigmoid(g_b) * (y_b @ w_fox_out)
    return merged @ w_out


def hier_moe(u, w_group_router, b_group_router, w_expert_router, b_expert_router,
             w_gate, w_up, w_down):
    B, L, D = u.shape
    t = u.reshape(B * L, D)
    g_prob = jax.nn.softmax((t @ w_group_router).astype(jnp.float32) + b_group_router.astype(jnp.float32), axis=-1)
    g_w, g_idx = lax.top_k(g_prob, 1)
    e_logits = ((t @ w_expert_router).astype(jnp.float32) + b_expert_router.astype(jnp.float32)).reshape(-1, N_GROUPS, EXPERTS_PER_GROUP)
    e_logits = jnp.take_along_axis(e_logits, g_idx[:, :, None], axis=1)[:, 0]
    e_w, e_idx = lax.top_k(jax.nn.softmax(e_logits, axis=-1), TOP_K)
    e_w = e_w / jnp.sum(e_w, axis=-1, keepdims=True)
    within = jnp.einsum('tk,tke->te', e_w, jax.nn.one_hot(e_idx, EXPERTS_PER_GROUP, dtype=jnp.float32))
    combine = ((g_w * jax.nn.one_hot(g_idx[:, 0], N_GROUPS, dtype=jnp.float32))[:, :, None]
               * within[:, None, :]).astype(u.dtype)
    y = jnp.zeros_like(t)
    for g in range(N_GROUPS):
        a = jnp.einsum('td,edf->tef', t, w_gate[g])
        b = jnp.einsum('td,edf->tef', t, w_up[g])
        hmid = jax.nn.silu(a) * b * combine[:, g, :, None]
        y = y + jnp.einsum('tef,efd->td', hmid, w_down[g])
    return y.reshape(B, L, D)


def setup_inputs(seed: int = 0) -> dict:
    key = jax.random.key(seed)
    ks = jax.random.split(key, 21)
    f32 = jnp.float32
    nrm = lambda k, shape, scale: jax.random.normal(k, shape, f32) * scale
    G, E, F, D = N_GROUPS, EXPERTS_PER_GROUP, D_EXPERT, D_MODEL
    return {
        "x": nrm(ks[0], (BATCH, SEQ, D), 1.0),
        "meta": nrm(ks[1], (N_META, D), 1.0),
        "attn_norm": 1.0 + nrm(ks[2], (DEPTH, D), 0.02),
        "w_in": nrm(ks[3], (DEPTH, D, D_IN), D ** -0.5),
        "b_forget": 2.0 + nrm(ks[4], (DEPTH, FOX_HEADS), 0.1),
        "q_a_norm": 1.0 + nrm(ks[5], (DEPTH, Q_LORA), 0.02),
        "w_q_up": nrm(ks[6], (DEPTH, Q_LORA, MLA_HEADS * (MLA_NOPE + MLA_ROPE)), Q_LORA ** -0.5),
        "kv_a_norm": 1.0 + nrm(ks[7], (DEPTH, KV_LORA), 0.02),
        "w_kv_up": nrm(ks[8], (DEPTH, KV_LORA, MLA_HEADS * (MLA_NOPE + MLA_V)), KV_LORA ** -0.5),
        "w_mla_out": nrm(ks[9], (DEPTH, MLA_HEADS * MLA_V, D), (MLA_HEADS * MLA_V) ** -0.5),
        "w_fox_out": nrm(ks[10], (DEPTH, FOX_HEADS * FOX_HD, D), (FOX_HEADS * FOX_HD) ** -0.5),
        "w_out": nrm(ks[11], (DEPTH, D, D), D ** -0.5),
        "ffn_norm": 1.0 + nrm(ks[12], (DEPTH, D), 0.02),
        "w_group_router": nrm(ks[13], (DEPTH, D, G), D ** -0.5),
        "b_group_router": nrm(ks[14], (DEPTH, G), 0.01),
        "w_expert_router": nrm(ks[15], (DEPTH, D, G * E), D ** -0.5),
        "b_expert_router": nrm(ks[16], (DEPTH, G * E), 0.01),
        "w_gate": nrm(ks[17], (DEPTH, G, E, D, F), D ** -0.5),
        "w_up": nrm(ks[18], (DEPTH, G, E, D, F), D ** -0.5),
        "w_down": nrm(ks[19], (DEPTH, G, E, F, D), F ** -0.5),
        "final_norm": 1.0 + nrm(ks[20], (D,), 0.02),
    }


def reference(x, meta, attn_norm, w_in, b_forget, q_a_norm, w_q_up, kv_a_norm, w_kv_up,
              w_mla_out, w_fox_out, w_out, ffn_norm, w_group_router, b_group_router,
              w_expert_router, b_expert_router, w_gate, w_up, w_down, final_norm):
    B = x.shape[0]
    h = jnp.concatenate([jnp.broadcast_to(meta[None].astype(x.dtype), (B, N_META, D_MODEL)), x], axis=1)
    cos, sin = rope_tables(h.shape[1])
    for l in range(DEPTH):
        h = h + mixer_block(rms_norm(h, attn_norm[l]), cos, sin, w_in[l], b_forget[l],
                            q_a_norm[l], w_q_up[l], kv_a_norm[l], w_kv_up[l],
                            w_mla_out[l], w_fox_out[l], w_out[l])
        h = h + hier_moe(rms_norm(h, ffn_norm[l]), w_group_router[l], b_group_router[l],
                         w_expert_router[l], b_expert_router[l], w_gate[l], w_up[l], w_down[l])
    h = rms_norm(h, final_norm)
    return h[:, N_META:]
```

```python
import contextlib
import numpy as np
import concourse.bass as bass
import concourse.mybir as mybir
from concourse.bass_utils import run_bass_kernel_spmd

F32 = mybir.dt.float32
BF16 = mybir.dt.bfloat16
AF = mybir.ActivationFunctionType
ALU = mybir.AluOpType
AX = mybir.AxisListType

D = 1024
SEQ = 8192
NMETA = 16
NK = NMETA + SEQ
NQ = 2048
NT = 16
NKT = 65
EPS = 1e-6
SC_MLA = 96 ** -0.5
SC_FOX = 0.125
O_CQ, O_CKV, O_KR, O_FQ, O_FK, O_FV, O_FL, O_GA, O_GB = 0, 512, 768, 800, 1312, 1824, 2336, 2344, 3368
E_PER_ROUND = 2
FILLER = 0


class Buf:
    __slots__ = ("name", "w", "r", "acc")

    def __init__(self, name, acc=False):
        self.name = name
        self.w = []
        self.r = []
        self.acc = acc


class _Rec:
    def __getattr__(self, name):
        def f(*a, **k):
            self.__dict__["call"] = (name, a, k)
            return self
        return f


class Prog:
    def __init__(self):
        self.streams = {k: [] for k in ("pe", "act", "dve", "pool", "sp")}
        self.cnt = {}
        self.seen = {k: {} for k in self.streams}
        self.load = {"act": 0.0, "dve": 0.0}

    def _deps(self, eng, reads, writes):
        need = {}
        for b in reads:
            for (s, v) in b.w:
                if need.get(s, 0) < v:
                    need[s] = v
        for b in writes:
            if not b.acc:
                for (s, v) in b.w:
                    if need.get(s, 0) < v:
                        need[s] = v
            for (s, v) in b.r:
                if need.get(s, 0) < v:
                    need[s] = v
        waits = []
        seen = self.seen[eng]
        for s, v in need.items():
            if eng == "pe" and s == "c_pe":
                continue
            if seen.get(s, 0) < v:
                waits.append((s, v))
                seen[s] = v
        return waits

    def _record(self, s, v, reads, writes):
        for b in reads:
            b.r.append((s, v))
            if len(b.r) > 64:
                m = {}
                for (ss, vv) in b.r:
                    if m.get(ss, 0) < vv:
                        m[ss] = vv
                b.r = list(m.items())
        for b in writes:
            if b.acc:
                b.w.append((s, v))
                if len(b.w) > 64:
                    m = {}
                    for (ss, vv) in b.w:
                        if m.get(ss, 0) < vv:
                            m[ss] = vv
                    b.w = list(m.items())
            else:
                b.w = [(s, v)]
                b.r = []

    @staticmethod
    def _bind(fn, deferred):
        if deferred:
            return fn
        rec = _Rec()
        fn(rec)
        name, a, k = rec.call
        return lambda e: getattr(e, name)(*a, **k)

    def op(self, eng, fn, reads=(), writes=(), inc=True):
        waits = self._deps(eng, reads, writes)
        s = "c_" + eng
        v = self.cnt.get(s, 0) + 1
        if inc:
            self.cnt[s] = v
        self.streams[eng].append((waits, self._bind(fn, False), s, 1 if inc else 0))
        self._record(s, v, reads, writes)

    def dma(self, eng, fn, key, reads=(), writes=(), deferred=False):
        waits = self._deps(eng, reads, writes)
        s = "d_" + key
        v = self.cnt.get(s, 0) + 16
        self.cnt[s] = v
        self.streams[eng].append((waits, self._bind(fn, deferred), s, 16))
        self._record(s, v, reads, writes)

    def pick(self, cost):
        e = "act" if self.load["act"] <= self.load["dve"] else "dve"
        self.load[e] += cost
        return e

    def barrier(self):
        for eng in self.streams:
            waits = []
            for s, v in self.cnt.items():
                if self.seen[eng].get(s, 0) < v:
                    waits.append((s, v))
                    self.seen[eng][s] = v
            if waits:
                self.streams[eng].append((waits, None, None, 0))

    def emit(self, nc, es):
        sems = {s: es.enter_context(nc.semaphore(s)) for s in self.cnt}
        engs = {"pe": "tensor", "act": "scalar", "dve": "vector", "pool": "gpsimd", "sp": "sync"}
        with nc.Block() as block:
            for k, attr in engs.items():
                stream = self.streams[k]

                def body(e, stream=stream):
                    for waits, fn, s, inc in stream:
                        for (ws, wv) in waits:
                            e.wait_ge(sems[ws], wv)
                        if fn is not None:
                            ins = fn(e)
                            if inc:
                                ins.then_inc(sems[s], inc)

                getattr(block, attr)(body)


def build_program(debug=False):
    nc = bass.Bass("TRN2", target_bir_lowering=False)
    nc.cache_partition_id()
    P = Prog()

    def dram_in(name, shape, dt=F32):
        return nc.dram_tensor(name, list(shape), dt, kind="ExternalInput").ap()

    x_all = dram_in("x_all", [SEQ, D])
    xq = dram_in("xq", [NQ, D])
    meta = dram_in("meta", [NMETA, D])
    attn_norm = dram_in("attn_norm", [D])
    w_in = dram_in("w_in", [D, 4392])
    b_forget = dram_in("b_forget", [8])
    q_a_norm = dram_in("q_a_norm", [512])
    w_q_up = dram_in("w_q_up", [512, 768])
    kv_a_norm = dram_in("kv_a_norm", [256])
    w_kv_up = dram_in("w_kv_up", [256, 1024])
    w_mla_out = dram_in("w_mla_out", [512, D])
    w_fox_out = dram_in("w_fox_out", [512, D])
    w_out = dram_in("w_out", [D, D])
    ffn_norm = dram_in("ffn_norm", [D])
    w_grt = dram_in("w_group_router", [D, 4])
    b_grt = dram_in("b_group_router", [4])
    w_ert = dram_in("w_expert_router", [D, 32])
    b_ert = dram_in("b_expert_router", [32])
    w_gate = dram_in("w_gate", [32, D, 256])
    w_up = dram_in("w_up", [32, D, 256])
    w_down = dram_in("w_down", [32, 256, D])
    final_norm = dram_in("final_norm", [D])
    cosk = dram_in("cosk", [32, NK])
    sink = dram_in("sink", [32, NK])
    cosq = dram_in("cosq", [32, NQ])
    sinq = dram_in("sinq", [32, NQ])
    masks = dram_in("masks", [4, 128, 128])
    ident_d = dram_in("ident", [128, 128])
    out = nc.dram_tensor("out", [NQ, D], F32, kind="ExternalOutput").ap()

    skind = "ExternalOutput" if debug else "Internal"

    def scratch(name, shape, dt=BF16):
        return nc.dram_tensor(name, list(shape), dt, kind=skind).ap()

    KM = scratch("KM", [8, 96, NK]); bKM = Buf("KM", acc=True)
    VM = scratch("VM", [8, 128, NKT, 65]); bVM = Buf("VM", acc=True)
    KF = scratch("KF", [8, 68, NK]); bKF = Buf("KF", acc=True)
    VF = scratch("VF", [8, 128, NKT, 65]); bVF = Buf("VF", acc=True)
    QM = scratch("QM", [8, 96, NQ]); bQM = Buf("QM", acc=True)
    QF = scratch("QF", [8, 68, NQ]); bQF = Buf("QF", acc=True)
    GS = scratch("GS", [16, 128, NQ]); bGS = Buf("GS", acc=True)
    CT = scratch("CT", [8, NK], F32); bCT = Buf("CT", acc=True)
    YD = scratch("YD", [NQ, D], BF16) if debug else None
    bOUT = Buf("out", acc=True)

    es = contextlib.ExitStack()
    with es:
        def sb(es_, name, shape, dt):
            return es_.enter_context(nc.sbuf_tensor(name, list(shape), dt))

        pS = [es.enter_context(nc.psum_tensor("pS%d" % i, [128, 1024], F32)) for i in range(3)]
        pf = [pS[0][:, 0:512], pS[0][:, 512:1024], pS[1][:, 0:512], pS[1][:, 512:1024]]
        pf += [es.enter_context(nc.psum_tensor("pf%d" % i, [128, 512], F32))[:, :] for i in (4, 5)]
        bpf = [Buf("pf%d" % i) for i in range(6)]
        pbf = [pS[2][:, 0:512].bitcast(BF16), pS[2][:, 512:1024].bitcast(BF16)]
        bpbf = [Buf("pbf%d" % i) for i in range(2)]
        rr = {"bf": 0, "f": 0}

        def next_bf():
            i = rr["bf"] % 2
            rr["bf"] += 1
            return pbf[i], bpbf[i]

        def next_f(lo=0, hi=6):
            i = lo + rr["f"] % (hi - lo)
            rr["f"] += 1
            return pf[i], bpf[i]

        ident32 = sb(es, "ident32", [128, 128], F32); bid32 = Buf("ident32")
        identb = sb(es, "identb", [128, 128], BF16); bidb = Buf("identb")
        epsD = sb(es, "epsD", [128, 1], F32); bepsD = Buf("epsD")
        ones_b = sb(es, "ones_b", [128, 512], BF16); bones = Buf("ones_b")
        zeros8 = sb(es, "zeros8", [8, 512], F32); bz8 = Buf("zeros8")
        P.dma("sp", lambda e: e.dma_start(out=ident32[:], in_=ident_d), "ident32", writes=[bid32])
        P.dma("pool", lambda e: e.dma_start(out=identb[:], in_=ident_d), "identb", writes=[bidb])
        P.op("pool", lambda e: e.memset(epsD[:], EPS), writes=[bepsD])
        P.op("pool", lambda e: e.memset(ones_b[:], 1.0), writes=[bones])
        P.op("pool", lambda e: e.memset(zeros8[:], 0.0), writes=[bz8])
        def rstd_from_ssq(ssq, bssq, rstd, brstd, n):
            P.op("act", lambda e: e.activation(out=rstd, in_=ssq, func=AF.Ln, bias=epsD[:, 0:1], scale=1.0 / n), reads=[bssq, bepsD], writes=[brstd])
            P.op("act", lambda e: e.activation(out=rstd, in_=rstd, func=AF.Exp, scale=-0.5), reads=[brstd], writes=[brstd])
            P.load["act"] += 0.6

        def copy(out_ap, in_ap, reads, writes, n, scale=None, eng=None):
            e_ = eng or P.pick(0.3 + n / 1000.0)
            if e_ == "act":
                if scale is None:
                    P.op("act", lambda e: e.copy(out=out_ap, in_=in_ap), reads=reads, writes=writes)
                else:
                    P.op("act", lambda e: e.mul(out=out_ap, in_=in_ap, mul=scale), reads=reads, writes=writes)
            else:
                if scale is None:
                    P.op("dve", lambda e: e.tensor_copy(out=out_ap, in_=in_ap), reads=reads, writes=writes)
                else:
                    P.op("dve", lambda e: e.tensor_scalar(out=out_ap, in0=in_ap, scalar1=scale, scalar2=None, op0=ALU.mult), reads=reads, writes=writes)

        def load_w(tile_ap, src_ap, key, btile):
            P.dma("pool", lambda e: e.dma_start(out=tile_ap, in_=src_ap), key, writes=[btile])

        def bcast_load(tile, btile, vec, key):
            P.dma("sp", lambda e: e.dma_start(out=tile[:], in_=vec.partition_broadcast(128)), key, writes=[btile])

        with contextlib.ExitStack() as ea:
            gattn = sb(ea, "gattn", [128, D], F32); bgattn = Buf("gattn")
            gkv = sb(ea, "gkv", [128, 256], F32); bgkv = Buf("gkv")
            gq = sb(ea, "gq", [128, 512], F32); bgq = Buf("gq")
            bcast_load(gattn, bgattn, attn_norm, "gattn")
            bcast_load(gkv, bgkv, kv_a_norm, "gkv")
            bcast_load(gq, bgq, q_a_norm, "gq")
            nbf = sb(ea, "nbf", [8, 1], F32); bnbf = Buf("nbf")
            P.dma("sp", lambda e: e.dma_start(out=nbf[:], in_=b_forget.rearrange("(h o) -> h o", o=1)), "nbf", writes=[bnbf])
            P.op("dve", lambda e: e.tensor_scalar(out=nbf[:], in0=nbf[:], scalar1=-1.0, scalar2=None, op0=ALU.mult), reads=[bnbf], writes=[bnbf])

            xin = [sb(ea, "xin%d" % i, [128, D], F32) for i in range(8)]
            bxin = [Buf("xin%d" % i) for i in range(8)]
            junk = sb(ea, "junk", [128, D], BF16); bjunk = Buf("junk")
            xs = [sb(ea, "xs%d" % i, [128, D], BF16) for i in range(4)]
            bxs = [Buf("xs%d" % i) for i in range(4)]
            ssq = [sb(ea, "ssq%d" % i, [128, 1], F32) for i in range(6)]
            bssq = [Buf("ssq%d" % i) for i in range(6)]
            rstd = [sb(ea, "rstd%d" % i, [128, 1], F32) for i in range(6)]
            brstd = [Buf("rstd%d" % i) for i in range(6)]
            xT = [sb(ea, "xT%d" % i, [128, 8, 512], BF16) for i in range(2)]
            bxT = [[Buf("xT%d_%d" % (i, t)) for t in range(4)] for i in range(2)]
            st = {"xin": 0, "xs": 0, "grp": 0}

            def issue_loads(srcs):
                res = []
                for (src, rows) in srcs:
                    i3 = st["xin"] % 8
                    st["xin"] += 1
                    if rows < 128:
                        P.op("pool", lambda e: e.memset(xin[i3][:], 0.0), writes=[bxin[i3]])
                    P.dma("sp", lambda e: e.dma_start(out=xin[i3][0:rows, :], in_=src), "xin%d" % i3, writes=[bxin[i3]])
                    res.append(i3)
                return res

            def norm_stage(loaded):
                for t, i3 in enumerate(loaded):
                    P.op("act", lambda e: e.activation(out=junk[:], in_=xin[i3][:], func=AF.Square, accum_out=ssq[t][:]), reads=[bxin[i3]], writes=[bjunk, bssq[t]])
                    P.load["act"] += 1.2
                    rstd_from_ssq(ssq[t][:], bssq[t], rstd[t][:], brstd[t], D)
                for t, i3 in enumerate(loaded):
                    P.op("dve", lambda e: e.scalar_tensor_tensor(out=xs[t][:], in0=xin[i3][:], scalar=rstd[t][:, 0:1], in1=gattn[:], op0=ALU.mult, op1=ALU.mult), reads=[bxin[i3], brstd[t], bgattn], writes=[bxs[t]])
                    P.load["dve"] += 1.2

            def tr_stage(nt_, slot):
                for t in range(nt_):
                    pt_, bpt_ = next_bf()
                    for c in range(8):
                        P.op("pe", lambda e: e.transpose(out=pt_[:, c * 128:(c + 1) * 128], in_=xs[t][:, c * 128:(c + 1) * 128], identity=identb[:]), reads=[bxs[t], bidb], writes=[bpt_], inc=(c == 7))
                    copy(xT[slot][:, :, t * 128:(t + 1) * 128], pt_[:, :].rearrange("p (c k) -> p c k", c=8), [bpt_], [bxT[slot][t]], 1024)

            with contextlib.ExitStack() as e1:
                wckv = sb(e1, "wckv", [128, 8, 256], BF16); bwckv = Buf("wckv")
                wkr = sb(e1, "wkr", [128, 8, 2, 96], BF16); bwkr = Buf("wkr")
                wfk = sb(e1, "wfk", [128, 8, 512], BF16); bwfk = Buf("wfk")
                wfv = sb(e1, "wfv", [128, 8, 512], BF16); bwfv = Buf("wfv")
                wfl = sb(e1, "wfl", [128, 8, 8], BF16); bwfl = Buf("wfl")
                wkvk = sb(e1, "wkvk", [128, 2, 8, 64], BF16); bwkvk = Buf("wkvk")
                wkvv = sb(e1, "wkvv", [128, 2, 8, 64], BF16); bwkvv = Buf("wkvv")
                w_in_c = w_in.rearrange("(c p) n -> p c n", p=128)
                load_w(wckv[:], w_in_c[:, :, O_CKV:O_CKV + 256], "wckv", bwckv)
                P.op("pool", lambda e: e.memset(wkr[:], 0.0), writes=[bwkr])
                load_w(wkr[:, :, 0, 64:96], w_in_c[:, :, O_KR:O_KR + 32], "wkr", bwkr)
                load_w(wkr[:, :, 1, 64:80], w_in_c[:, :, O_KR + 16:O_KR + 32], "wkr", bwkr)
                load_w(wkr[:, :, 1, 80:96], w_in_c[:, :, O_KR:O_KR + 16], "wkr", bwkr)
                load_w(wfk[:], w_in_c[:, :, O_FK:O_FK + 512], "wfk", bwfk)
                load_w(wfv[:], w_in_c[:, :, O_FV:O_FV + 512], "wfv", bwfv)
                load_w(wfl[:], w_in_c[:, :, O_FL:O_FL + 8], "wfl", bwfl)
                wkv_c = w_kv_up.rearrange("(c p) (h d) -> p c h d", p=128, d=128)
                for c in range(2):
                    load_w(wkvk[:, c, :, :], wkv_c[:, c, :, 0:64], "wkvk", bwkvk)
                    load_w(wkvv[:, c, :, :], wkv_c[:, c, :, 64:128], "wkvv", bwkvv)

                ckvn = [sb(e1, "ckvn%d" % i, [128, 256], BF16) for i in range(2)]; bckvn = [Buf("ckvn%d" % i) for i in range(2)]
                ckvnT = [sb(e1, "ckvnT%d" % i, [128, 2, 512], BF16) for i in range(2)]
                bckvnT = [[Buf("ckvnT%d_%d" % (i, t)) for t in range(4)] for i in range(2)]
                vf = [sb(e1, "vf%d" % i, [128, 8, 4, 65], BF16) for i in range(2)]
                bvf = [Buf("vf%d" % i) for i in range(2)]
                vm = [sb(e1, "vm%d" % i, [128, 8, 4, 65], BF16) for i in range(2)]
                bvm = [Buf("vm%d" % i) for i in range(2)]
                for i in range(2):
                    P.op("pool", lambda e, i=i: e.memset(vf[i][:], 1.0), writes=[bvf[i]])
                    P.op("pool", lambda e, i=i: e.memset(vm[i][:], 1.0), writes=[bvm[i]])
                kf = [sb(e1, "kf%d" % i, [128, 4, 512], BF16) for i in range(2)]
                bkf = [Buf("kf%d" % i) for i in range(2)]
                km = [sb(e1, "km%d" % i, [128, 4, 512], BF16) for i in range(2)]
                bkm = [Buf("km%d" % i) for i in range(2)]
                krT = [sb(e1, "krT%d" % i, [96, 512], BF16) for i in range(2)]
                bkrT = [Buf("krT%d" % i) for i in range(2)]
                ktab = [sb(e1, "ktab%d" % i, [96, 2, 512], F32) for i in range(2)]
                bktab = [Buf("ktab%d" % i) for i in range(2)]
                rt1 = sb(e1, "rt1", [96, 512], F32); brt1 = Buf("rt1")
                rt2 = sb(e1, "rt2", [96, 512], F32); brt2 = Buf("rt2")
                lf = sb(e1, "lf", [8, 512], F32); blf = Buf("lf")
                negc = [sb(e1, "negc%d" % i, [8, 512], F32) for i in range(2)]
                bnegc = [Buf("negc%d" % i) for i in range(2)]
                pcs = [sb(e1, "pcs%d" % i, [8, 3, 512], BF16) for i in range(2)]
                bpcs = [Buf("pcs%d" % i) for i in range(2)]
                r1 = sb(e1, "r1", [8, 512], F32); br1 = Buf("r1")
                r2 = sb(e1, "r2", [8, 512], F32); br2 = Buf("r2")
                carry = {"ap": None, "buf": None}

                def kside(loaded, col0, ncol, vt0):
                    g = st["grp"]
                    st["grp"] += 1
                    s = g % 2
                    nt = len(loaded)
                    ntok = nt * 128
                    tr_stage(nt, s)
                    rd_xT = [bxT[s][t] for t in range(nt)]
                    P.dma("sp", lambda e: e.dma_start(out=ktab[s][64:96, 0, 0:ncol], in_=cosk[:, col0:col0 + ncol]), "ktab%d" % s, writes=[bktab[s]])
                    P.dma("sp", lambda e: e.dma_start(out=ktab[s][64:96, 1, 0:ncol], in_=sink[:, col0:col0 + ncol]), "ktab%d" % s, writes=[bktab[s]])
                    def tm_front(t):
                        tc_ = slice(t * 128, (t + 1) * 128)
                        pa, bpa = next_f()
                        for c in range(8):
                            P.op("pe", lambda e: e.matmul(pa[:, 0:256], lhsT=xT[s][:, c, tc_], rhs=wckv[:, c, :], start=(c == 0), stop=(c == 7)), reads=[bxT[s][t], bwckv], writes=[bpa], inc=(c == 7))
                        i2 = 4 + t % 2
                        P.op("act", lambda e: e.activation(out=junk[:, 0:256], in_=pa[:, 0:256], func=AF.Square, accum_out=ssq[i2][:]), reads=[bpa], writes=[bjunk, bssq[i2]])
                        P.load["act"] += 0.5
                        rstd_from_ssq(ssq[i2][:], bssq[i2], rstd[i2][:], brstd[i2], 256)
                        P.op("dve", lambda e: e.scalar_tensor_tensor(out=ckvn[t % 2][:], in0=pa[:, 0:256], scalar=rstd[i2][:, 0:1], in1=gkv[:], op0=ALU.mult, op1=ALU.mult), reads=[bpa, brstd[i2], bgkv], writes=[bckvn[t % 2]])
                        P.load["dve"] += 0.5
                        pb, bpb = next_f()
                        for c in range(8):
                            P.op("pe", lambda e: e.matmul(pb[:, :], lhsT=xT[s][:, c, tc_], rhs=wfv[:, c, :], start=(c == 0), stop=(c == 7)), reads=[bxT[s][t], bwfv], writes=[bpb], inc=(c == 7))
                        copy(vf[s][:, :, t, 0:64], pb[:, :].rearrange("p (h d) -> p h d", h=8), [bpb], [bvf[s]], 512)

                    def tm_back(t):
                        tc_ = slice(t * 128, (t + 1) * 128)
                        pt_, bpt_ = next_bf()
                        for c in range(2):
                            P.op("pe", lambda e: e.transpose(out=pt_[:, c * 128:(c + 1) * 128], in_=ckvn[t % 2][:, c * 128:(c + 1) * 128], identity=identb[:]), reads=[bckvn[t % 2], bidb], writes=[bpt_], inc=(c == 1))
                        copy(ckvnT[s][:, :, tc_], pt_[:, 0:256].rearrange("p (c k) -> p c k", c=2), [bpt_], [bckvnT[s][t]], 256)
                        pc, bpc = next_f()
                        for c in range(2):
                            P.op("pe", lambda e: e.matmul(pc[:, :], lhsT=ckvnT[s][:, c, tc_], rhs=wkvv[:, c, :, :], start=(c == 0), stop=(c == 1)), reads=[bckvnT[s][t], bwkvv], writes=[bpc], inc=(c == 1))
                        copy(vm[s][:, :, t, 0:64], pc[:, :].rearrange("p (h d) -> p h d", h=8), [bpc], [bvm[s]], 512)

                    tm_front(0)
                    for t in range(nt):
                        if t + 1 < nt:
                            tm_front(t + 1)
                        tm_back(t)
                    for h in range(8):
                        P.dma("sp", lambda e: e.dma_start(out=VF[h, :, vt0:vt0 + nt, :], in_=vf[s][:, h, 0:nt, :]), "vf%d" % s, reads=[bvf[s]], writes=[bVF])
                        P.dma("sp", lambda e: e.dma_start(out=VM[h, :, vt0:vt0 + nt, :], in_=vm[s][:, h, 0:nt, :]), "vm%d" % s, reads=[bvm[s]], writes=[bVM])
                    rd_ck = [bckvnT[s][t] for t in range(nt)]
                    yield
                    for pr in range(4):
                        pa, bpa = next_f()
                        for c in range(8):
                            P.op("pe", lambda e, c=c, pa=pa, pr=pr: e.matmul(pa[:, 0:ntok], lhsT=wfk[:, c, pr * 128:(pr + 1) * 128], rhs=xT[s][:, c, 0:ntok], start=(c == 0), stop=(c == 7)), reads=rd_xT + [bwfk], writes=[bpa], inc=(c == 7))
                        copy(kf[s][:, pr, 0:ntok], pa[:, 0:ntok], [bpa], [bkf[s]], ntok)
                    for two in range(2):
                        P.dma("sp", lambda e, two=two: e.dma_start(out=KF[:, 0:64, col0:col0 + ncol].rearrange("(p two) r c -> two r p c", two=2)[two], in_=kf[s][two * 64:(two + 1) * 64, :, 0:ncol]), "kf%d" % s, reads=[bkf[s]], writes=[bKF])
                    for pr in range(4):
                        pa, bpa = next_f()
                        for c in range(2):
                            P.op("pe", lambda e, c=c, pa=pa, pr=pr: e.matmul(pa[:, 0:ntok], lhsT=wkvk[:, c, 2 * pr:2 * pr + 2, :], rhs=ckvnT[s][:, c, 0:ntok], start=(c == 0), stop=(c == 1)), reads=rd_ck + [bwkvk], writes=[bpa], inc=(c == 1))
                        copy(km[s][:, pr, 0:ntok], pa[:, 0:ntok], [bpa], [bkm[s]], ntok)
                    for two in range(2):
                        P.dma("sp", lambda e, two=two: e.dma_start(out=KM[:, 0:64, col0:col0 + ncol].rearrange("(p two) r c -> two r p c", two=2)[two], in_=km[s][two * 64:(two + 1) * 64, :, 0:ncol]), "km%d" % s, reads=[bkm[s]], writes=[bKM])
                    pa, bpa = next_f()
                    pb, bpb = next_f()
                    for v, (pp, bpp) in enumerate(((pa, bpa), (pb, bpb))):
                        for c in range(8):
                            P.op("pe", lambda e, c=c, pp=pp, v=v: e.matmul(pp[0:96, 0:ntok], lhsT=wkr[:, c, v, :], rhs=xT[s][:, c, 0:ntok], start=(c == 0), stop=(c == 7)), reads=rd_xT + [bwkr], writes=[bpp], inc=(c == 7))
                    P.op("dve", lambda e, pa=pa: e.tensor_tensor(out=rt1[64:96, 0:ncol], in0=pa[64:96, 0:ncol], in1=ktab[s][64:96, 0, 0:ncol], op=ALU.mult), reads=[bpa, bktab[s]], writes=[brt1])
                    P.op("dve", lambda e, pb=pb: e.tensor_tensor(out=rt2[64:96, 0:ncol], in0=pb[64:96, 0:ncol], in1=ktab[s][64:96, 1, 0:ncol], op=ALU.mult), reads=[bpb, bktab[s]], writes=[brt2])
                    P.op("pool", lambda e: e.tensor_tensor(out=krT[s][64:96, 0:ncol], in0=rt1[64:96, 0:ncol], in1=rt2[64:96, 0:ncol], op=ALU.add), reads=[brt1, brt2], writes=[bkrT[s]])
                    P.load["dve"] += 1.0
                    for h in range(8):
                        P.dma("sp", lambda e, h=h: e.dma_start(out=KM[h, 64:96, col0:col0 + ncol], in_=krT[s][64:96, 0:ncol]), "krT%d" % s, reads=[bkrT[s]], writes=[bKM])
                    pa, bpa = next_f()
                    for c in range(8):
                        P.op("pe", lambda e, c=c, pa=pa: e.matmul(pa[0:8, 0:ntok], lhsT=wfl[:, c, :], rhs=xT[s][:, c, 0:ntok], start=(c == 0), stop=(c == 7)), reads=rd_xT + [bwfl], writes=[bpa], inc=(c == 7))
                    P.op("act", lambda e, pa=pa: e.activation(out=lf[:, 0:ncol], in_=pa[0:8, 0:ncol], func=AF.Exp, bias=nbf[:, 0:1], scale=-1.0), reads=[bpa, bnbf], writes=[blf])
                    P.op("act", lambda e: e.activation(out=lf[:, 0:ncol], in_=lf[:, 0:ncol], func=AF.Ln, bias=1.0, scale=1.0), reads=[blf], writes=[blf])
                    init = carry["ap"] if carry["ap"] is not None else 0.0
                    rds = [blf, bz8] + ([carry["buf"]] if carry["buf"] is not None else [])
                    P.op("dve", lambda e, init=init: e.tensor_tensor_scan(out=negc[s][:, 0:ncol], data0=lf[:, 0:ncol], data1=zeros8[:, 0:ncol], initial=init, op0=ALU.add, op1=ALU.add), reads=rds, writes=[bnegc[s]])
                    carry["ap"] = negc[s][:, ncol - 1:ncol]
                    carry["buf"] = bnegc[s]
                    P.dma("sp", lambda e: e.dma_start(out=CT[:, col0:col0 + ncol], in_=negc[s][:, 0:ncol]), "negc%d" % s, reads=[bnegc[s]], writes=[bCT])
                    P.op("dve", lambda e: e.tensor_copy(out=pcs[s][:, 0, 0:ncol], in_=negc[s][:, 0:ncol]), reads=[bnegc[s]], writes=[bpcs[s]])
                    P.op("dve", lambda e: e.tensor_tensor(out=r1[:, 0:ncol], in0=negc[s][:, 0:ncol], in1=pcs[s][:, 0, 0:ncol], op=ALU.subtract), reads=[bnegc[s], bpcs[s]], writes=[br1])
                    P.op("dve", lambda e: e.tensor_copy(out=pcs[s][:, 1, 0:ncol], in_=r1[:, 0:ncol]), reads=[br1, bpcs[s]], writes=[bpcs[s]])
                    P.op("dve", lambda e: e.tensor_tensor(out=r2[:, 0:ncol], in0=r1[:, 0:ncol], in1=pcs[s][:, 1, 0:ncol], op=ALU.subtract), reads=[br1, bpcs[s]], writes=[br2])
                    P.op("dve", lambda e: e.tensor_copy(out=pcs[s][:, 2, 0:ncol], in_=r2[:, 0:ncol]), reads=[br2, bpcs[s]], writes=[bpcs[s]])
                    P.load["dve"] += 1.5
                    P.dma("sp", lambda e: e.dma_start(out=KF[:, 65:68, col0:col0 + ncol], in_=pcs[s][:, :, 0:ncol]), "pcs%d" % s, reads=[bpcs[s]], writes=[bKF])

                groups = [([(meta, NMETA)], 0, NMETA, 0)]
                for g in range(16):
                    groups.append(([(x_all[g * 512 + t * 128:g * 512 + (t + 1) * 128, :], 128) for t in range(4)], NMETA + g * 512, 512, 1 + 4 * g))
                pending = issue_loads(groups[0][0])
                norm_stage(pending)
                for gi, (srcs_, col0_, ncol_, vt0_) in enumerate(groups):
                    cur = pending
                    pending = issue_loads(groups[gi + 1][0]) if gi + 1 < len(groups) else None
                    gen = kside(cur, col0_, ncol_, vt0_)
                    next(gen)
                    if pending is not None:
                        norm_stage(pending)
                    for _ in gen:
                        pass
                for h in range(8):
                    for c0 in range(0, NK, 4104):
                        P.dma("sp", lambda e: e.dma_start(out=KF[h, 64:65, c0:c0 + 4104].rearrange("a (k c) -> (a k) c", k=9), in_=ones_b[0:9, 0:456]), "ones_b", reads=[bones], writes=[bKF])
                    P.dma("sp", lambda e: e.dma_start(out=QF[h, 65:68, :].rearrange("r (k c) -> (r k) c", k=4), in_=ones_b[0:12, 0:512]), "ones_b", reads=[bones], writes=[bQF])

            P.barrier()

            with contextlib.ExitStack() as e2:
                wcq = sb(e2, "wcq", [128, 8, 512], BF16); bwcq = Buf("wcq")
                wfq = sb(e2, "wfq", [128, 8, 512], BF16); bwfq = Buf("wfq")
                wg = sb(e2, "wg", [128, 8, 2048], BF16); bwg = Buf("wg")
                wq = sb(e2, "wq", [128, 4, 768], BF16); bwq = Buf("wq")
                wqs = sb(e2, "wqs", [128, 4, 768], BF16); bwqs = Buf("wqs")
                w_in_c = w_in.rearrange("(c p) n -> p c n", p=128)
                load_w(wcq[:], w_in_c[:, :, O_CQ:O_CQ + 512], "wcq", bwcq)
                load_w(wfq[:], w_in_c[:, :, O_FQ:O_FQ + 512], "wfq", bwfq)
                for c in range(8):
                    load_w(wg[:, c, :], w_in_c[:, c, O_GA:O_GA + 2048], "wg", bwg)
                wq_c = w_q_up.rearrange("(c p) n -> p c n", p=128)
                load_w(wq[:], wq_c, "wq", bwq)
                load_w(wqs[:], wq_c, "wqs", bwqs)
                wq_h = w_q_up.rearrange("(c p) (h d) -> p c h d", p=128, d=96)
                wqs_h = wqs[:, :, :].rearrange("p c (h d) -> p c h d", d=96)
                for c in range(4):
                    load_w(wqs_h[:, c, :, 64:80], wq_h[:, c, :, 80:96], "wqs", bwqs)
                    load_w(wqs_h[:, c, :, 80:96], wq_h[:, c, :, 64:80], "wqs", bwqs)
                qtab = sb(e2, "qtab", [96, 2, NQ], F32); bqtab = Buf("qtab")
                P.dma("sp", lambda e: e.dma_start(out=qtab[64:96, 0, :], in_=cosq), "qtab", writes=[bqtab])
                P.dma("sp", lambda e: e.dma_start(out=qtab[64:96, 1, :], in_=sinq), "qtab", writes=[bqtab])
                cqn = [sb(e2, "cqn%d" % i, [128, 512], BF16) for i in range(2)]; bcqn = [Buf("cqn%d" % i) for i in range(2)]
                cqnT = sb(e2, "cqnT", [128, 4, 512], BF16)
                bcqnT = [Buf("cqnT%d" % t) for t in range(4)]
                qm = [sb(e2, "qm%d" % i, [96, 8, 512], BF16) for i in range(2)]
                bqm = [Buf("qm%d" % i) for i in range(2)]
                qf = [sb(e2, "qf%d" % i, [128, 4, 512], BF16) for i in range(2)]
                bqf = [Buf("qf%d" % i) for i in range(2)]
                qt1 = sb(e2, "qt1", [96, 512], F32); bqt1 = Buf("qt1")
                qt2 = sb(e2, "qt2", [96, 512], F32); bqt2 = Buf("qt2")
                cq32 = sb(e2, "cq32", [8, 512], F32); bcq32 = Buf("cq32")
                cqb = sb(e2, "cqb", [8, 512], BF16); bcqb = Buf("cqb")
                sg = [sb(e2, "sg%d" % i, [128, 512], BF16) for i in range(3)]
                bsg = [Buf("sg%d" % i) for i in range(3)]
                st["grp"] = 0
                qsrc = [[(xq[m * 512 + t * 128:m * 512 + (t + 1) * 128, :], 128) for t in range(4)] for m in range(4)]
                pending = issue_loads(qsrc[0])
                norm_stage(pending)
                for m in range(4):
                    s = m % 2
                    mc = slice(m * 512, (m + 1) * 512)
                    cur = pending
                    pending = issue_loads(qsrc[m + 1]) if m + 1 < 4 else None
                    tr_stage(4, s)
                    rd_xT = [bxT[s][t] for t in range(4)]
                    def cq_front(t):
                        tc_ = slice(t * 128, (t + 1) * 128)
                        pa, bpa = next_f()
                        for c in range(8):
                            P.op("pe", lambda e: e.matmul(pa[:, :], lhsT=xT[s][:, c, tc_], rhs=wcq[:, c, :], start=(c == 0), stop=(c == 7)), reads=[bxT[s][t], bwcq], writes=[bpa], inc=(c == 7))
                        i2 = 4 + t % 2
                        P.op("act", lambda e: e.activation(out=junk[:, 0:512], in_=pa[:, :], func=AF.Square, accum_out=ssq[i2][:]), reads=[bpa], writes=[bjunk, bssq[i2]])
                        rstd_from_ssq(ssq[i2][:], bssq[i2], rstd[i2][:], brstd[i2], 512)
                        P.op("dve", lambda e: e.scalar_tensor_tensor(out=cqn[t % 2][:], in0=pa[:, :], scalar=rstd[i2][:, 0:1], in1=gq[:], op0=ALU.mult, op1=ALU.mult), reads=[bpa, brstd[i2], bgq], writes=[bcqn[t % 2]])

                    def cq_back(t):
                        tc_ = slice(t * 128, (t + 1) * 128)
                        pt_, bpt_ = next_bf()
                        for c in range(4):
                            P.op("pe", lambda e: e.transpose(out=pt_[:, c * 128:(c + 1) * 128], in_=cqn[t % 2][:, c * 128:(c + 1) * 128], identity=identb[:]), reads=[bcqn[t % 2], bidb], writes=[bpt_], inc=(c == 3))
                        copy(cqnT[:, :, tc_], pt_[:, 0:512].rearrange("p (c k) -> p c k", c=4), [bpt_], [bcqnT[t]], 512)

                    def gate(jg):
                        pa, bpa = next_f()
                        for c in range(8):
                            P.op("pe", lambda e: e.matmul(pa[:, :], lhsT=wg[:, c, jg * 128:(jg + 1) * 128], rhs=xT[s][:, c, :], start=(c == 0), stop=(c == 7)), reads=rd_xT + [bwg], writes=[bpa], inc=(c == 7))
                        k3 = jg % 3
                        P.op("act", lambda e: e.activation(out=sg[k3][:], in_=pa[:, :], func=AF.Sigmoid), reads=[bpa], writes=[bsg[k3]])
                        P.load["act"] += 0.7
                        P.dma("sp", lambda e: e.dma_start(out=GS[jg, :, mc], in_=sg[k3][:]), "sg%d" % k3, reads=[bsg[k3]], writes=[bGS])

                    cq_front(0)
                    for t in range(4):
                        if t + 1 < 4:
                            cq_front(t + 1)
                        for jg in range(4 * t, 4 * t + 4):
                            gate(jg)
                        cq_back(t)
                    if pending is not None:
                        norm_stage(pending)
                    for h in range(8):
                        pa, bpa = next_f()
                        pb, bpb = next_f()
                        for (pp, bpp, ww, bww) in ((pa, bpa, wq, bwq), (pb, bpb, wqs, bwqs)):
                            for c in range(4):
                                P.op("pe", lambda e, c=c, pp=pp, ww=ww, h=h: e.matmul(pp[0:96, :], lhsT=ww[:, c, h * 96:(h + 1) * 96], rhs=cqnT[:, c, :], start=(c == 0), stop=(c == 3)), reads=bcqnT + [bww], writes=[bpp], inc=(c == 3))
                        copy(qm[s][0:64, h, :], pa[0:64, :], [bpa], [bqm[s]], 512, scale=SC_MLA)
                        P.op("dve", lambda e, pa=pa: e.tensor_tensor(out=qt1[64:96, :], in0=pa[64:96, :], in1=qtab[64:96, 0, mc], op=ALU.mult), reads=[bpa, bqtab], writes=[bqt1])
                        P.op("dve", lambda e, pb=pb: e.tensor_tensor(out=qt2[64:96, :], in0=pb[64:96, :], in1=qtab[64:96, 1, mc], op=ALU.mult), reads=[bpb, bqtab], writes=[bqt2])
                        P.op("pool", lambda e, h=h: e.tensor_tensor(out=qm[s][64:96, h, :], in0=qt1[64:96, :], in1=qt2[64:96, :], op=ALU.add), reads=[bqt1, bqt2], writes=[bqm[s]])
                        P.load["dve"] += 0.6
                    P.dma("sp", lambda e: e.dma_start(out=QM[:, :, mc].rearrange("h r c -> r h c"), in_=qm[s][:, :, :]), "qm%d" % s, reads=[bqm[s]], writes=[bQM])
                    for pr in range(4):
                        pa, bpa = next_f()
                        for c in range(8):
                            P.op("pe", lambda e, c=c, pa=pa, pr=pr: e.matmul(pa[:, :], lhsT=wfq[:, c, pr * 128:(pr + 1) * 128], rhs=xT[s][:, c, :], start=(c == 0), stop=(c == 7)), reads=rd_xT + [bwfq], writes=[bpa], inc=(c == 7))
                        copy(qf[s][:, pr, :], pa[:, :], [bpa], [bqf[s]], 512, scale=SC_FOX)
                    for two in range(2):
                        P.dma("sp", lambda e, two=two: e.dma_start(out=QF[:, 0:64, mc].rearrange("(p two) r c -> two r p c", two=2)[two], in_=qf[s][two * 64:(two + 1) * 64, :, :]), "qf%d" % s, reads=[bqf[s]], writes=[bQF])
                    for t in range(4):
                        def gat(e, t=t, m=m):
                            j = nc.partition_id() % 4
                            return e.dma_start(out=cq32[:, t * 128:(t + 1) * 128], in_=CT[:, bass.ds(j * 128 + NMETA + 512 * (4 * m + t), 128)])
                        P.dma("sp", gat, "cq32", reads=[bCT], writes=[bcq32], deferred=True)
                    P.op("dve", lambda e: e.tensor_scalar(out=cqb[:], in0=cq32[:], scalar1=-1.0, scalar2=None, op0=ALU.mult), reads=[bcq32], writes=[bcqb])
                    P.dma("sp", lambda e: e.dma_start(out=QF[:, 64, mc], in_=cqb[:]), "cqb", reads=[bcqb], writes=[bQF])
            P.barrier()

        acc = sb(es, "acc", [128, NT, D], F32)
        bacc = [[Buf("acc%d_%d" % (t, hf)) for hf in range(2)] for t in range(NT)]
        ey = contextlib.ExitStack()
        ybuf = sb(ey, "ybuf", [128, NT, D], BF16)
        bybuf = [Buf("ybuf%d" % t) for t in range(NT)]
        with contextlib.ExitStack() as eb:
            ksb = [sb(eb, "ksb%d" % i, [96, NK], BF16) for i in range(2)]
            bksb = [Buf("ksb%d" % i) for i in range(2)]
            vsb = [sb(eb, "vsb%d" % i, [128, NKT, 65], BF16) for i in range(2)]
            bvsb = [Buf("vsb%d" % i) for i in range(2)]
            qsb = [sb(eb, "qsb%d" % i, [96, NQ], BF16) for i in range(2)]
            bqsb = [Buf("qsb%d" % i) for i in range(2)]
            mk = sb(eb, "mk", [128, 4, 128], BF16); bmk = Buf("mk")
            P.dma("pool", lambda e: e.dma_start(out=mk[:], in_=masks.rearrange("d p c -> p d c")), "mk", writes=[bmk])
            ptb = [sb(eb, "ptb%d" % i, [128, 2, 512], BF16) for i in range(3)]
            bptb = [Buf("ptb%d" % i) for i in range(3)]
            sbank = [pS[i // 2][:, (i % 2) * 512:(i % 2 + 1) * 512] for i in range(6)]
            bsbank = [bpf[0], bpf[1], bpf[2], bpf[3], bpbf[0], bpbf[1]]
            fillv = pbf[0][:, :].bitcast(F32)
            rden = sb(eb, "rden", [128, 4], F32); brden = Buf("rden")
            rounds = []
            for hh in range(16):
                for m in range(4):
                    k_ = len(rounds) % 2
                    rounds.append(dict(hh=hh, m=m, mla=hh < 8, h=hh % 8, dk=96 if hh < 8 else 68, slot=hh % 2, po=pf[4 + k_], bpo=bpf[4 + k_]))
            flat = []
            for r in rounds:
                nkt = 16 * r["m"] + 17
                us = [(0,)] + [(k, k + 1) for k in range(1, nkt, 2)]
                for ui, u in enumerate(us):
                    flat.append((r, u, ui == 0, ui == len(us) - 1))

            def loads(hh):
                mla = hh < 8
                h = hh % 8
                dk = 96 if mla else 68
                slot = hh % 2
                Ks, Vs, Qs = (KM, VM, QM) if mla else (KF, VF, QF)
                bKs, bVs, bQs = (bKM, bVM, bQM) if mla else (bKF, bVF, bQF)
                for c0 in range(0, NK, 2052):
                    P.dma("sp", lambda e: e.dma_start(out=ksb[slot][0:dk, c0:c0 + 2052], in_=Ks[h, :, c0:c0 + 2052]), "ksb%d" % slot, reads=[bKs], writes=[bksb[slot]])
                P.dma("sp", lambda e: e.dma_start(out=vsb[slot][:], in_=Vs[h]), "vsb%d" % slot, reads=[bVs], writes=[bvsb[slot]])
                P.dma("sp", lambda e: e.dma_start(out=qsb[slot][0:dk, :], in_=Qs[h]), "qsb%d" % slot, reads=[bQs], writes=[bqsb[slot]])

            def geom(m, kt):
                i0 = max(0, -(-(kt - 16 * m - 4) // 4))
                kp = NMETA if kt == 0 else 128
                kc0 = 0 if kt == 0 else NMETA + 128 * (kt - 1)
                return i0, kp, kc0

            def emit_S(r, kt, ps_, bps_):
                m, slot, dk = r["m"], r["slot"], r["dk"]
                i0, kp, kc0 = geom(m, kt)
                msk = [(i, kt - (16 * m + 4 * i + 1)) for i in range(i0, 4) if 0 <= kt - (16 * m + 4 * i + 1) <= 3]
                P.op("pe", lambda e: e.matmul(ps_[0:kp, i0 * 128:512], lhsT=ksb[slot][0:dk, kc0:kc0 + kp], rhs=qsb[slot][0:dk, m * 512 + i0 * 128:(m + 1) * 512], start=True, stop=(len(msk) == 0), skip_group_check=True), reads=[bksb[slot], bqsb[slot]], writes=[bps_], inc=(len(msk) == 0))
                for n_, (i, d) in enumerate(msk):
                    P.op("pe", lambda e: e.matmul(ps_[0:128, i * 128:(i + 1) * 128], lhsT=identb[:, :], rhs=mk[:, d, :], start=False, stop=(n_ == len(msk) - 1), skip_group_check=True), reads=[bidb, bmk], writes=[bps_], inc=(n_ == len(msk) - 1))

            def emit_unit_S(fi):
                r, u, _, _ = flat[fi]
                sl3 = fi % 3
                for jj, kt in enumerate(u):
                    emit_S(r, kt, sbank[2 * sl3 + jj], bsbank[2 * sl3 + jj])

            loads(0)
            emit_unit_S(0)
            emit_unit_S(1)
            for fi, (r, unit, first, last) in enumerate(flat):
                m, slot, po, bpo = r["m"], r["slot"], r["po"], r["bpo"]
                if first and m == 0 and r["hh"] + 1 < 16:
                    loads(r["hh"] + 1)
                if fi + 2 < len(flat):
                    emit_unit_S(fi + 2)
                sl3 = fi % 3
                i0, kp, _ = geom(m, unit[0])
                assert all(geom(m, kt)[0] == i0 for kt in unit)
                pt_, bpt_ = ptb[sl3], bptb[sl3]
                rdS = [bsbank[2 * sl3 + jj] for jj in range(len(unit))]
                if len(unit) == 1:
                    P.op("act", lambda e: e.activation(out=pt_[0:kp, 0, i0 * 128:512], in_=sbank[2 * sl3][0:kp, i0 * 128:512], func=AF.Exp), reads=rdS, writes=[bpt_])
                else:
                    P.op("act", lambda e: e.activation(out=pt_[:, :, i0 * 128:512], in_=pS[sl3][:, :].rearrange("p (b c) -> p b c", b=2)[:, :, i0 * 128:512], func=AF.Exp), reads=rdS, writes=[bpt_])
                for jj, kt in enumerate(unit):
                    if FILLER and kt % FILLER == 0 and kt > 0:
                        P.op("pe", lambda e: e.matmul(po[:, 260:512], lhsT=identb[:, :], rhs=ones_b[:, 0:252], start=False, stop=False, skip_group_check=True), inc=False)
                    for i in range(i0, 4):
                        P.op("pe", lambda e: e.matmul(po[:, i * 65:(i + 1) * 65], lhsT=pt_[0:kp, jj, i * 128:(i + 1) * 128], rhs=vsb[slot][0:kp, kt, :], start=(kt == 0 and i == 0), stop=(kt == 16 * m + 4 * i + 4), skip_group_check=True), reads=[bpt_, bvsb[slot]], writes=[bpo], inc=(i == 3))
                if last:
                    pov = po[:, 0:260].rearrange("p (i d) -> p i d", i=4)
                    P.op("dve", lambda e: e.reciprocal(out=rden[:], in_=pov[:, :, 64]), reads=[bpo], writes=[brden])
                    col = (0 if r["mla"] else 512) + r["h"] * 64
                    for i in range(4):
                        P.op("dve", lambda e: e.tensor_scalar(out=ybuf[:, 4 * m + i, col:col + 64], in0=pov[:, i, 0:64], scalar1=rden[:, i:i + 1], scalar2=None, op0=ALU.mult), reads=[bpo, brden], writes=[bybuf[4 * m + i]])
            P.barrier()
        if debug:
            bYD = Buf("YD", acc=True)
            for t in range(NT):
                P.dma("sp", lambda e, t=t: e.dma_start(out=YD[t * 128:(t + 1) * 128, :], in_=ybuf[:, t, :]), "ybuf", reads=[bybuf[t]], writes=[bYD])

        with contextlib.ExitStack() as ec:
            wmo = sb(ec, "wmo", [128, 4, D], BF16); bwmo = Buf("wmo")
            wfo = sb(ec, "wfo", [128, 4, D], BF16); bwfo = Buf("wfo")
            wo = sb(ec, "wo", [128, 8, D], BF16); bwo = Buf("wo")
            load_w(wmo[:], w_mla_out.rearrange("(c p) n -> p c n", p=128), "wmo", bwmo)
            load_w(wfo[:], w_fox_out.rearrange("(c p) n -> p c n", p=128), "wfo", bwfo)
            for c in range(8):
                load_w(wo[:, c, :], w_out[c * 128:(c + 1) * 128, :], "wo", bwo)
            yT = sb(ec, "yT", [128, 8, 512], BF16)
            byT = [Buf("yT%d" % c) for c in range(8)]
            ga = [sb(ec, "ga%d" % i, [128, 16, 512], BF16) for i in range(2)]
            bga = [Buf("ga%d" % i) for i in range(2)]
            mT = sb(ec, "mT", [128, 8, 512], BF16)
            bmT = [Buf("mT%d" % c) for c in range(8)]
            mt1 = sb(ec, "mt1", [128, 512], F32); bmt1 = Buf("mt1")
            mt2 = sb(ec, "mt2", [128, 512], F32); bmt2 = Buf("mt2")
            xr = [sb(ec, "xr%d" % i, [128, D], F32) for i in range(2)]
            bxr = [Buf("xr%d" % i) for i in range(2)]
            for m in range(4):
                mc = slice(m * 512, (m + 1) * 512)
                gk = m % 2
                if m == 0:
                    P.dma("sp", lambda e: e.dma_start(out=ga[0][:], in_=GS[:, :, 0:512].rearrange("j p c -> p j c")), "ga0", reads=[bGS], writes=[bga[0]])
                if m + 1 < 4:
                    P.dma("sp", lambda e: e.dma_start(out=ga[1 - gk][:], in_=GS[:, :, (m + 1) * 512:(m + 2) * 512].rearrange("j p c -> p j c")), "ga%d" % (1 - gk), reads=[bGS], writes=[bga[1 - gk]])
                for c in range(8):
                    pt_, bpt_ = next_bf()
                    for t in range(4):
                        P.op("pe", lambda e, c=c, t=t, pt_=pt_: e.transpose(out=pt_[:, t * 128:(t + 1) * 128], in_=ybuf[:, 4 * m + t, c * 128:(c + 1) * 128], identity=identb[:]), reads=[bybuf[4 * m + t], bidb], writes=[bpt_], inc=(t == 3))
                    copy(yT[:, c, :], pt_[:, 0:512], [bpt_], [byT[c]], 512)
                for jc in range(8):
                    pa, bpa = next_f()
                    pb, bpb = next_f()
                    for c in range(4):
                        P.op("pe", lambda e, c=c, pa=pa, jc=jc: e.matmul(pa[:, :], lhsT=wmo[:, c, jc * 128:(jc + 1) * 128], rhs=yT[:, c, :], start=(c == 0), stop=(c == 3)), reads=byT[0:4] + [bwmo], writes=[bpa], inc=(c == 3))
                    for c in range(4):
                        P.op("pe", lambda e, c=c, pb=pb, jc=jc: e.matmul(pb[:, :], lhsT=wfo[:, c, jc * 128:(jc + 1) * 128], rhs=yT[:, 4 + c, :], start=(c == 0), stop=(c == 3)), reads=byT[4:8] + [bwfo], writes=[bpb], inc=(c == 3))
                    P.op("dve", lambda e, pa=pa, jc=jc: e.tensor_tensor(out=mt1[:], in0=pa[:, :], in1=ga[gk][:, jc, :], op=ALU.mult), reads=[bpa, bga[gk]], writes=[bmt1])
                    P.op("dve", lambda e, pb=pb, jc=jc: e.tensor_tensor(out=mt2[:], in0=pb[:, :], in1=ga[gk][:, 8 + jc, :], op=ALU.mult), reads=[bpb, bga[gk]], writes=[bmt2])
                    P.op("pool", lambda e, jc=jc: e.tensor_tensor(out=mT[:, jc, :], in0=mt1[:], in1=mt2[:], op=ALU.add), reads=[bmt1, bmt2], writes=[bmT[jc]])
                for t in range(4):
                    tt = 4 * m + t
                    k2 = tt % 2
                    P.dma("sp", lambda e, tt=tt, k2=k2: e.dma_start(out=xr[k2][:], in_=xq[tt * 128:(tt + 1) * 128, :]), "xr%d" % k2, writes=[bxr[k2]])
                    for hf in range(2):
                        pa, bpa = next_f()
                        for c in range(8):
                            P.op("pe", lambda e, c=c, pa=pa, t=t, hf=hf: e.matmul(pa[:, :], lhsT=mT[:, c, t * 128:(t + 1) * 128], rhs=wo[:, c, hf * 512:(hf + 1) * 512], start=(c == 0), stop=(c == 7)), reads=bmT + [bwo], writes=[bpa], inc=(c == 7))
                        P.op("dve", lambda e, pa=pa, tt=tt, hf=hf, k2=k2: e.tensor_tensor(out=acc[:, tt, hf * 512:(hf + 1) * 512], in0=pa[:, :], in1=xr[k2][:, hf * 512:(hf + 1) * 512], op=ALU.add), reads=[bpa, bxr[k2]], writes=[bacc[tt][hf]])
            P.barrier()

        ey.close()
        with contextlib.ExitStack() as em:
            er = contextlib.ExitStack()
            gffn = sb(em, "gffn", [128, D], F32); bgffn = Buf("gffn")
            gfin = sb(em, "gfin", [128, D], F32); bgfin = Buf("gfin")
            bcast_load(gffn, bgffn, ffn_norm, "gffn")
            bcast_load(gfin, bgfin, final_norm, "gfin")
            sel = sb(em, "sel", [32, 32, 128], BF16); bsel = Buf("sel")
            for e_ in range(32):
                P.op("pool", lambda e, e_=e_: e.tensor_copy(out=sel[:, e_, :], in_=ident32[0:32, e_:e_ + 1].to_broadcast([32, 128])), reads=[bid32], writes=[bsel])
            hT = sb(em, "hT", [128, 8, NQ], BF16)
            bhT = [Buf("hT%d" % t) for t in range(NT)]
            combT = sb(em, "combT", [32, NQ], BF16)
            bcombT = [Buf("combT%d" % t) for t in range(NT)]
            ER = E_PER_ROUND
            ew0 = contextlib.ExitStack()
            wgt = [sb(ew0, "wgt%d" % i, [128, ER, 8, 256], BF16) for i in range(2)]
            bwgt = [Buf("wgt%d" % i) for i in range(2)]
            wup = [sb(ew0, "wup%d" % i, [128, ER, 8, 256], BF16) for i in range(2)]
            bwup = [Buf("wup%d" % i) for i in range(2)]
            wdn = [sb(ew0, "wdn%d" % i, [128, ER, 2, D], BF16) for i in range(2)]
            bwdn = [Buf("wdn%d" % i) for i in range(2)]
            nround = 32 // ER

            def load_round(r):
                sl = r % 2
                for x_ in range(ER):
                    e_ = r * ER + x_
                    P.dma("pool", lambda e, e_=e_, x_=x_: e.dma_start(out=wgt[sl][:, x_, :, :], in_=w_gate[e_].rearrange("(c p) f -> p c f", p=128)), "wgt%d" % sl, writes=[bwgt[sl]])
                    P.dma("pool", lambda e, e_=e_, x_=x_: e.dma_start(out=wup[sl][:, x_, :, :], in_=w_up[e_].rearrange("(c p) f -> p c f", p=128)), "wup%d" % sl, writes=[bwup[sl]])
                    P.dma("pool", lambda e, e_=e_, x_=x_: e.dma_start(out=wdn[sl][:, x_, :, :], in_=w_down[e_].rearrange("(c p) d -> p c d", p=128)), "wdn%d" % sl, writes=[bwdn[sl]])

            load_round(0)
            wr = sb(er, "wr", [128, 8, 36], BF16); bwr = Buf("wr")
            load_w(wr[:, :, 0:4], w_grt.rearrange("(c p) n -> p c n", p=128), "wr", bwr)
            load_w(wr[:, :, 4:36], w_ert.rearrange("(c p) n -> p c n", p=128), "wr", bwr)
            rb = sb(er, "rb", [128, 36], F32); brb = Buf("rb")
            P.dma("sp", lambda e: e.dma_start(out=rb[:, 0:4], in_=b_grt.partition_broadcast(128)), "rb", writes=[brb])
            P.dma("sp", lambda e: e.dma_start(out=rb[:, 4:36], in_=b_ert.partition_broadcast(128)), "rb", writes=[brb])
            junk2 = sb(er, "junk2", [128, D], BF16); bjunk2 = Buf("junk2")
            hs = [sb(er, "hs%d" % i, [128, D], BF16) for i in range(2)]
            bhs = [Buf("hs%d" % i) for i in range(2)]
            ssq2 = [sb(er, "ssq2_%d" % i, [128, 1], F32) for i in range(2)]
            bssq2 = [Buf("ssq2_%d" % i) for i in range(2)]
            rstd2 = [sb(er, "rstd2_%d" % i, [128, 1], F32) for i in range(2)]
            brstd2 = [Buf("rstd2_%d" % i) for i in range(2)]
            lgA = sb(er, "lgA", [128, NT, 36], F32); blgA = [Buf("lgA%d" % t) for t in range(NT)]
            smA = sb(er, "smA", [128, NT, 8], F32); bsmA = [Buf("smA%d" % t) for t in range(NT)]
            ohA = sb(er, "ohA", [128, NT, 4], F32); bohA = [Buf("ohA%d" % t) for t in range(NT)]
            mlA = sb(er, "mlA", [128, NT, 32], F32); bmlA = [Buf("mlA%d" % t) for t in range(NT)]
            t8A = sb(er, "t8A", [128, NT, 8], F32); bt8A = [Buf("t8A%d" % t) for t in range(NT)]
            c1A = sb(er, "c1A", [128, NT, 32], F32); bc1A = [Buf("c1A%d" % t) for t in range(NT)]
            c2A = sb(er, "c2A", [128, NT, 32], F32); bc2A = [Buf("c2A%d" % t) for t in range(NT)]
            for t in range(NT):
                k2 = t % 2
                tc_ = slice(t * 128, (t + 1) * 128)
                P.op("act", lambda e, t=t, k2=k2: e.activation(out=junk2[:], in_=acc[:, t, :], func=AF.Square, accum_out=ssq2[k2][:]), reads=bacc[t], writes=[bjunk2, bssq2[k2]])
                rstd_from_ssq(ssq2[k2][:], bssq2[k2], rstd2[k2][:], brstd2[k2], D)
                P.op("dve", lambda e, t=t, k2=k2: e.scalar_tensor_tensor(out=hs[k2][:], in0=acc[:, t, :], scalar=rstd2[k2][:, 0:1], in1=gffn[:], op0=ALU.mult, op1=ALU.mult), reads=bacc[t] + [brstd2[k2], bgffn], writes=[bhs[k2]])
                pt_, bpt_ = next_bf()
                for c in range(8):
                    P.op("pe", lambda e, c=c, k2=k2, pt_=pt_: e.transpose(out=pt_[:, c * 128:(c + 1) * 128], in_=hs[k2][:, c * 128:(c + 1) * 128], identity=identb[:]), reads=[bhs[k2], bidb], writes=[bpt_], inc=(c == 7))
                copy(hT[:, :, tc_], pt_[:, :].rearrange("p (c k) -> p c k", c=8), [bpt_], [bhT[t]], 1024)
                pa, bpa = next_f()
                for c in range(8):
                    P.op("pe", lambda e, c=c, pa=pa, tc_=tc_: e.matmul(pa[:, 0:36], lhsT=hT[:, c, tc_], rhs=wr[:, c, :], start=(c == 0), stop=(c == 7)), reads=[bhT[t], bwr], writes=[bpa], inc=(c == 7))
                P.op("dve", lambda e: e.tensor_tensor(out=lgA[:, t, :], in0=pa[:, 0:36], in1=rb[:], op=ALU.add), reads=[bpa, brb], writes=[blgA[t]])

            def stage(fn):
                for t in range(NT):
                    fn(t)
            stage(lambda t: P.op("dve", lambda e: e.reduce_max(out=smA[:, t, 0:1], in_=lgA[:, t, 0:4], axis=AX.X), reads=[blgA[t]], writes=[bsmA[t]]))
            stage(lambda t: P.op("dve", lambda e: e.tensor_scalar(out=smA[:, t, 1:2], in0=smA[:, t, 0:1], scalar1=-1.0, scalar2=None, op0=ALU.mult), reads=[bsmA[t]], writes=[bsmA[t]]))
            stage(lambda t: P.op("act", lambda e: e.activation(out=ohA[:, t, :], in_=lgA[:, t, 0:4], func=AF.Exp, bias=smA[:, t, 1:2], scale=1.0, accum_out=smA[:, t, 2:3]), reads=[blgA[t], bsmA[t]], writes=[bohA[t], bsmA[t]]))
            stage(lambda t: P.op("dve", lambda e: e.reciprocal(out=smA[:, t, 3:4], in_=smA[:, t, 2:3]), reads=[bsmA[t]], writes=[bsmA[t]]))
            stage(lambda t: P.op("dve", lambda e: e.tensor_scalar(out=ohA[:, t, :], in0=lgA[:, t, 0:4], scalar1=smA[:, t, 0:1], scalar2=1e30, op0=ALU.is_lt, op1=ALU.mult), reads=[blgA[t], bsmA[t], bohA[t]], writes=[bohA[t]]))
            for g_ in range(4):
                stage(lambda t: P.op("dve", lambda e: e.tensor_scalar(out=mlA[:, t, g_ * 8:(g_ + 1) * 8], in0=lgA[:, t, 4 + g_ * 8:4 + (g_ + 1) * 8], scalar1=ohA[:, t, g_:g_ + 1], scalar2=None, op0=ALU.subtract), reads=[blgA[t], bohA[t], bmlA[t]], writes=[bmlA[t]]))
            stage(lambda t: P.op("dve", lambda e: e.max(out=t8A[:, t, :], in_=mlA[:, t, :]), reads=[bmlA[t]], writes=[bt8A[t]]))
            stage(lambda t: P.op("dve", lambda e: e.tensor_tensor(out=smA[:, t, 4:5], in0=t8A[:, t, 0:1], in1=t8A[:, t, 1:2], op=ALU.subtract), reads=[bt8A[t], bsmA[t]], writes=[bsmA[t]]))
            stage(lambda t: P.op("act", lambda e: e.activation(out=smA[:, t, 5:6], in_=smA[:, t, 4:5], func=AF.Sigmoid), reads=[bsmA[t]], writes=[bsmA[t]]))
            stage(lambda t: P.op("dve", lambda e: e.tensor_tensor(out=smA[:, t, 6:7], in0=smA[:, t, 5:6], in1=smA[:, t, 3:4], op=ALU.mult), reads=[bsmA[t]], writes=[bsmA[t]]))
            stage(lambda t: P.op("dve", lambda e: e.tensor_tensor(out=smA[:, t, 7:8], in0=smA[:, t, 3:4], in1=smA[:, t, 6:7], op=ALU.subtract), reads=[bsmA[t]], writes=[bsmA[t]]))
            stage(lambda t: P.op("dve", lambda e: e.tensor_scalar(out=c1A[:, t, :], in0=mlA[:, t, :], scalar1=t8A[:, t, 0:1], scalar2=smA[:, t, 6:7], op0=ALU.is_equal, op1=ALU.mult), reads=[bmlA[t], bt8A[t], bsmA[t]], writes=[bc1A[t]]))
            stage(lambda t: P.op("dve", lambda e: e.tensor_scalar(out=c2A[:, t, :], in0=mlA[:, t, :], scalar1=t8A[:, t, 1:2], scalar2=smA[:, t, 7:8], op0=ALU.is_equal, op1=ALU.mult), reads=[bmlA[t], bt8A[t], bsmA[t]], writes=[bc2A[t]]))
            stage(lambda t: P.op("dve", lambda e: e.tensor_tensor(out=c1A[:, t, :], in0=c1A[:, t, :], in1=c2A[:, t, :], op=ALU.add), reads=[bc1A[t], bc2A[t]], writes=[bc1A[t]]))

            def to_combT(t):
                pb, bpb = next_f()
                P.op("pe", lambda e: e.matmul(pb[0:32, 0:128], lhsT=c1A[:, t, :], rhs=ident32[:], start=True, stop=True), reads=[bc1A[t], bid32], writes=[bpb])
                copy(combT[:, t * 128:(t + 1) * 128], pb[0:32, 0:128], [bpb], [bcombT[t]], 128)
            stage(to_combT)

            P.barrier()
            er.close()
            ew = contextlib.ExitStack()
            ER = E_PER_ROUND
            cbs = [sb(ew, "cbs%d" % i, [128, 512], BF16) for i in range(2)]
            bcbs = [Buf("cbs%d" % i) for i in range(2)]
            sa = [sb(ew, "sa%d" % i, [128, 512], F32) for i in range(2)]
            bsa = [Buf("sa%d" % i) for i in range(2)]
            tm = [sb(ew, "tm%d" % i, [128, 512], F32) for i in range(2)]
            btm = [Buf("tm%d" % i) for i in range(2)]
            hm = [sb(ew, "hm%d" % i, [128, ER, 2, 512], BF16) for i in range(2)]
            bhm = [Buf("hm%d" % i) for i in range(2)]
            pdv = [pbf[0][:, :].bitcast(F32), pbf[1][:, :].bitcast(F32)]
            itc = {"it": 0, "pd": 0}

            def front(r, m):
                sl = r % 2
                mc = slice(m * 512, (m + 1) * 512)
                hsl = (r * 4 + m) % 2
                for x_ in range(ER):
                    e_ = r * ER + x_
                    pc, bpc = pf[4], bpf[4]
                    P.op("pe", lambda e: e.matmul(pc[:, :], lhsT=sel[:, e_, :], rhs=combT[:, mc], start=True, stop=True), reads=bcombT[4 * m:4 * m + 4] + [bsel], writes=[bpc])
                    k2 = (itc["it"] // 2) % 2
                    P.op("act", lambda e: e.copy(out=cbs[k2][:], in_=pc[:, :]), reads=[bpc], writes=[bcbs[k2]])
                    for fc in range(2):
                        kk = itc["it"] % 2
                        itc["it"] += 1
                        pa, bpa = pf[0 + kk], bpf[0 + kk]
                        pb, bpb = pf[2 + kk], bpf[2 + kk]
                        for c in range(8):
                            P.op("pe", lambda e: e.matmul(pa[:, :], lhsT=wgt[sl][:, x_, c, fc * 128:(fc + 1) * 128], rhs=hT[:, c, mc], start=(c == 0), stop=(c == 7)), reads=bhT[4 * m:4 * m + 4] + [bwgt[sl]], writes=[bpa], inc=(c == 7))
                        for c in range(8):
                            P.op("pe", lambda e: e.matmul(pb[:, :], lhsT=wup[sl][:, x_, c, fc * 128:(fc + 1) * 128], rhs=hT[:, c, mc], start=(c == 0), stop=(c == 7)), reads=bhT[4 * m:4 * m + 4] + [bwup[sl]], writes=[bpb], inc=(c == 7))
                        P.op("act", lambda e: e.activation(out=sa[kk][:], in_=pa[:, :], func=AF.Silu), reads=[bpa], writes=[bsa[kk]])
                        P.op("dve", lambda e: e.tensor_tensor(out=tm[kk][:], in0=pb[:, :], in1=sa[kk][:], op=ALU.mult), reads=[bpb, bsa[kk]], writes=[btm[kk]])
                        P.op("pool", lambda e: e.tensor_tensor(out=hm[hsl][:, x_, fc, :], in0=tm[kk][:], in1=cbs[k2][:], op=ALU.mult), reads=[btm[kk], bcbs[k2]], writes=[bhm[hsl]])

            def back(r, m):
                sl = r % 2
                hsl = (r * 4 + m) % 2
                for t in range(4):
                    tt = 4 * m + t
                    for hf in range(2):
                        kd = itc["pd"] % 2
                        itc["pd"] += 1
                        pd, bpd = pdv[kd], bpbf[kd]
                        n_ = 0
                        for x_ in range(ER):
                            for fc in range(2):
                                P.op("pe", lambda e: e.matmul(pd, lhsT=hm[hsl][:, x_, fc, t * 128:(t + 1) * 128], rhs=wdn[sl][:, x_, fc, hf * 512:(hf + 1) * 512], start=(n_ == 0), stop=(n_ == 2 * ER - 1)), reads=[bhm[hsl], bwdn[sl]], writes=[bpd], inc=(n_ == 2 * ER - 1))
                                n_ += 1
                        P.op("dve", lambda e: e.tensor_tensor(out=acc[:, tt, hf * 512:(hf + 1) * 512], in0=pd, in1=acc[:, tt, hf * 512:(hf + 1) * 512], op=ALU.add), reads=[bpd, bacc[tt][hf]], writes=[bacc[tt][hf]])

            units = [(r, m) for r in range(nround) for m in range(4)]
            for ui, (r, m) in enumerate(units):
                front(r, m)
                if ui > 0:
                    back(*units[ui - 1])
                if m == 0 and r + 1 < nround:
                    load_round(r + 1)
            back(*units[-1])

            P.barrier()
            ew.close()
            ew0.close()
            junk2 = sb(em, "junk2b", [128, D], BF16); bjunk2 = Buf("junk2b")
            ssq2 = [sb(em, "ssq3_%d" % i, [128, 1], F32) for i in range(2)]
            bssq2 = [Buf("ssq3_%d" % i) for i in range(2)]
            rstd2 = [sb(em, "rstd3_%d" % i, [128, 1], F32) for i in range(2)]
            brstd2 = [Buf("rstd3_%d" % i) for i in range(2)]
            ot = [sb(em, "ot%d" % i, [128, D], F32) for i in range(2)]
            bot = [Buf("ot%d" % i) for i in range(2)]
            for t in range(NT):
                k2 = t % 2
                P.op("act", lambda e, t=t, k2=k2: e.activation(out=junk2[:], in_=acc[:, t, :], func=AF.Square, accum_out=ssq2[k2][:]), reads=bacc[t], writes=[bjunk2, bssq2[k2]])
                rstd_from_ssq(ssq2[k2][:], bssq2[k2], rstd2[k2][:], brstd2[k2], D)
                P.op("dve", lambda e, t=t, k2=k2: e.scalar_tensor_tensor(out=ot[k2][:], in0=acc[:, t, :], scalar=rstd2[k2][:, 0:1], in1=gfin[:], op0=ALU.mult, op1=ALU.mult), reads=bacc[t] + [brstd2[k2], bgfin], writes=[bot[k2]])
                P.dma("sp", lambda e, t=t, k2=k2: e.dma_start(out=out[t * 128:(t + 1) * 128, :], in_=ot[k2][:]), "ot%d" % k2, reads=[bot[k2]], writes=[bOUT])
            P.barrier()

        P.emit(nc, es)
    return nc


def _host_consts(j):
    inv = (np.float32(10000.0) ** (-np.arange(0, 32, 2, dtype=np.float32) / np.float32(32))).astype(np.float32)
    pos = np.arange(NK, dtype=np.float32)
    ang = (pos[:, None] * inv[None, :]).astype(np.float32)
    cos = np.cos(ang).astype(np.float32).T
    sin = np.sin(ang).astype(np.float32).T
    cosk = np.concatenate([cos, cos], 0)
    sink = np.concatenate([-sin, sin], 0)
    qpos = np.concatenate([NMETA + 128 * (j + 4 * i) + np.arange(128) for i in range(NT)])
    cosq = (cosk[:, qpos] * np.float32(SC_MLA)).astype(np.float32)
    sinq = (sink[:, qpos] * np.float32(SC_MLA)).astype(np.float32)
    r = np.arange(128)
    tri = (r[None, :] >= r[:, None]).astype(np.float32)
    masks = np.full((4, 128, 128), -30000.0, np.float32)
    for d in range(4):
        if d < j:
            masks[d] = 0.0
        elif d == j:
            masks[d] = (tri - 1.0) * 30000.0
    return dict(cosk=np.ascontiguousarray(cosk), sink=np.ascontiguousarray(sink), cosq=np.ascontiguousarray(cosq),
                sinq=np.ascontiguousarray(sinq), masks=masks, ident=np.eye(128, dtype=np.float32))


def make_in_maps(inputs):
    f = lambda a: np.ascontiguousarray(np.asarray(a, dtype=np.float32))
    x = f(inputs["x"])
    shared = {
        "meta": f(inputs["meta"]), "attn_norm": f(inputs["attn_norm"]).reshape(D),
        "w_in": f(inputs["w_in"]).reshape(D, 4392), "b_forget": f(inputs["b_forget"]).reshape(8),
        "q_a_norm": f(inputs["q_a_norm"]).reshape(512), "w_q_up": f(inputs["w_q_up"]).reshape(512, 768),
        "kv_a_norm": f(inputs["kv_a_norm"]).reshape(256), "w_kv_up": f(inputs["w_kv_up"]).reshape(256, 1024),
        "w_mla_out": f(inputs["w_mla_out"]).reshape(512, D), "w_fox_out": f(inputs["w_fox_out"]).reshape(512, D),
        "w_out": f(inputs["w_out"]).reshape(D, D), "ffn_norm": f(inputs["ffn_norm"]).reshape(D),
        "w_group_router": f(inputs["w_group_router"]).reshape(D, 4), "b_group_router": f(inputs["b_group_router"]).reshape(4),
        "w_expert_router": f(inputs["w_expert_router"]).reshape(D, 32), "b_expert_router": f(inputs["b_expert_router"]).reshape(32),
        "w_gate": f(inputs["w_gate"]).reshape(32, D, 256), "w_up": f(inputs["w_up"]).reshape(32, D, 256),
        "w_down": f(inputs["w_down"]).reshape(32, 256, D), "final_norm": f(inputs["final_norm"]).reshape(D),
    }
    maps = []
    for c in range(8):
        b, j = c // 4, c % 4
        m = dict(shared)
        m["x_all"] = x[b]
        xb = x[b].reshape(64, 128, D)
        m["xq"] = np.ascontiguousarray(xb[j::4].reshape(NQ, D))
        m.update(_host_consts(j))
        maps.append(m)
    return maps


def kernel(**inputs):
    maps = make_in_maps(inputs)
    nc = build_program()
    res = run_bass_kernel_spmd(nc, maps, core_ids=list(range(8)))
    outp = np.zeros((2, 64, 128, D), np.float32)
    for c in range(8):
        b, j = c // 4, c % 4
        outp[b, j::4] = np.asarray(res.results[c]["out"], dtype=np.float32).reshape(NT, 128, D)
    return outp.reshape(2, SEQ, D)
```

```python
import contextlib
import numpy as np
import concourse.bass as bass
import concourse.mybir as mybir
from concourse.bass_utils import run_bass_kernel_spmd

F32 = mybir.dt.float32
BF16 = mybir.dt.bfloat16
AF = mybir.ActivationFunctionType
ALU = mybir.AluOpType
AX = mybir.AxisListType

D = 1024
SEQ = 8192
NMETA = 16
NK = NMETA + SEQ
NQ = 2048
NT = 16
NKT = 65
EPS = 1e-6
SC_MLA = 96 ** -0.5
SC_FOX = 0.125
O_CQ, O_CKV, O_KR, O_FQ, O_FK, O_FV, O_FL, O_GA, O_GB = 0, 512, 768, 800, 1312, 1824, 2336, 2344, 3368
E_PER_ROUND = 2
FILLER = 0


class Buf:
    __slots__ = ("name", "w", "r", "acc")

    def __init__(self, name, acc=False):
        self.name = name
        self.w = []
        self.r = []
        self.acc = acc


class _Rec:
    def __getattr__(self, name):
        def f(*a, **k):
            self.__dict__["call"] = (name, a, k)
            return self
        return f


class Prog:
    def __init__(self):
        self.streams = {k: [] for k in ("pe", "act", "dve", "pool", "sp")}
        self.cnt = {}
        self.seen = {k: {} for k in self.streams}
        self.load = {"act": 0.0, "dve": 0.0}

    def _deps(self, eng, reads, writes):
        need = {}
        for b in reads:
            for (s, v) in b.w:
                if need.get(s, 0) < v:
                    need[s] = v
        for b in writes:
            if not b.acc:
                for (s, v) in b.w:
                    if need.get(s, 0) < v:
                        need[s] = v
            for (s, v) in b.r:
                if need.get(s, 0) < v:
                    need[s] = v
        waits = []
        seen = self.seen[eng]
        for s, v in need.items():
            if eng == "pe" and s == "c_pe":
                continue
            if seen.get(s, 0) < v:
                waits.append((s, v))
                seen[s] = v
        return waits

    def _record(self, s, v, reads, writes):
        for b in reads:
            b.r.append((s, v))
            if len(b.r) > 64:
                m = {}
                for (ss, vv) in b.r:
                    if m.get(ss, 0) < vv:
                        m[ss] = vv
                b.r = list(m.items())
        for b in writes:
            if b.acc:
                b.w.append((s, v))
                if len(b.w) > 64:
                    m = {}
                    for (ss, vv) in b.w:
                        if m.get(ss, 0) < vv:
                            m[ss] = vv
                    b.w = list(m.items())
            else:
                b.w = [(s, v)]
                b.r = []

    @staticmethod
    def _bind(fn, deferred):
        if deferred:
            return fn
        rec = _Rec()
        fn(rec)
        name, a, k = rec.call
        return lambda e: getattr(e, name)(*a, **k)

    def op(self, eng, fn, reads=(), writes=(), inc=True):
        waits = self._deps(eng, reads, writes)
        s = "c_" + eng
        v = self.cnt.get(s, 0) + 1
        if inc:
            self.cnt[s] = v
        self.streams[eng].append((waits, self._bind(fn, False), s, 1 if inc else 0))
        self._record(s, v, reads, writes)

    def dma(self, eng, fn, key, reads=(), writes=(), deferred=False):
        waits = self._deps(eng, reads, writes)
        s = "d_" + key
        v = self.cnt.get(s, 0) + 16
        self.cnt[s] = v
        self.streams[eng].append((waits, self._bind(fn, deferred), s, 16))
        self._record(s, v, reads, writes)

    def pick(self, cost):
        e = "act" if self.load["act"] <= self.load["dve"] else "dve"
        self.load[e] += cost
        return e

    def barrier(self):
        for eng in self.streams:
            waits = []
            for s, v in self.cnt.items():
                if self.seen[eng].get(s, 0) < v:
                    waits.append((s, v))
                    self.seen[eng][s] = v
            if waits:
                self.streams[eng].append((waits, None, None, 0))

    def emit(self, nc, es):
        sems = {s: es.enter_context(nc.semaphore(s)) for s in self.cnt}
        engs = {"pe": "tensor", "act": "scalar", "dve": "vector", "pool": "gpsimd", "sp": "sync"}
        with nc.Block() as block:
            for k, attr in engs.items():
                stream = self.streams[k]

                def body(e, stream=stream):
                    for waits, fn, s, inc in stream:
                        for (ws, wv) in waits:
                            e.wait_ge(sems[ws], wv)
                        if fn is not None:
                            ins = fn(e)
                            if inc:
                                ins.then_inc(sems[s], inc)

                getattr(block, attr)(body)


def build_program(debug=False):
    nc = bass.Bass("TRN2", target_bir_lowering=False)
    nc.cache_partition_id()
    P = Prog()

    def dram_in(name, shape, dt=F32):
        return nc.dram_tensor(name, list(shape), dt, kind="ExternalInput").ap()

    x_all = dram_in("x_all", [SEQ, D])
    xq = dram_in("xq", [NQ, D])
    meta = dram_in("meta", [NMETA, D])
    attn_norm = dram_in("attn_norm", [D])
    w_in = dram_in("w_in", [D, 4392])
    b_forget = dram_in("b_forget", [8])
    q_a_norm = dram_in("q_a_norm", [512])
    w_q_up = dram_in("w_q_up", [512, 768])
    kv_a_norm = dram_in("kv_a_norm", [256])
    w_kv_up = dram_in("w_kv_up", [256, 1024])
    w_mla_out = dram_in("w_mla_out", [512, D])
    w_fox_out = dram_in("w_fox_out", [512, D])
    w_out = dram_in("w_out", [D, D])
    ffn_norm = dram_in("ffn_norm", [D])
    w_grt = dram_in("w_group_router", [D, 4])
    b_grt = dram_in("b_group_router", [4])
    w_ert = dram_in("w_expert_router", [D, 32])
    b_ert = dram_in("b_expert_router", [32])
    w_gate = dram_in("w_gate", [32, D, 256])
    w_up = dram_in("w_up", [32, D, 256])
    w_down = dram_in("w_down", [32, 256, D])
    final_norm = dram_in("final_norm", [D])
    cosk = dram_in("cosk", [32, NK])
    sink = dram_in("sink", [32, NK])
    cosq = dram_in("cosq", [32, NQ])
    sinq = dram_in("sinq", [32, NQ])
    masks = dram_in("masks", [4, 128, 128])
    ident_d = dram_in("ident", [128, 128])
    out = nc.dram_tensor("out", [NQ, D], F32, kind="ExternalOutput").ap()

    skind = "ExternalOutput" if debug else "Internal"

    def scratch(name, shape, dt=BF16):
        return nc.dram_tensor(name, list(shape), dt, kind=skind).ap()

    KM = scratch("KM", [8, 96, NK]); bKM = Buf("KM", acc=True)
    VM = scratch("VM", [8, 128, NKT, 65]); bVM = Buf("VM", acc=True)
    KF = scratch("KF", [8, 68, NK]); bKF = Buf("KF", acc=True)
    VF = scratch("VF", [8, 128, NKT, 65]); bVF = Buf("VF", acc=True)
    QM = scratch("QM", [8, 96, NQ]); bQM = Buf("QM", acc=True)
    QF = scratch("QF", [8, 68, NQ]); bQF = Buf("QF", acc=True)
    GS = scratch("GS", [16, 128, NQ]); bGS = Buf("GS", acc=True)
    CT = scratch("CT", [8, NK], F32); bCT = Buf("CT", acc=True)
    YD = scratch("YD", [NQ, D], BF16) if debug else None
    bOUT = Buf("out", acc=True)

    es = contextlib.ExitStack()
    with es:
        def sb(es_, name, shape, dt):
            return es_.enter_context(nc.sbuf_tensor(name, list(shape), dt))

        pS = [es.enter_context(nc.psum_tensor("pS%d" % i, [128, 1024], F32)) for i in range(3)]
        pf = [pS[0][:, 0:512], pS[0][:, 512:1024], pS[1][:, 0:512], pS[1][:, 512:1024]]
        pf += [es.enter_context(nc.psum_tensor("pf%d" % i, [128, 512], F32))[:, :] for i in (4, 5)]
        bpf = [Buf("pf%d" % i) for i in range(6)]
        pbf = [pS[2][:, 0:512].bitcast(BF16), pS[2][:, 512:1024].bitcast(BF16)]
        bpbf = [Buf("pbf%d" % i) for i in range(2)]
        rr = {"bf": 0, "f": 0}

        def next_bf():
            i = rr["bf"] % 2
            rr["bf"] += 1
            return pbf[i], bpbf[i]

        def next_f(lo=0, hi=6):
            i = lo + rr["f"] % (hi - lo)
            rr["f"] += 1
            return pf[i], bpf[i]

        ident32 = sb(es, "ident32", [128, 128], F32); bid32 = Buf("ident32")
        identb = sb(es, "identb", [128, 128], BF16); bidb = Buf("identb")
        epsD = sb(es, "epsD", [128, 1], F32); bepsD = Buf("epsD")
        ones_b = sb(es, "ones_b", [128, 512], BF16); bones = Buf("ones_b")
        zeros8 = sb(es, "zeros8", [8, 512], F32); bz8 = Buf("zeros8")
        P.dma("sp", lambda e: e.dma_start(out=ident32[:], in_=ident_d), "ident32", writes=[bid32])
        P.dma("pool", lambda e: e.dma_start(out=identb[:], in_=ident_d), "identb", writes=[bidb])
        P.op("pool", lambda e: e.memset(epsD[:], EPS), writes=[bepsD])
        P.op("pool", lambda e: e.memset(ones_b[:], 1.0), writes=[bones])
        P.op("pool", lambda e: e.memset(zeros8[:], 0.0), writes=[bz8])
        def rstd_from_ssq(ssq, bssq, rstd, brstd, n):
            P.op("act", lambda e: e.activation(out=rstd, in_=ssq, func=AF.Ln, bias=epsD[:, 0:1], scale=1.0 / n), reads=[bssq, bepsD], writes=[brstd])
            P.op("act", lambda e: e.activation(out=rstd, in_=rstd, func=AF.Exp, scale=-0.5), reads=[brstd], writes=[brstd])
            P.load["act"] += 0.6

        def copy(out_ap, in_ap, reads, writes, n, scale=None, eng=None):
            e_ = eng or P.pick(0.3 + n / 1000.0)
            if e_ == "act":
                if scale is None:
                    P.op("act", lambda e: e.copy(out=out_ap, in_=in_ap), reads=reads, writes=writes)
                else:
                    P.op("act", lambda e: e.mul(out=out_ap, in_=in_ap, mul=scale), reads=reads, writes=writes)
            else:
                if scale is None:
                    P.op("dve", lambda e: e.tensor_copy(out=out_ap, in_=in_ap), reads=reads, writes=writes)
                else:
                    P.op("dve", lambda e: e.tensor_scalar(out=out_ap, in0=in_ap, scalar1=scale, scalar2=None, op0=ALU.mult), reads=reads, writes=writes)

        def load_w(tile_ap, src_ap, key, btile):
            P.dma("pool", lambda e: e.dma_start(out=tile_ap, in_=src_ap), key, writes=[btile])

        def bcast_load(tile, btile, vec, key):
            P.dma("sp", lambda e: e.dma_start(out=tile[:], in_=vec.partition_broadcast(128)), key, writes=[btile])

        with contextlib.ExitStack() as ea:
            gattn = sb(ea, "gattn", [128, D], F32); bgattn = Buf("gattn")
            gkv = sb(ea, "gkv", [128, 256], F32); bgkv = Buf("gkv")
            gq = sb(ea, "gq", [128, 512], F32); bgq = Buf("gq")
            bcast_load(gattn, bgattn, attn_norm, "gattn")
            bcast_load(gkv, bgkv, kv_a_norm, "gkv")
            bcast_load(gq, bgq, q_a_norm, "gq")
            nbf = sb(ea, "nbf", [8, 1], F32); bnbf = Buf("nbf")
            P.dma("sp", lambda e: e.dma_start(out=nbf[:], in_=b_forget.rearrange("(h o) -> h o", o=1)), "nbf", writes=[bnbf])
            P.op("dve", lambda e: e.tensor_scalar(out=nbf[:], in0=nbf[:], scalar1=-1.0, scalar2=None, op0=ALU.mult), reads=[bnbf], writes=[bnbf])

            xin = [sb(ea, "xin%d" % i, [128, D], F32) for i in range(8)]
            bxin = [Buf("xin%d" % i) for i in range(8)]
            junk = sb(ea, "junk", [128, D], BF16); bjunk = Buf("junk")
            xs = [sb(ea, "xs%d" % i, [128, D], BF16) for i in range(4)]
            bxs = [Buf("xs%d" % i) for i in range(4)]
            ssq = [sb(ea, "ssq%d" % i, [128, 1], F32) for i in range(6)]
            bssq = [Buf("ssq%d" % i) for i in range(6)]
            rstd = [sb(ea, "rstd%d" % i, [128, 1], F32) for i in range(6)]
            brstd = [Buf("rstd%d" % i) for i in range(6)]
            xT = [sb(ea, "xT%d" % i, [128, 8, 512], BF16) for i in range(2)]
            bxT = [[Buf("xT%d_%d" % (i, t)) for t in range(4)] for i in range(2)]
            st = {"xin": 0, "xs": 0, "grp": 0}

            def issue_loads(srcs):
                res = []
                for (src, rows) in srcs:
                    i3 = st["xin"] % 8
                    st["xin"] += 1
                    if rows < 128:
                        P.op("pool", lambda e: e.memset(xin[i3][:], 0.0), writes=[bxin[i3]])
                    P.dma("sp", lambda e: e.dma_start(out=xin[i3][0:rows, :], in_=src), "xin%d" % i3, writes=[bxin[i3]])
                    res.append(i3)
                return res

            def norm_stage(loaded):
                for t, i3 in enumerate(loaded):
                    P.op("act", lambda e: e.activation(out=junk[:], in_=xin[i3][:], func=AF.Square, accum_out=ssq[t][:]), reads=[bxin[i3]], writes=[bjunk, bssq[t]])
                    P.load["act"] += 1.2
                    rstd_from_ssq(ssq[t][:], bssq[t], rstd[t][:], brstd[t], D)
                for t, i3 in enumerate(loaded):
                    P.op("dve", lambda e: e.scalar_tensor_tensor(out=xs[t][:], in0=xin[i3][:], scalar=rstd[t][:, 0:1], in1=gattn[:], op0=ALU.mult, op1=ALU.mult), reads=[bxin[i3], brstd[t], bgattn], writes=[bxs[t]])
                    P.load["dve"] += 1.2

            def tr_stage(nt_, slot):
                for t in range(nt_):
                    pt_, bpt_ = next_bf()
                    for c in range(8):
                        P.op("pe", lambda e: e.transpose(out=pt_[:, c * 128:(c + 1) * 128], in_=xs[t][:, c * 128:(c + 1) * 128], identity=identb[:]), reads=[bxs[t], bidb], writes=[bpt_], inc=(c == 7))
                    copy(xT[slot][:, :, t * 128:(t + 1) * 128], pt_[:, :].rearrange("p (c k) -> p c k", c=8), [bpt_], [bxT[slot][t]], 1024)

            with contextlib.ExitStack() as e1:
                wckv = sb(e1, "wckv", [128, 8, 256], BF16); bwckv = Buf("wckv")
                wkr = sb(e1, "wkr", [128, 8, 2, 96], BF16); bwkr = Buf("wkr")
                wfk = sb(e1, "wfk", [128, 8, 512], BF16); bwfk = Buf("wfk")
                wfv = sb(e1, "wfv", [128, 8, 512], BF16); bwfv = Buf("wfv")
                wkvk = sb(e1, "wkvk", [128, 2, 8, 64], BF16); bwkvk = Buf("wkvk")
                wkvv = sb(e1, "wkvv", [128, 2, 8, 64], BF16); bwkvv = Buf("wkvv")
                w_in_c = w_in.rearrange("(c p) n -> p c n", p=128)
                load_w(wckv[:], w_in_c[:, :, O_CKV:O_CKV + 256], "wckv", bwckv)
                P.op("pool", lambda e: e.memset(wkr[:], 0.0), writes=[bwkr])
                load_w(wkr[:, :, 0, 64:96], w_in_c[:, :, O_KR:O_KR + 32], "wkr", bwkr)
                load_w(wkr[:, :, 1, 64:80], w_in_c[:, :, O_KR + 16:O_KR + 32], "wkr", bwkr)
                load_w(wkr[:, :, 1, 80:96], w_in_c[:, :, O_KR:O_KR + 16], "wkr", bwkr)
                load_w(wfk[:], w_in_c[:, :, O_FK:O_FK + 512], "wfk", bwfk)
                load_w(wfv[:], w_in_c[:, :, O_FV:O_FV + 512], "wfv", bwfv)
                load_w(wkr[:, :, 0, 0:8], w_in_c[:, :, O_FL:O_FL + 8], "wkr", bwkr)
                wkv_c = w_kv_up.rearrange("(c p) (h d) -> p c h d", p=128, d=128)
                for c in range(2):
                    load_w(wkvk[:, c, :, :], wkv_c[:, c, :, 0:64], "wkvk", bwkvk)
                    load_w(wkvv[:, c, :, :], wkv_c[:, c, :, 64:128], "wkvv", bwkvv)

                ckvn = [sb(e1, "ckvn%d" % i, [128, 256], BF16) for i in range(2)]; bckvn = [Buf("ckvn%d" % i) for i in range(2)]
                ckvnT = [sb(e1, "ckvnT%d" % i, [128, 2, 512], BF16) for i in range(2)]
                bckvnT = [[Buf("ckvnT%d_%d" % (i, t)) for t in range(4)] for i in range(2)]
                vf = [sb(e1, "vf%d" % i, [128, 8, 4, 65], BF16) for i in range(2)]
                bvf = [Buf("vf%d" % i) for i in range(2)]
                vm = [sb(e1, "vm%d" % i, [128, 8, 4, 65], BF16) for i in range(2)]
                bvm = [Buf("vm%d" % i) for i in range(2)]
                for i in range(2):
                    P.op("pool", lambda e, i=i: e.memset(vf[i][:], 1.0), writes=[bvf[i]])
                    P.op("pool", lambda e, i=i: e.memset(vm[i][:], 1.0), writes=[bvm[i]])
                kf = [sb(e1, "kf%d" % i, [128, 4, 512], BF16) for i in range(2)]
                bkf = [Buf("kf%d" % i) for i in range(2)]
                km = [sb(e1, "km%d" % i, [128, 4, 512], BF16) for i in range(2)]
                bkm = [Buf("km%d" % i) for i in range(2)]
                krT = [sb(e1, "krT%d" % i, [96, 512], BF16) for i in range(2)]
                bkrT = [Buf("krT%d" % i) for i in range(2)]
                ktab = [sb(e1, "ktab%d" % i, [96, 2, 512], F32) for i in range(2)]
                bktab = [Buf("ktab%d" % i) for i in range(2)]
                rt1 = sb(e1, "rt1", [96, 512], F32); brt1 = Buf("rt1")
                rt2 = sb(e1, "rt2", [96, 512], F32); brt2 = Buf("rt2")
                lf = sb(e1, "lf", [8, 512], F32); blf = Buf("lf")
                negc = [sb(e1, "negc%d" % i, [8, 512], F32) for i in range(2)]
                bnegc = [Buf("negc%d" % i) for i in range(2)]
                pcs = [sb(e1, "pcs%d" % i, [8, 3, 512], BF16) for i in range(2)]
                bpcs = [Buf("pcs%d" % i) for i in range(2)]
                r1 = sb(e1, "r1", [8, 512], F32); br1 = Buf("r1")
                r2 = sb(e1, "r2", [8, 512], F32); br2 = Buf("r2")
                carry = {"ap": None, "buf": None}

                def kside(loaded, col0, ncol, vt0):
                    g = st["grp"]
                    st["grp"] += 1
                    s = g % 2
                    nt = len(loaded)
                    ntok = nt * 128
                    tr_stage(nt, s)
                    rd_xT = [bxT[s][t] for t in range(nt)]
                    P.dma("sp", lambda e: e.dma_start(out=ktab[s][64:96, 0, 0:ncol], in_=cosk[:, col0:col0 + ncol]), "ktab%d" % s, writes=[bktab[s]])
                    P.dma("sp", lambda e: e.dma_start(out=ktab[s][64:96, 1, 0:ncol], in_=sink[:, col0:col0 + ncol]), "ktab%d" % s, writes=[bktab[s]])
                    def tm_front(t):
                        tc_ = slice(t * 128, (t + 1) * 128)
                        pa, bpa = next_f()
                        for c in range(8):
                            P.op("pe", lambda e: e.matmul(pa[:, 0:256], lhsT=xT[s][:, c, tc_], rhs=wckv[:, c, :], start=(c == 0), stop=(c == 7)), reads=[bxT[s][t], bwckv], writes=[bpa], inc=(c == 7))
                        i2 = 4 + t % 2
                        P.op("act", lambda e: e.activation(out=junk[:, 0:256], in_=pa[:, 0:256], func=AF.Square, accum_out=ssq[i2][:]), reads=[bpa], writes=[bjunk, bssq[i2]])
                        P.load["act"] += 0.5
                        rstd_from_ssq(ssq[i2][:], bssq[i2], rstd[i2][:], brstd[i2], 256)
                        P.op("dve", lambda e: e.scalar_tensor_tensor(out=ckvn[t % 2][:], in0=pa[:, 0:256], scalar=rstd[i2][:, 0:1], in1=gkv[:], op0=ALU.mult, op1=ALU.mult), reads=[bpa, brstd[i2], bgkv], writes=[bckvn[t % 2]])
                        P.load["dve"] += 0.5
                        pb, bpb = next_f()
                        for c in range(8):
                            P.op("pe", lambda e: e.matmul(pb[:, :], lhsT=xT[s][:, c, tc_], rhs=wfv[:, c, :], start=(c == 0), stop=(c == 7)), reads=[bxT[s][t], bwfv], writes=[bpb], inc=(c == 7))
                        copy(vf[s][:, :, t, 0:64], pb[:, :].rearrange("p (h d) -> p h d", h=8), [bpb], [bvf[s]], 512)

                    def tm_back(t):
                        tc_ = slice(t * 128, (t + 1) * 128)
                        pt_, bpt_ = next_bf()
                        for c in range(2):
                            P.op("pe", lambda e: e.transpose(out=pt_[:, c * 128:(c + 1) * 128], in_=ckvn[t % 2][:, c * 128:(c + 1) * 128], identity=identb[:]), reads=[bckvn[t % 2], bidb], writes=[bpt_], inc=(c == 1))
                        copy(ckvnT[s][:, :, tc_], pt_[:, 0:256].rearrange("p (c k) -> p c k", c=2), [bpt_], [bckvnT[s][t]], 256)
                        pc, bpc = next_f()
                        for c in range(2):
                            P.op("pe", lambda e: e.matmul(pc[:, :], lhsT=ckvnT[s][:, c, tc_], rhs=wkvv[:, c, :, :], start=(c == 0), stop=(c == 1)), reads=[bckvnT[s][t], bwkvv], writes=[bpc], inc=(c == 1))
                        copy(vm[s][:, :, t, 0:64], pc[:, :].rearrange("p (h d) -> p h d", h=8), [bpc], [bvm[s]], 512)

                    tm_front(0)
                    for t in range(nt):
                        if t + 1 < nt:
                            tm_front(t + 1)
                        tm_back(t)
                    for h in range(8):
                        P.dma("sp", lambda e: e.dma_start(out=VF[h, :, vt0:vt0 + nt, :], in_=vf[s][:, h, 0:nt, :]), "vf%d" % s, reads=[bvf[s]], writes=[bVF])
                        P.dma("sp", lambda e: e.dma_start(out=VM[h, :, vt0:vt0 + nt, :], in_=vm[s][:, h, 0:nt, :]), "vm%d" % s, reads=[bvm[s]], writes=[bVM])
                    rd_ck = [bckvnT[s][t] for t in range(nt)]
                    yield
                    for pr in range(4):
                        pa, bpa = next_f()
                        for c in range(8):
                            P.op("pe", lambda e, c=c, pa=pa, pr=pr: e.matmul(pa[:, 0:ntok], lhsT=wfk[:, c, pr * 128:(pr + 1) * 128], rhs=xT[s][:, c, 0:ntok], start=(c == 0), stop=(c == 7)), reads=rd_xT + [bwfk], writes=[bpa], inc=(c == 7))
                        copy(kf[s][:, pr, 0:ntok], pa[:, 0:ntok], [bpa], [bkf[s]], ntok)
                    for two in range(2):
                        P.dma("sp", lambda e, two=two: e.dma_start(out=KF[:, 0:64, col0:col0 + ncol].rearrange("(p two) r c -> two r p c", two=2)[two], in_=kf[s][two * 64:(two + 1) * 64, :, 0:ncol]), "kf%d" % s, reads=[bkf[s]], writes=[bKF])
                    for pr in range(4):
                        pa, bpa = next_f()
                        for c in range(2):
                            P.op("pe", lambda e, c=c, pa=pa, pr=pr: e.matmul(pa[:, 0:ntok], lhsT=wkvk[:, c, 2 * pr:2 * pr + 2, :], rhs=ckvnT[s][:, c, 0:ntok], start=(c == 0), stop=(c == 1)), reads=rd_ck + [bwkvk], writes=[bpa], inc=(c == 1))
                        copy(km[s][:, pr, 0:ntok], pa[:, 0:ntok], [bpa], [bkm[s]], ntok)
                    for two in range(2):
                        P.dma("sp", lambda e, two=two: e.dma_start(out=KM[:, 0:64, col0:col0 + ncol].rearrange("(p two) r c -> two r p c", two=2)[two], in_=km[s][two * 64:(two + 1) * 64, :, 0:ncol]), "km%d" % s, reads=[bkm[s]], writes=[bKM])
                    pa, bpa = next_f()
                    pb, bpb = next_f()
                    pkr, bpkr = pa, bpa
                    for v, (pp, bpp) in enumerate(((pa, bpa), (pb, bpb))):
                        for c in range(8):
                            P.op("pe", lambda e, c=c, pp=pp, v=v: e.matmul(pp[0:96, 0:ntok], lhsT=wkr[:, c, v, :], rhs=xT[s][:, c, 0:ntok], start=(c == 0), stop=(c == 7)), reads=rd_xT + [bwkr], writes=[bpp], inc=(c == 7))
                    P.op("dve", lambda e, pa=pa: e.tensor_tensor(out=rt1[64:96, 0:ncol], in0=pa[64:96, 0:ncol], in1=ktab[s][64:96, 0, 0:ncol], op=ALU.mult), reads=[bpa, bktab[s]], writes=[brt1])
                    P.op("dve", lambda e, pb=pb: e.tensor_tensor(out=rt2[64:96, 0:ncol], in0=pb[64:96, 0:ncol], in1=ktab[s][64:96, 1, 0:ncol], op=ALU.mult), reads=[bpb, bktab[s]], writes=[brt2])
                    P.op("pool", lambda e: e.tensor_tensor(out=krT[s][64:96, 0:ncol], in0=rt1[64:96, 0:ncol], in1=rt2[64:96, 0:ncol], op=ALU.add), reads=[brt1, brt2], writes=[bkrT[s]])
                    P.load["dve"] += 1.0
                    for h in range(8):
                        P.dma("sp", lambda e, h=h: e.dma_start(out=KM[h, 64:96, col0:col0 + ncol], in_=krT[s][64:96, 0:ncol]), "krT%d" % s, reads=[bkrT[s]], writes=[bKM])
                    pa, bpa = pkr, bpkr
                    P.op("act", lambda e, pa=pa: e.activation(out=lf[:, 0:ncol], in_=pa[0:8, 0:ncol], func=AF.Exp, bias=nbf[:, 0:1], scale=-1.0), reads=[bpa, bnbf], writes=[blf])
                    P.op("act", lambda e: e.activation(out=lf[:, 0:ncol], in_=lf[:, 0:ncol], func=AF.Ln, bias=1.0, scale=1.0), reads=[blf], writes=[blf])
                    init = carry["ap"] if carry["ap"] is not None else 0.0
                    rds = [blf, bz8] + ([carry["buf"]] if carry["buf"] is not None else [])
                    P.op("dve", lambda e, init=init: e.tensor_tensor_scan(out=negc[s][:, 0:ncol], data0=lf[:, 0:ncol], data1=zeros8[:, 0:ncol], initial=init, op0=ALU.add, op1=ALU.add), reads=rds, writes=[bnegc[s]])
                    carry["ap"] = negc[s][:, ncol - 1:ncol]
                    carry["buf"] = bnegc[s]
                    P.dma("sp", lambda e: e.dma_start(out=CT[:, col0:col0 + ncol], in_=negc[s][:, 0:ncol]), "negc%d" % s, reads=[bnegc[s]], writes=[bCT])
                    P.op("dve", lambda e: e.tensor_copy(out=pcs[s][:, 0, 0:ncol], in_=negc[s][:, 0:ncol]), reads=[bnegc[s]], writes=[bpcs[s]])
                    P.op("dve", lambda e: e.tensor_tensor(out=r1[:, 0:ncol], in0=negc[s][:, 0:ncol], in1=pcs[s][:, 0, 0:ncol], op=ALU.subtract), reads=[bnegc[s], bpcs[s]], writes=[br1])
                    P.op("dve", lambda e: e.tensor_copy(out=pcs[s][:, 1, 0:ncol], in_=r1[:, 0:ncol]), reads=[br1, bpcs[s]], writes=[bpcs[s]])
                    P.op("dve", lambda e: e.tensor_tensor(out=r2[:, 0:ncol], in0=r1[:, 0:ncol], in1=pcs[s][:, 1, 0:ncol], op=ALU.subtract), reads=[br1, bpcs[s]], writes=[br2])
                    P.op("dve", lambda e: e.tensor_copy(out=pcs[s][:, 2, 0:ncol], in_=r2[:, 0:ncol]), reads=[br2, bpcs[s]], writes=[bpcs[s]])
                    P.load["dve"] += 1.5
                    P.dma("sp", lambda e: e.dma_start(out=KF[:, 65:68, col0:col0 + ncol], in_=pcs[s][:, :, 0:ncol]), "pcs%d" % s, reads=[bpcs[s]], writes=[bKF])

                groups = [([(meta, NMETA)], 0, NMETA, 0)]
                for g in range(16):
                    groups.append(([(x_all[g * 512 + t * 128:g * 512 + (t + 1) * 128, :], 128) for t in range(4)], NMETA + g * 512, 512, 1 + 4 * g))
                pending = issue_loads(groups[0][0])
                norm_stage(pending)
                for gi, (srcs_, col0_, ncol_, vt0_) in enumerate(groups):
                    cur = pending
                    pending = issue_loads(groups[gi + 1][0]) if gi + 1 < len(groups) else None
                    gen = kside(cur, col0_, ncol_, vt0_)
                    next(gen)
                    if pending is not None:
                        norm_stage(pending)
                    for _ in gen:
                        pass
                for h in range(8):
                    for c0 in range(0, NK, 4104):
                        P.dma("sp", lambda e: e.dma_start(out=KF[h, 64:65, c0:c0 + 4104].rearrange("a (k c) -> (a k) c", k=9), in_=ones_b[0:9, 0:456]), "ones_b", reads=[bones], writes=[bKF])
                    P.dma("sp", lambda e: e.dma_start(out=QF[h, 65:68, :].rearrange("r (k c) -> (r k) c", k=4), in_=ones_b[0:12, 0:512]), "ones_b", reads=[bones], writes=[bQF])

            P.barrier()

            with contextlib.ExitStack() as e2:
                wcq = sb(e2, "wcq", [128, 8, 512], BF16); bwcq = Buf("wcq")
                wfq = sb(e2, "wfq", [128, 8, 512], BF16); bwfq = Buf("wfq")
                wg = sb(e2, "wg", [128, 8, 2048], BF16); bwg = Buf("wg")
                wq = sb(e2, "wq", [128, 4, 768], BF16); bwq = Buf("wq")
                wqs = sb(e2, "wqs", [128, 4, 768], BF16); bwqs = Buf("wqs")
                w_in_c = w_in.rearrange("(c p) n -> p c n", p=128)
                load_w(wcq[:], w_in_c[:, :, O_CQ:O_CQ + 512], "wcq", bwcq)
                load_w(wfq[:], w_in_c[:, :, O_FQ:O_FQ + 512], "wfq", bwfq)
                for c in range(8):
                    load_w(wg[:, c, :], w_in_c[:, c, O_GA:O_GA + 2048], "wg", bwg)
                wq_c = w_q_up.rearrange("(c p) n -> p c n", p=128)
                load_w(wq[:], wq_c, "wq", bwq)
                load_w(wqs[:], wq_c, "wqs", bwqs)
                wq_h = w_q_up.rearrange("(c p) (h d) -> p c h d", p=128, d=96)
                wqs_h = wqs[:, :, :].rearrange("p c (h d) -> p c h d", d=96)
                for c in range(4):
                    load_w(wqs_h[:, c, :, 64:80], wq_h[:, c, :, 80:96], "wqs", bwqs)
                    load_w(wqs_h[:, c, :, 80:96], wq_h[:, c, :, 64:80], "wqs", bwqs)
                qtab = sb(e2, "qtab", [96, 2, NQ], F32); bqtab = Buf("qtab")
                P.dma("sp", lambda e: e.dma_start(out=qtab[64:96, 0, :], in_=cosq), "qtab", writes=[bqtab])
                P.dma("sp", lambda e: e.dma_start(out=qtab[64:96, 1, :], in_=sinq), "qtab", writes=[bqtab])
                cqn = [sb(e2, "cqn%d" % i, [128, 512], BF16) for i in range(2)]; bcqn = [Buf("cqn%d" % i) for i in range(2)]
                cqnT = sb(e2, "cqnT", [128, 4, 512], BF16)
                bcqnT = [Buf("cqnT%d" % t) for t in range(4)]
                qm = [sb(e2, "qm%d" % i, [96, 8, 512], BF16) for i in range(2)]
                bqm = [Buf("qm%d" % i) for i in range(2)]
                qf = [sb(e2, "qf%d" % i, [128, 4, 512], BF16) for i in range(2)]
                bqf = [Buf("qf%d" % i) for i in range(2)]
                qt1 = sb(e2, "qt1", [96, 512], F32); bqt1 = Buf("qt1")
                qt2 = sb(e2, "qt2", [96, 512], F32); bqt2 = Buf("qt2")
                cq32 = sb(e2, "cq32", [8, 512], F32); bcq32 = Buf("cq32")
                cqb = sb(e2, "cqb", [8, 512], BF16); bcqb = Buf("cqb")
                sg = [sb(e2, "sg%d" % i, [128, 512], BF16) for i in range(3)]
                bsg = [Buf("sg%d" % i) for i in range(3)]
                st["grp"] = 0
                qsrc = [[(xq[m * 512 + t * 128:m * 512 + (t + 1) * 128, :], 128) for t in range(4)] for m in range(4)]
                pending = issue_loads(qsrc[0])
                norm_stage(pending)
                for m in range(4):
                    s = m % 2
                    mc = slice(m * 512, (m + 1) * 512)
                    cur = pending
                    pending = issue_loads(qsrc[m + 1]) if m + 1 < 4 else None
                    tr_stage(4, s)
                    rd_xT = [bxT[s][t] for t in range(4)]
                    def cq_front(t):
                        tc_ = slice(t * 128, (t + 1) * 128)
                        pa, bpa = next_f()
                        for c in range(8):
                            P.op("pe", lambda e: e.matmul(pa[:, :], lhsT=xT[s][:, c, tc_], rhs=wcq[:, c, :], start=(c == 0), stop=(c == 7)), reads=[bxT[s][t], bwcq], writes=[bpa], inc=(c == 7))
                        i2 = 4 + t % 2
                        P.op("act", lambda e: e.activation(out=junk[:, 0:512], in_=pa[:, :], func=AF.Square, accum_out=ssq[i2][:]), reads=[bpa], writes=[bjunk, bssq[i2]])
                        rstd_from_ssq(ssq[i2][:], bssq[i2], rstd[i2][:], brstd[i2], 512)
                        P.op("dve", lambda e: e.scalar_tensor_tensor(out=cqn[t % 2][:], in0=pa[:, :], scalar=rstd[i2][:, 0:1], in1=gq[:], op0=ALU.mult, op1=ALU.mult), reads=[bpa, brstd[i2], bgq], writes=[bcqn[t % 2]])

                    def cq_back(t):
                        tc_ = slice(t * 128, (t + 1) * 128)
                        pt_, bpt_ = next_bf()
                        for c in range(4):
                            P.op("pe", lambda e: e.transpose(out=pt_[:, c * 128:(c + 1) * 128], in_=cqn[t % 2][:, c * 128:(c + 1) * 128], identity=identb[:]), reads=[bcqn[t % 2], bidb], writes=[bpt_], inc=(c == 3))
                        copy(cqnT[:, :, tc_], pt_[:, 0:512].rearrange("p (c k) -> p c k", c=4), [bpt_], [bcqnT[t]], 512)

                    def gate(jg):
                        pa, bpa = next_f()
                        for c in range(8):
                            P.op("pe", lambda e: e.matmul(pa[:, :], lhsT=wg[:, c, jg * 128:(jg + 1) * 128], rhs=xT[s][:, c, :], start=(c == 0), stop=(c == 7)), reads=rd_xT + [bwg], writes=[bpa], inc=(c == 7))
                        k3 = jg % 3
                        P.op("act", lambda e: e.activation(out=sg[k3][:], in_=pa[:, :], func=AF.Sigmoid), reads=[bpa], writes=[bsg[k3]])
                        P.load["act"] += 0.7
                        P.dma("sp", lambda e: e.dma_start(out=GS[jg, :, mc], in_=sg[k3][:]), "sg%d" % k3, reads=[bsg[k3]], writes=[bGS])

                    cq_front(0)
                    for t in range(4):
                        if t + 1 < 4:
                            cq_front(t + 1)
                        for jg in range(4 * t, 4 * t + 4):
                            gate(jg)
                        cq_back(t)
                    for h in range(8):
                        pa, bpa = next_f()
                        pb, bpb = next_f()
                        for (pp, bpp, ww, bww) in ((pa, bpa, wq, bwq), (pb, bpb, wqs, bwqs)):
                            for c in range(4):
                                P.op("pe", lambda e, c=c, pp=pp, ww=ww, h=h: e.matmul(pp[0:96, :], lhsT=ww[:, c, h * 96:(h + 1) * 96], rhs=cqnT[:, c, :], start=(c == 0), stop=(c == 3)), reads=bcqnT + [bww], writes=[bpp], inc=(c == 3))
                        copy(qm[s][0:64, h, :], pa[0:64, :], [bpa], [bqm[s]], 512, scale=SC_MLA)
                        P.op("dve", lambda e, pa=pa: e.tensor_tensor(out=qt1[64:96, :], in0=pa[64:96, :], in1=qtab[64:96, 0, mc], op=ALU.mult), reads=[bpa, bqtab], writes=[bqt1])
                        P.op("dve", lambda e, pb=pb: e.tensor_tensor(out=qt2[64:96, :], in0=pb[64:96, :], in1=qtab[64:96, 1, mc], op=ALU.mult), reads=[bpb, bqtab], writes=[bqt2])
                        P.op("pool", lambda e, h=h: e.tensor_tensor(out=qm[s][64:96, h, :], in0=qt1[64:96, :], in1=qt2[64:96, :], op=ALU.add), reads=[bqt1, bqt2], writes=[bqm[s]])
                        P.load["dve"] += 0.6
                    P.dma("sp", lambda e: e.dma_start(out=QM[:, :, mc].rearrange("h r c -> r h c"), in_=qm[s][:, :, :]), "qm%d" % s, reads=[bqm[s]], writes=[bQM])
                    if pending is not None:
                        norm_stage(pending)
                    for pr in range(4):
                        pa, bpa = next_f()
                        for c in range(8):
                            P.op("pe", lambda e, c=c, pa=pa, pr=pr: e.matmul(pa[:, :], lhsT=wfq[:, c, pr * 128:(pr + 1) * 128], rhs=xT[s][:, c, :], start=(c == 0), stop=(c == 7)), reads=rd_xT + [bwfq], writes=[bpa], inc=(c == 7))
                        copy(qf[s][:, pr, :], pa[:, :], [bpa], [bqf[s]], 512, scale=SC_FOX)
                    for two in range(2):
                        P.dma("sp", lambda e, two=two: e.dma_start(out=QF[:, 0:64, mc].rearrange("(p two) r c -> two r p c", two=2)[two], in_=qf[s][two * 64:(two + 1) * 64, :, :]), "qf%d" % s, reads=[bqf[s]], writes=[bQF])
                    for t in range(4):
                        def gat(e, t=t, m=m):
                            j = nc.partition_id() % 4
                            return e.dma_start(out=cq32[:, t * 128:(t + 1) * 128], in_=CT[:, bass.ds(j * 128 + NMETA + 512 * (4 * m + t), 128)])
                        P.dma("sp", gat, "cq32", reads=[bCT], writes=[bcq32], deferred=True)
                    P.op("dve", lambda e: e.tensor_scalar(out=cqb[:], in0=cq32[:], scalar1=-1.0, scalar2=None, op0=ALU.mult), reads=[bcq32], writes=[bcqb])
                    P.dma("sp", lambda e: e.dma_start(out=QF[:, 64, mc], in_=cqb[:]), "cqb", reads=[bcqb], writes=[bQF])
            P.barrier()

        acc = sb(es, "acc", [128, NT, D], F32)
        bacc = [[Buf("acc%d_%d" % (t, hf)) for hf in range(2)] for t in range(NT)]
        ey = contextlib.ExitStack()
        ybuf = sb(ey, "ybuf", [128, NT, D], BF16)
        bybuf = [Buf("ybuf%d" % t) for t in range(NT)]
        with contextlib.ExitStack() as eb:
            ksb = [sb(eb, "ksb%d" % i, [96, NK], BF16) for i in range(2)]
            bksb = [Buf("ksb%d" % i) for i in range(2)]
            vsb = [sb(eb, "vsb%d" % i, [128, NKT, 65], BF16) for i in range(2)]
            bvsb = [Buf("vsb%d" % i) for i in range(2)]
            qsb = [sb(eb, "qsb%d" % i, [96, NQ], BF16) for i in range(2)]
            bqsb = [Buf("qsb%d" % i) for i in range(2)]
            mk = sb(eb, "mk", [128, 4, 128], BF16); bmk = Buf("mk")
            P.dma("pool", lambda e: e.dma_start(out=mk[:], in_=masks.rearrange("d p c -> p d c")), "mk", writes=[bmk])
            ptb = [sb(eb, "ptb%d" % i, [128, 2, 512], BF16) for i in range(3)]
            bptb = [Buf("ptb%d" % i) for i in range(3)]
            sbank = [pS[i // 2][:, (i % 2) * 512:(i % 2 + 1) * 512] for i in range(6)]
            bsbank = [bpf[0], bpf[1], bpf[2], bpf[3], bpbf[0], bpbf[1]]
            fillv = pbf[0][:, :].bitcast(F32)
            rden = sb(eb, "rden", [128, 4], F32); brden = Buf("rden")
            rounds = []
            for hh in range(16):
                for m in range(4):
                    k_ = len(rounds) % 2
                    rounds.append(dict(hh=hh, m=m, mla=hh < 8, h=hh % 8, dk=96 if hh < 8 else 68, slot=hh % 2, po=pf[4 + k_], bpo=bpf[4 + k_]))
            flat = []
            for r in rounds:
                nkt = 16 * r["m"] + 17
                us = [(0,)] + [(k, k + 1) for k in range(1, nkt, 2)]
                for ui, u in enumerate(us):
                    flat.append((r, u, ui == 0, ui == len(us) - 1))

            def loads(hh):
                mla = hh < 8
                h = hh % 8
                dk = 96 if mla else 68
                slot = hh % 2
                Ks, Vs, Qs = (KM, VM, QM) if mla else (KF, VF, QF)
                bKs, bVs, bQs = (bKM, bVM, bQM) if mla else (bKF, bVF, bQF)
                for c0 in range(0, NK, 2052):
                    P.dma("sp", lambda e: e.dma_start(out=ksb[slot][0:dk, c0:c0 + 2052], in_=Ks[h, :, c0:c0 + 2052]), "ksb%d" % slot, reads=[bKs], writes=[bksb[slot]])
                P.dma("sp", lambda e: e.dma_start(out=vsb[slot][:], in_=Vs[h]), "vsb%d" % slot, reads=[bVs], writes=[bvsb[slot]])
                P.dma("sp", lambda e: e.dma_start(out=qsb[slot][0:dk, :], in_=Qs[h]), "qsb%d" % slot, reads=[bQs], writes=[bqsb[slot]])

            def geom(m, kt):
                i0 = max(0, -(-(kt - 16 * m - 4) // 4))
                kp = NMETA if kt == 0 else 128
                kc0 = 0 if kt == 0 else NMETA + 128 * (kt - 1)
                return i0, kp, kc0

            def emit_S(r, kt, ps_, bps_):
                m, slot, dk = r["m"], r["slot"], r["dk"]
                i0, kp, kc0 = geom(m, kt)
                msk = [(i, kt - (16 * m + 4 * i + 1)) for i in range(i0, 4) if 0 <= kt - (16 * m + 4 * i + 1) <= 3]
                P.op("pe", lambda e: e.matmul(ps_[0:kp, i0 * 128:512], lhsT=ksb[slot][0:dk, kc0:kc0 + kp], rhs=qsb[slot][0:dk, m * 512 + i0 * 128:(m + 1) * 512], start=True, stop=(len(msk) == 0), skip_group_check=True), reads=[bksb[slot], bqsb[slot]], writes=[bps_], inc=(len(msk) == 0))
                for n_, (i, d) in enumerate(msk):
                    P.op("pe", lambda e: e.matmul(ps_[0:128, i * 128:(i + 1) * 128], lhsT=identb[:, :], rhs=mk[:, d, :], start=False, stop=(n_ == len(msk) - 1), skip_group_check=True), reads=[bidb, bmk], writes=[bps_], inc=(n_ == len(msk) - 1))

            def emit_unit_S(fi):
                r, u, _, _ = flat[fi]
                sl3 = fi % 3
                for jj, kt in enumerate(u):
                    emit_S(r, kt, sbank[2 * sl3 + jj], bsbank[2 * sl3 + jj])

            loads(0)
            emit_unit_S(0)
            emit_unit_S(1)
            for fi, (r, unit, first, last) in enumerate(flat):
                m, slot, po, bpo = r["m"], r["slot"], r["po"], r["bpo"]
                if first and m == 0 and r["hh"] + 1 < 16:
                    loads(r["hh"] + 1)
                if fi + 2 < len(flat):
                    emit_unit_S(fi + 2)
                sl3 = fi % 3
                i0, kp, _ = geom(m, unit[0])
                assert all(geom(m, kt)[0] == i0 for kt in unit)
                pt_, bpt_ = ptb[sl3], bptb[sl3]
                rdS = [bsbank[2 * sl3 + jj] for jj in range(len(unit))]
                if len(unit) == 1:
                    P.op("act", lambda e: e.activation(out=pt_[0:kp, 0, i0 * 128:512], in_=sbank[2 * sl3][0:kp, i0 * 128:512], func=AF.Exp), reads=rdS, writes=[bpt_])
                else:
                    P.op("act", lambda e: e.activation(out=pt_[:, :, i0 * 128:512], in_=pS[sl3][:, :].rearrange("p (b c) -> p b c", b=2)[:, :, i0 * 128:512], func=AF.Exp), reads=rdS, writes=[bpt_])
                for jj, kt in enumerate(unit):
                    if FILLER and kt % FILLER == 0 and kt > 0:
                        P.op("pe", lambda e: e.matmul(po[:, 260:512], lhsT=identb[:, :], rhs=ones_b[:, 0:252], start=False, stop=False, skip_group_check=True), inc=False)
                    for i in range(i0, 4):
                        P.op("pe", lambda e: e.matmul(po[:, i * 65:(i + 1) * 65], lhsT=pt_[0:kp, jj, i * 128:(i + 1) * 128], rhs=vsb[slot][0:kp, kt, :], start=(kt == 0 and i == 0), stop=(kt == 16 * m + 4 * i + 4), skip_group_check=True), reads=[bpt_, bvsb[slot]], writes=[bpo], inc=(i == 3))
                if last:
                    pov = po[:, 0:260].rearrange("p (i d) -> p i d", i=4)
                    P.op("dve", lambda e: e.reciprocal(out=rden[:], in_=pov[:, :, 64]), reads=[bpo], writes=[brden])
                    col = (0 if r["mla"] else 512) + r["h"] * 64
                    for i in range(4):
                        P.op("dve", lambda e: e.tensor_scalar(out=ybuf[:, 4 * m + i, col:col + 64], in0=pov[:, i, 0:64], scalar1=rden[:, i:i + 1], scalar2=None, op0=ALU.mult), reads=[bpo, brden], writes=[bybuf[4 * m + i]])
            P.barrier()
        if debug:
            bYD = Buf("YD", acc=True)
            for t in range(NT):
                P.dma("sp", lambda e, t=t: e.dma_start(out=YD[t * 128:(t + 1) * 128, :], in_=ybuf[:, t, :]), "ybuf", reads=[bybuf[t]], writes=[bYD])

        with contextlib.ExitStack() as ec:
            wmo = sb(ec, "wmo", [128, 4, D], BF16); bwmo = Buf("wmo")
            wfo = sb(ec, "wfo", [128, 4, D], BF16); bwfo = Buf("wfo")
            wo = sb(ec, "wo", [128, 8, D], BF16); bwo = Buf("wo")
            load_w(wmo[:], w_mla_out.rearrange("(c p) n -> p c n", p=128), "wmo", bwmo)
            load_w(wfo[:], w_fox_out.rearrange("(c p) n -> p c n", p=128), "wfo", bwfo)
            for c in range(8):
                load_w(wo[:, c, :], w_out[c * 128:(c + 1) * 128, :], "wo", bwo)
            yT = sb(ec, "yT", [128, 8, 512], BF16)
            byT = [Buf("yT%d" % c) for c in range(8)]
            ga = [sb(ec, "ga%d" % i, [128, 16, 512], BF16) for i in range(1)]
            bga = [Buf("ga%d" % i) for i in range(1)]
            mT = sb(ec, "mT", [128, 8, 512], BF16)
            bmT = [Buf("mT%d" % c) for c in range(8)]
            mt1 = sb(ec, "mt1", [128, 512], F32); bmt1 = Buf("mt1")
            mt2 = sb(ec, "mt2", [128, 512], F32); bmt2 = Buf("mt2")
            xr = [sb(ec, "xr%d" % i, [128, D], F32) for i in range(2)]
            bxr = [Buf("xr%d" % i) for i in range(2)]
            for m in range(4):
                mc = slice(m * 512, (m + 1) * 512)
                P.dma("sp", lambda e, mc=mc: e.dma_start(out=ga[0][:], in_=GS[:, :, mc].rearrange("j p c -> p j c")), "ga0", reads=[bGS], writes=[bga[0]])
                for c in range(8):
                    pt_, bpt_ = next_bf()
                    for t in range(4):
                        P.op("pe", lambda e, c=c, t=t, pt_=pt_: e.transpose(out=pt_[:, t * 128:(t + 1) * 128], in_=ybuf[:, 4 * m + t, c * 128:(c + 1) * 128], identity=identb[:]), reads=[bybuf[4 * m + t], bidb], writes=[bpt_], inc=(t == 3))
                    copy(yT[:, c, :], pt_[:, 0:512], [bpt_], [byT[c]], 512)
                for jc in range(8):
                    pa, bpa = next_f()
                    pb, bpb = next_f()
                    for c in range(4):
                        P.op("pe", lambda e, c=c, pa=pa, jc=jc: e.matmul(pa[:, :], lhsT=wmo[:, c, jc * 128:(jc + 1) * 128], rhs=yT[:, c, :], start=(c == 0), stop=(c == 3)), reads=byT[0:4] + [bwmo], writes=[bpa], inc=(c == 3))
                    for c in range(4):
                        P.op("pe", lambda e, c=c, pb=pb, jc=jc: e.matmul(pb[:, :], lhsT=wfo[:, c, jc * 128:(jc + 1) * 128], rhs=yT[:, 4 + c, :], start=(c == 0), stop=(c == 3)), reads=byT[4:8] + [bwfo], writes=[bpb], inc=(c == 3))
                    P.op("dve", lambda e, pa=pa, jc=jc: e.tensor_tensor(out=mt1[:], in0=pa[:, :], in1=ga[0][:, jc, :], op=ALU.mult), reads=[bpa, bga[0]], writes=[bmt1])
                    P.op("dve", lambda e, pb=pb, jc=jc: e.tensor_tensor(out=mt2[:], in0=pb[:, :], in1=ga[0][:, 8 + jc, :], op=ALU.mult), reads=[bpb, bga[0]], writes=[bmt2])
                    P.op("pool", lambda e, jc=jc: e.tensor_tensor(out=mT[:, jc, :], in0=mt1[:], in1=mt2[:], op=ALU.add), reads=[bmt1, bmt2], writes=[bmT[jc]])
                for t in range(4):
                    tt = 4 * m + t
                    k2 = tt % 2
                    P.dma("sp", lambda e, tt=tt, k2=k2: e.dma_start(out=xr[k2][:], in_=xq[tt * 128:(tt + 1) * 128, :]), "xr%d" % k2, writes=[bxr[k2]])
                    for hf in range(2):
                        pa, bpa = next_f()
                        for c in range(8):
                            P.op("pe", lambda e, c=c, pa=pa, t=t, hf=hf: e.matmul(pa[:, :], lhsT=mT[:, c, t * 128:(t + 1) * 128], rhs=wo[:, c, hf * 512:(hf + 1) * 512], start=(c == 0), stop=(c == 7)), reads=bmT + [bwo], writes=[bpa], inc=(c == 7))
                        P.op("dve", lambda e, pa=pa, tt=tt, hf=hf, k2=k2: e.tensor_tensor(out=acc[:, tt, hf * 512:(hf + 1) * 512], in0=pa[:, :], in1=xr[k2][:, hf * 512:(hf + 1) * 512], op=ALU.add), reads=[bpa, bxr[k2]], writes=[bacc[tt][hf]])
            P.barrier()

        ey.close()
        with contextlib.ExitStack() as em:
            er = contextlib.ExitStack()
            gffn = sb(em, "gffn", [128, D], F32); bgffn = Buf("gffn")
            gfin = sb(em, "gfin", [128, D], F32); bgfin = Buf("gfin")
            bcast_load(gffn, bgffn, ffn_norm, "gffn")
            bcast_load(gfin, bgfin, final_norm, "gfin")
            sel = sb(em, "sel", [32, 32, 128], BF16); bsel = Buf("sel")
            for e_ in range(32):
                P.op("pool", lambda e, e_=e_: e.tensor_copy(out=sel[:, e_, :], in_=ident32[0:32, e_:e_ + 1].to_broadcast([32, 128])), reads=[bid32], writes=[bsel])
            hT = sb(em, "hT", [128, 8, NQ], BF16)
            bhT = [Buf("hT%d" % t) for t in range(NT)]
            combT = sb(em, "combT", [32, NQ], BF16)
            bcombT = [Buf("combT%d" % t) for t in range(NT)]
            ER = E_PER_ROUND
            ew0 = contextlib.ExitStack()
            wgt = [sb(ew0, "wgt%d" % i, [128, ER, 8, 256], BF16) for i in range(2)]
            bwgt = [Buf("wgt%d" % i) for i in range(2)]
            wup = [sb(ew0, "wup%d" % i, [128, ER, 8, 256], BF16) for i in range(2)]
            bwup = [Buf("wup%d" % i) for i in range(2)]
            wdn = [sb(ew0, "wdn%d" % i, [128, ER, 2, D], BF16) for i in range(2)]
            bwdn = [Buf("wdn%d" % i) for i in range(2)]
            nround = 32 // ER

            def load_round(r):
                sl = r % 2
                for x_ in range(ER):
                    e_ = r * ER + x_
                    P.dma("pool", lambda e, e_=e_, x_=x_: e.dma_start(out=wgt[sl][:, x_, :, :], in_=w_gate[e_].rearrange("(c p) f -> p c f", p=128)), "wgt%d" % sl, writes=[bwgt[sl]])
                    P.dma("pool", lambda e, e_=e_, x_=x_: e.dma_start(out=wup[sl][:, x_, :, :], in_=w_up[e_].rearrange("(c p) f -> p c f", p=128)), "wup%d" % sl, writes=[bwup[sl]])
                    P.dma("pool", lambda e, e_=e_, x_=x_: e.dma_start(out=wdn[sl][:, x_, :, :], in_=w_down[e_].rearrange("(c p) d -> p c d", p=128)), "wdn%d" % sl, writes=[bwdn[sl]])

            load_round(0)
            wr = sb(er, "wr", [128, 8, 36], BF16); bwr = Buf("wr")
            load_w(wr[:, :, 0:4], w_grt.rearrange("(c p) n -> p c n", p=128), "wr", bwr)
            load_w(wr[:, :, 4:36], w_ert.rearrange("(c p) n -> p c n", p=128), "wr", bwr)
            rb = sb(er, "rb", [128, 36], F32); brb = Buf("rb")
            P.dma("sp", lambda e: e.dma_start(out=rb[:, 0:4], in_=b_grt.partition_broadcast(128)), "rb", writes=[brb])
            P.dma("sp", lambda e: e.dma_start(out=rb[:, 4:36], in_=b_ert.partition_broadcast(128)), "rb", writes=[brb])
            junk2 = sb(er, "junk2", [128, D], BF16); bjunk2 = Buf("junk2")
            hs = [sb(er, "hs%d" % i, [128, D], BF16) for i in range(4)]
            bhs = [Buf("hs%d" % i) for i in range(4)]
            ssq2 = [sb(er, "ssq2_%d" % i, [128, 1], F32) for i in range(4)]
            bssq2 = [Buf("ssq2_%d" % i) for i in range(4)]
            rstd2 = [sb(er, "rstd2_%d" % i, [128, 1], F32) for i in range(4)]
            brstd2 = [Buf("rstd2_%d" % i) for i in range(4)]
            lgA = sb(er, "lgA", [128, NT, 36], F32); blgA = [Buf("lgA%d" % t) for t in range(NT)]
            smA = sb(er, "smA", [128, NT, 8], F32); bsmA = [Buf("smA%d" % t) for t in range(NT)]
            ohA = sb(er, "ohA", [128, NT, 4], F32); bohA = [Buf("ohA%d" % t) for t in range(NT)]
            mlA = sb(er, "mlA", [128, NT, 32], F32); bmlA = [Buf("mlA%d" % t) for t in range(NT)]
            t8A = sb(er, "t8A", [128, NT, 8], F32); bt8A = [Buf("t8A%d" % t) for t in range(NT)]
            c1A = sb(er, "c1A", [128, NT, 32], F32); bc1A = [Buf("c1A%d" % t) for t in range(NT)]
            c2A = sb(er, "c2A", [128, NT, 32], F32); bc2A = [Buf("c2A%d" % t) for t in range(NT)]
            for t0_ in range(0, NT, 4):
                tl = list(range(t0_, t0_ + 4))
                for t in tl:
                    k4 = t % 4
                    P.op("act", lambda e: e.activation(out=junk2[:], in_=acc[:, t, :], func=AF.Square, accum_out=ssq2[k4][:]), reads=bacc[t], writes=[bjunk2, bssq2[k4]])
                    rstd_from_ssq(ssq2[k4][:], bssq2[k4], rstd2[k4][:], brstd2[k4], D)
                for t in tl:
                    k4 = t % 4
                    P.op("dve", lambda e: e.scalar_tensor_tensor(out=hs[k4][:], in0=acc[:, t, :], scalar=rstd2[k4][:, 0:1], in1=gffn[:], op0=ALU.mult, op1=ALU.mult), reads=bacc[t] + [brstd2[k4], bgffn], writes=[bhs[k4]])
                for t in tl:
                    k4 = t % 4
                    tc_ = slice(t * 128, (t + 1) * 128)
                    pt_, bpt_ = next_bf()
                    for c in range(8):
                        P.op("pe", lambda e: e.transpose(out=pt_[:, c * 128:(c + 1) * 128], in_=hs[k4][:, c * 128:(c + 1) * 128], identity=identb[:]), reads=[bhs[k4], bidb], writes=[bpt_], inc=(c == 7))
                    copy(hT[:, :, tc_], pt_[:, :].rearrange("p (c k) -> p c k", c=8), [bpt_], [bhT[t]], 1024)
                for t in tl:
                    tc_ = slice(t * 128, (t + 1) * 128)
                    pa, bpa = next_f()
                    for c in range(8):
                        P.op("pe", lambda e: e.matmul(pa[:, 0:36], lhsT=hT[:, c, tc_], rhs=wr[:, c, :], start=(c == 0), stop=(c == 7)), reads=[bhT[t], bwr], writes=[bpa], inc=(c == 7))
                    P.op("dve", lambda e: e.tensor_tensor(out=lgA[:, t, :], in0=pa[:, 0:36], in1=rb[:], op=ALU.add), reads=[bpa, brb], writes=[blgA[t]])

            def stage(fn):
                for t in range(NT):
                    fn(t)
            stage(lambda t: P.op("dve", lambda e: e.reduce_max(out=smA[:, t, 0:1], in_=lgA[:, t, 0:4], axis=AX.X), reads=[blgA[t]], writes=[bsmA[t]]))
            stage(lambda t: P.op("dve", lambda e: e.tensor_scalar(out=smA[:, t, 1:2], in0=smA[:, t, 0:1], scalar1=-1.0, scalar2=None, op0=ALU.mult), reads=[bsmA[t]], writes=[bsmA[t]]))
            stage(lambda t: P.op("act", lambda e: e.activation(out=ohA[:, t, :], in_=lgA[:, t, 0:4], func=AF.Exp, bias=smA[:, t, 1:2], scale=1.0, accum_out=smA[:, t, 2:3]), reads=[blgA[t], bsmA[t]], writes=[bohA[t], bsmA[t]]))
            stage(lambda t: P.op("dve", lambda e: e.reciprocal(out=smA[:, t, 3:4], in_=smA[:, t, 2:3]), reads=[bsmA[t]], writes=[bsmA[t]]))
            stage(lambda t: P.op("dve", lambda e: e.tensor_scalar(out=ohA[:, t, :], in0=lgA[:, t, 0:4], scalar1=smA[:, t, 0:1], scalar2=1e30, op0=ALU.is_lt, op1=ALU.mult), reads=[blgA[t], bsmA[t], bohA[t]], writes=[bohA[t]]))
            for g_ in range(4):
                stage(lambda t: P.op("dve", lambda e: e.tensor_scalar(out=mlA[:, t, g_ * 8:(g_ + 1) * 8], in0=lgA[:, t, 4 + g_ * 8:4 + (g_ + 1) * 8], scalar1=ohA[:, t, g_:g_ + 1], scalar2=None, op0=ALU.subtract), reads=[blgA[t], bohA[t], bmlA[t]], writes=[bmlA[t]]))
            stage(lambda t: P.op("dve", lambda e: e.max(out=t8A[:, t, :], in_=mlA[:, t, :]), reads=[bmlA[t]], writes=[bt8A[t]]))
            stage(lambda t: P.op("dve", lambda e: e.tensor_tensor(out=smA[:, t, 4:5], in0=t8A[:, t, 0:1], in1=t8A[:, t, 1:2], op=ALU.subtract), reads=[bt8A[t], bsmA[t]], writes=[bsmA[t]]))
            stage(lambda t: P.op("act", lambda e: e.activation(out=smA[:, t, 5:6], in_=smA[:, t, 4:5], func=AF.Sigmoid), reads=[bsmA[t]], writes=[bsmA[t]]))
            stage(lambda t: P.op("dve", lambda e: e.tensor_tensor(out=smA[:, t, 6:7], in0=smA[:, t, 5:6], in1=smA[:, t, 3:4], op=ALU.mult), reads=[bsmA[t]], writes=[bsmA[t]]))
            stage(lambda t: P.op("dve", lambda e: e.tensor_tensor(out=smA[:, t, 7:8], in0=smA[:, t, 3:4], in1=smA[:, t, 6:7], op=ALU.subtract), reads=[bsmA[t]], writes=[bsmA[t]]))
            stage(lambda t: P.op("dve", lambda e: e.tensor_scalar(out=c1A[:, t, :], in0=mlA[:, t, :], scalar1=t8A[:, t, 0:1], scalar2=smA[:, t, 6:7], op0=ALU.is_equal, op1=ALU.mult), reads=[bmlA[t], bt8A[t], bsmA[t]], writes=[bc1A[t]]))
            stage(lambda t: P.op("dve", lambda e: e.tensor_scalar(out=c2A[:, t, :], in0=mlA[:, t, :], scalar1=t8A[:, t, 1:2], scalar2=smA[:, t, 7:8], op0=ALU.is_equal, op1=ALU.mult), reads=[bmlA[t], bt8A[t], bsmA[t]], writes=[bc2A[t]]))
            stage(lambda t: P.op("dve", lambda e: e.tensor_tensor(out=c1A[:, t, :], in0=c1A[:, t, :], in1=c2A[:, t, :], op=ALU.add), reads=[bc1A[t], bc2A[t]], writes=[bc1A[t]]))

            def to_combT(t):
                pb, bpb = next_f()
                P.op("pe", lambda e: e.matmul(pb[0:32, 0:128], lhsT=c1A[:, t, :], rhs=ident32[:], start=True, stop=True), reads=[bc1A[t], bid32], writes=[bpb])
                copy(combT[:, t * 128:(t + 1) * 128], pb[0:32, 0:128], [bpb], [bcombT[t]], 128)
            stage(to_combT)

            P.barrier()
            er.close()
            ew = contextlib.ExitStack()
            ER = E_PER_ROUND
            cbs = [sb(ew, "cbs%d" % i, [128, 512], BF16) for i in range(2)]
            bcbs = [Buf("cbs%d" % i) for i in range(2)]
            sa = [sb(ew, "sa%d" % i, [128, 512], F32) for i in range(2)]
            bsa = [Buf("sa%d" % i) for i in range(2)]
            tm = [sb(ew, "tm%d" % i, [128, 512], F32) for i in range(2)]
            btm = [Buf("tm%d" % i) for i in range(2)]
            hm = [sb(ew, "hm%d" % i, [128, ER, 2, 512], BF16) for i in range(2)]
            bhm = [Buf("hm%d" % i) for i in range(2)]
            pdv = [pbf[0][:, :].bitcast(F32), pbf[1][:, :].bitcast(F32)]
            itc = {"it": 0, "pd": 0}

            def front(r, m):
                sl = r % 2
                mc = slice(m * 512, (m + 1) * 512)
                hsl = (r * 4 + m) % 2
                for x_ in range(ER):
                    e_ = r * ER + x_
                    pc, bpc = pf[4], bpf[4]
                    P.op("pe", lambda e: e.matmul(pc[:, :], lhsT=sel[:, e_, :], rhs=combT[:, mc], start=True, stop=True), reads=bcombT[4 * m:4 * m + 4] + [bsel], writes=[bpc])
                    k2 = (itc["it"] // 2) % 2
                    P.op("act", lambda e: e.copy(out=cbs[k2][:], in_=pc[:, :]), reads=[bpc], writes=[bcbs[k2]])
                    for fc in range(2):
                        kk = itc["it"] % 2
                        itc["it"] += 1
                        pa, bpa = pf[0 + kk], bpf[0 + kk]
                        pb, bpb = pf[2 + kk], bpf[2 + kk]
                        for c in range(8):
                            P.op("pe", lambda e: e.matmul(pa[:, :], lhsT=wgt[sl][:, x_, c, fc * 128:(fc + 1) * 128], rhs=hT[:, c, mc], start=(c == 0), stop=(c == 7)), reads=bhT[4 * m:4 * m + 4] + [bwgt[sl]], writes=[bpa], inc=(c == 7))
                        for c in range(8):
                            P.op("pe", lambda e: e.matmul(pb[:, :], lhsT=wup[sl][:, x_, c, fc * 128:(fc + 1) * 128], rhs=hT[:, c, mc], start=(c == 0), stop=(c == 7)), reads=bhT[4 * m:4 * m + 4] + [bwup[sl]], writes=[bpb], inc=(c == 7))
                        P.op("act", lambda e: e.activation(out=sa[kk][:], in_=pa[:, :], func=AF.Silu), reads=[bpa], writes=[bsa[kk]])
                        P.op("dve", lambda e: e.tensor_tensor(out=tm[kk][:], in0=pb[:, :], in1=sa[kk][:], op=ALU.mult), reads=[bpb, bsa[kk]], writes=[btm[kk]])
                        P.op("pool", lambda e: e.tensor_tensor(out=hm[hsl][:, x_, fc, :], in0=tm[kk][:], in1=cbs[k2][:], op=ALU.mult), reads=[btm[kk], bcbs[k2]], writes=[bhm[hsl]])

            def back(r, m):
                sl = r % 2
                hsl = (r * 4 + m) % 2
                for t in range(4):
                    tt = 4 * m + t
                    for hf in range(2):
                        kd = itc["pd"] % 2
                        itc["pd"] += 1
                        pd, bpd = pdv[kd], bpbf[kd]
                        n_ = 0
                        for x_ in range(ER):
                            for fc in range(2):
                                P.op("pe", lambda e: e.matmul(pd, lhsT=hm[hsl][:, x_, fc, t * 128:(t + 1) * 128], rhs=wdn[sl][:, x_, fc, hf * 512:(hf + 1) * 512], start=(n_ == 0), stop=(n_ == 2 * ER - 1)), reads=[bhm[hsl], bwdn[sl]], writes=[bpd], inc=(n_ == 2 * ER - 1))
                                n_ += 1
                        P.op("dve", lambda e: e.tensor_tensor(out=acc[:, tt, hf * 512:(hf + 1) * 512], in0=pd, in1=acc[:, tt, hf * 512:(hf + 1) * 512], op=ALU.add), reads=[bpd, bacc[tt][hf]], writes=[bacc[tt][hf]])

            units = [(r, m) for r in range(nround) for m in range(4)]
            for ui, (r, m) in enumerate(units):
                front(r, m)
                if ui > 0:
                    back(*units[ui - 1])
                if m == 0 and r + 1 < nround:
                    load_round(r + 1)
            back(*units[-1])

            P.barrier()
            ew.close()
            ew0.close()
            junk2 = sb(em, "junk2b", [128, D], BF16); bjunk2 = Buf("junk2b")
            ssq2 = [sb(em, "ssq3_%d" % i, [128, 1], F32) for i in range(2)]
            bssq2 = [Buf("ssq3_%d" % i) for i in range(2)]
            rstd2 = [sb(em, "rstd3_%d" % i, [128, 1], F32) for i in range(2)]
            brstd2 = [Buf("rstd3_%d" % i) for i in range(2)]
            ot = [sb(em, "ot%d" % i, [128, D], F32) for i in range(2)]
            bot = [Buf("ot%d" % i) for i in range(2)]
            for t in range(NT):
                k2 = t % 2
                P.op("act", lambda e, t=t, k2=k2: e.activation(out=junk2[:], in_=acc[:, t, :], func=AF.Square, accum_out=ssq2[k2][:]), reads=bacc[t], writes=[bjunk2, bssq2[k2]])
                rstd_from_ssq(ssq2[k2][:], bssq2[k2], rstd2[k2][:], brstd2[k2], D)
                P.op("dve", lambda e, t=t, k2=k2: e.scalar_tensor_tensor(out=ot[k2][:], in0=acc[:, t, :], scalar=rstd2[k2][:, 0:1], in1=gfin[:], op0=ALU.mult, op1=ALU.mult), reads=bacc[t] + [brstd2[k2], bgfin], writes=[bot[k2]])
                P.dma("sp", lambda e, t=t, k2=k2: e.dma_start(out=out[t * 128:(t + 1) * 128, :], in_=ot[k2][:]), "ot%d" % k2, reads=[bot[k2]], writes=[bOUT])
            P.barrier()

        P.emit(nc, es)
    return nc


def _host_consts(j):
    inv = (np.float32(10000.0) ** (-np.arange(0, 32, 2, dtype=np.float32) / np.float32(32))).astype(np.float32)
    pos = np.arange(NK, dtype=np.float32)
    ang = (pos[:, None] * inv[None, :]).astype(np.float32)
    cos = np.cos(ang).astype(np.float32).T
    sin = np.sin(ang).astype(np.float32).T
    cosk = np.concatenate([cos, cos], 0)
    sink = np.concatenate([-sin, sin], 0)
    qpos = np.concatenate([NMETA + 128 * (j + 4 * i) + np.arange(128) for i in range(NT)])
    cosq = (cosk[:, qpos] * np.float32(SC_MLA)).astype(np.float32)
    sinq = (sink[:, qpos] * np.float32(SC_MLA)).astype(np.float32)
    r = np.arange(128)
    tri = (r[None, :] >= r[:, None]).astype(np.float32)
    masks = np.full((4, 128, 128), -30000.0, np.float32)
    for d in range(4):
        if d < j:
            masks[d] = 0.0
        elif d == j:
            masks[d] = (tri - 1.0) * 30000.0
    return dict(cosk=np.ascontiguousarray(cosk), sink=np.ascontiguousarray(sink), cosq=np.ascontiguousarray(cosq),
                sinq=np.ascontiguousarray(sinq), masks=masks, ident=np.eye(128, dtype=np.float32))


def make_in_maps(inputs):
    f = lambda a: np.ascontiguousarray(np.asarray(a, dtype=np.float32))
    x = f(inputs["x"])
    shared = {
        "meta": f(inputs["meta"]), "attn_norm": f(inputs["attn_norm"]).reshape(D),
        "w_in": f(inputs["w_in"]).reshape(D, 4392), "b_forget": f(inputs["b_forget"]).reshape(8),
        "q_a_norm": f(inputs["q_a_norm"]).reshape(512), "w_q_up": f(inputs["w_q_up"]).reshape(512, 768),
        "kv_a_norm": f(inputs["kv_a_norm"]).reshape(256), "w_kv_up": f(inputs["w_kv_up"]).reshape(256, 1024),
        "w_mla_out": f(inputs["w_mla_out"]).reshape(512, D), "w_fox_out": f(inputs["w_fox_out"]).reshape(512, D),
        "w_out": f(inputs["w_out"]).reshape(D, D), "ffn_norm": f(inputs["ffn_norm"]).reshape(D),
        "w_group_router": f(inputs["w_group_router"]).reshape(D, 4), "b_group_router": f(inputs["b_group_router"]).reshape(4),
        "w_expert_router": f(inputs["w_expert_router"]).reshape(D, 32), "b_expert_router": f(inputs["b_expert_router"]).reshape(32),
        "w_gate": f(inputs["w_gate"]).reshape(32, D, 256), "w_up": f(inputs["w_up"]).reshape(32, D, 256),
        "w_down": f(inputs["w_down"]).reshape(32, 256, D), "final_norm": f(inputs["final_norm"]).reshape(D),
    }
    maps = []
    for c in range(8):
        b, j = c // 4, c % 4
        m = dict(shared)
        m["x_all"] = x[b]
        xb = x[b].reshape(64, 128, D)
        m["xq"] = np.ascontiguousarray(xb[j::4].reshape(NQ, D))
        m.update(_host_consts(j))
        maps.append(m)
    return maps


def kernel(**inputs):
    maps = make_in_maps(inputs)
    nc = build_program()
    res = run_bass_kernel_spmd(nc, maps, core_ids=list(range(8)))
    outp = np.zeros((2, 64, 128, D), np.float32)
    for c in range(8):
        b, j = c // 4, c % 4
        outp[b, j::4] = np.asarray(res.results[c]["out"], dtype=np.float32).reshape(NT, 128, D)
    return outp.reshape(2, SEQ, D)
```

```python
import contextlib
import numpy as np
import concourse.bass as bass
import concourse.mybir as mybir
from concourse.bass_utils import run_bass_kernel_spmd

F32 = mybir.dt.float32
BF16 = mybir.dt.bfloat16
AF = mybir.ActivationFunctionType
ALU = mybir.AluOpType
AX = mybir.AxisListType

D = 1024
SEQ = 8192
NMETA = 16
NK = NMETA + SEQ
NQ = 2048
NT = 16
NKT = 65
EPS = 1e-6
SC_MLA = 96 ** -0.5
SC_FOX = 0.125
O_CQ, O_CKV, O_KR, O_FQ, O_FK, O_FV, O_FL, O_GA, O_GB = 0, 512, 768, 800, 1312, 1824, 2336, 2344, 3368
E_PER_ROUND = 2
FILLER = 0


class Buf:
    __slots__ = ("name", "w", "r", "acc")

    def __init__(self, name, acc=False):
        self.name = name
        self.w = []
        self.r = []
        self.acc = acc


class _Rec:
    def __getattr__(self, name):
        def f(*a, **k):
            self.__dict__["call"] = (name, a, k)
            return self
        return f


class Prog:
    def __init__(self):
        self.streams = {k: [] for k in ("pe", "act", "dve", "pool", "sp")}
        self.cnt = {}
        self.seen = {k: {} for k in self.streams}
        self.load = {"act": 0.0, "dve": 0.0}

    def _deps(self, eng, reads, writes):
        need = {}
        for b in reads:
            for (s, v) in b.w:
                if need.get(s, 0) < v:
                    need[s] = v
        for b in writes:
            if not b.acc:
                for (s, v) in b.w:
                    if need.get(s, 0) < v:
                        need[s] = v
            for (s, v) in b.r:
                if need.get(s, 0) < v:
                    need[s] = v
        waits = []
        seen = self.seen[eng]
        for s, v in need.items():
            if eng == "pe" and s == "c_pe":
                continue
            if seen.get(s, 0) < v:
                waits.append((s, v))
                seen[s] = v
        return waits

    def _record(self, s, v, reads, writes):
        for b in reads:
            b.r.append((s, v))
            if len(b.r) > 64:
                m = {}
                for (ss, vv) in b.r:
                    if m.get(ss, 0) < vv:
                        m[ss] = vv
                b.r = list(m.items())
        for b in writes:
            if b.acc:
                b.w.append((s, v))
                if len(b.w) > 64:
                    m = {}
                    for (ss, vv) in b.w:
                        if m.get(ss, 0) < vv:
                            m[ss] = vv
                    b.w = list(m.items())
            else:
                b.w = [(s, v)]
                b.r = []

    @staticmethod
    def _bind(fn, deferred):
        if deferred:
            return fn
        rec = _Rec()
        fn(rec)
        name, a, k = rec.call
        return lambda e: getattr(e, name)(*a, **k)

    def op(self, eng, fn, reads=(), writes=(), inc=True):
        waits = self._deps(eng, reads, writes)
        s = "c_" + eng
        v = self.cnt.get(s, 0) + 1
        if inc:
            self.cnt[s] = v
        self.streams[eng].append((waits, self._bind(fn, False), s, 1 if inc else 0))
        self._record(s, v, reads, writes)

    def dma(self, eng, fn, key, reads=(), writes=(), deferred=False):
        waits = self._deps(eng, reads, writes)
        s = "d_" + key
        v = self.cnt.get(s, 0) + 16
        self.cnt[s] = v
        self.streams[eng].append((waits, self._bind(fn, deferred), s, 16))
        self._record(s, v, reads, writes)

    def pick(self, cost):
        e = "act" if self.load["act"] <= self.load["dve"] else "dve"
        self.load[e] += cost
        return e

    def barrier(self):
        for eng in self.streams:
            waits = []
            for s, v in self.cnt.items():
                if self.seen[eng].get(s, 0) < v:
                    waits.append((s, v))
                    self.seen[eng][s] = v
            if waits:
                self.streams[eng].append((waits, None, None, 0))

    def emit(self, nc, es):
        sems = {s: es.enter_context(nc.semaphore(s)) for s in self.cnt}
        engs = {"pe": "tensor", "act": "scalar", "dve": "vector", "pool": "gpsimd", "sp": "sync"}
        with nc.Block() as block:
            for k, attr in engs.items():
                stream = self.streams[k]

                def body(e, stream=stream):
                    for waits, fn, s, inc in stream:
                        for (ws, wv) in waits:
                            e.wait_ge(sems[ws], wv)
                        if fn is not None:
                            ins = fn(e)
                            if inc:
                                ins.then_inc(sems[s], inc)

                getattr(block, attr)(body)


def build_program(debug=False):
    nc = bass.Bass("TRN2", target_bir_lowering=False)
    nc.cache_partition_id()
    P = Prog()

    def dram_in(name, shape, dt=F32):
        return nc.dram_tensor(name, list(shape), dt, kind="ExternalInput").ap()

    x_all = dram_in("x_all", [SEQ, D])
    xq = dram_in("xq", [NQ, D])
    meta = dram_in("meta", [NMETA, D])
    attn_norm = dram_in("attn_norm", [D])
    w_in = dram_in("w_in", [D, 4392])
    b_forget = dram_in("b_forget", [8])
    q_a_norm = dram_in("q_a_norm", [512])
    w_q_up = dram_in("w_q_up", [512, 768])
    kv_a_norm = dram_in("kv_a_norm", [256])
    w_kv_up = dram_in("w_kv_up", [256, 1024])
    w_mla_out = dram_in("w_mla_out", [512, D])
    w_fox_out = dram_in("w_fox_out", [512, D])
    w_out = dram_in("w_out", [D, D])
    ffn_norm = dram_in("ffn_norm", [D])
    w_grt = dram_in("w_group_router", [D, 4])
    b_grt = dram_in("b_group_router", [4])
    w_ert = dram_in("w_expert_router", [D, 32])
    b_ert = dram_in("b_expert_router", [32])
    w_gate = dram_in("w_gate", [32, D, 256])
    w_up = dram_in("w_up", [32, D, 256])
    w_down = dram_in("w_down", [32, 256, D])
    final_norm = dram_in("final_norm", [D])
    cosk = dram_in("cosk", [32, NK])
    sink = dram_in("sink", [32, NK])
    cosq = dram_in("cosq", [32, NQ])
    sinq = dram_in("sinq", [32, NQ])
    masks = dram_in("masks", [4, 128, 128])
    ident_d = dram_in("ident", [128, 128])
    out = nc.dram_tensor("out", [NQ, D], F32, kind="ExternalOutput").ap()

    skind = "ExternalOutput" if debug else "Internal"

    def scratch(name, shape, dt=BF16):
        return nc.dram_tensor(name, list(shape), dt, kind=skind).ap()

    KM = scratch("KM", [8, 96, NK]); bKM = Buf("KM", acc=True)
    VM = scratch("VM", [8, 128, NKT, 65]); bVM = Buf("VM", acc=True)
    KF = scratch("KF", [8, 68, NK]); bKF = Buf("KF", acc=True)
    VF = scratch("VF", [8, 128, NKT, 65]); bVF = Buf("VF", acc=True)
    QM = scratch("QM", [8, 96, NQ]); bQM = Buf("QM", acc=True)
    QF = scratch("QF", [8, 68, NQ]); bQF = Buf("QF", acc=True)
    GS = scratch("GS", [16, 128, NQ]); bGS = Buf("GS", acc=True)
    CT = scratch("CT", [8, NK], F32); bCT = Buf("CT", acc=True)
    YD = scratch("YD", [NQ, D], BF16) if debug else None
    bOUT = Buf("out", acc=True)

    es = contextlib.ExitStack()
    with es:
        def sb(es_, name, shape, dt):
            return es_.enter_context(nc.sbuf_tensor(name, list(shape), dt))

        pS = [es.enter_context(nc.psum_tensor("pS%d" % i, [128, 1024], F32)) for i in range(3)]
        pf = [pS[0][:, 0:512], pS[0][:, 512:1024], pS[1][:, 0:512], pS[1][:, 512:1024]]
        pf += [es.enter_context(nc.psum_tensor("pf%d" % i, [128, 512], F32))[:, :] for i in (4, 5)]
        bpf = [Buf("pf%d" % i) for i in range(6)]
        pbf = [pS[2][:, 0:512].bitcast(BF16), pS[2][:, 512:1024].bitcast(BF16)]
        bpbf = [Buf("pbf%d" % i) for i in range(2)]
        rr = {"bf": 0, "f": 0}

        def next_bf():
            i = rr["bf"] % 2
            rr["bf"] += 1
            return pbf[i], bpbf[i]

        def next_f(lo=0, hi=6):
            i = lo + rr["f"] % (hi - lo)
            rr["f"] += 1
            return pf[i], bpf[i]

        ident32 = sb(es, "ident32", [128, 128], F32); bid32 = Buf("ident32")
        identb = sb(es, "identb", [128, 128], BF16); bidb = Buf("identb")
        epsD = sb(es, "epsD", [128, 1], F32); bepsD = Buf("epsD")
        ones_b = sb(es, "ones_b", [128, 512], BF16); bones = Buf("ones_b")
        zeros8 = sb(es, "zeros8", [8, 512], F32); bz8 = Buf("zeros8")
        P.dma("sp", lambda e: e.dma_start(out=ident32[:], in_=ident_d), "ident32", writes=[bid32])
        P.dma("pool", lambda e: e.dma_start(out=identb[:], in_=ident_d), "identb", writes=[bidb])
        P.op("pool", lambda e: e.memset(epsD[:], EPS), writes=[bepsD])
        P.op("pool", lambda e: e.memset(ones_b[:], 1.0), writes=[bones])
        P.op("pool", lambda e: e.memset(zeros8[:], 0.0), writes=[bz8])
        def rstd_from_ssq(ssq, bssq, rstd, brstd, n):
            P.op("act", lambda e: e.activation(out=rstd, in_=ssq, func=AF.Ln, bias=epsD[:, 0:1], scale=1.0 / n), reads=[bssq, bepsD], writes=[brstd])
            P.op("act", lambda e: e.activation(out=rstd, in_=rstd, func=AF.Exp, scale=-0.5), reads=[brstd], writes=[brstd])
            P.load["act"] += 0.6

        def copy(out_ap, in_ap, reads, writes, n, scale=None, eng=None):
            e_ = eng or P.pick(0.3 + n / 1000.0)
            if e_ == "act":
                if scale is None:
                    P.op("act", lambda e: e.copy(out=out_ap, in_=in_ap), reads=reads, writes=writes)
                else:
                    P.op("act", lambda e: e.mul(out=out_ap, in_=in_ap, mul=scale), reads=reads, writes=writes)
            else:
                if scale is None:
                    P.op("dve", lambda e: e.tensor_copy(out=out_ap, in_=in_ap), reads=reads, writes=writes)
                else:
                    P.op("dve", lambda e: e.tensor_scalar(out=out_ap, in0=in_ap, scalar1=scale, scalar2=None, op0=ALU.mult), reads=reads, writes=writes)

        def load_w(tile_ap, src_ap, key, btile):
            P.dma("pool", lambda e: e.dma_start(out=tile_ap, in_=src_ap), key, writes=[btile])

        def bcast_load(tile, btile, vec, key):
            P.dma("sp", lambda e: e.dma_start(out=tile[:], in_=vec.partition_broadcast(128)), key, writes=[btile])

        with contextlib.ExitStack() as ea:
            gattn = sb(ea, "gattn", [128, D], F32); bgattn = Buf("gattn")
            gkv = sb(ea, "gkv", [128, 256], F32); bgkv = Buf("gkv")
            gq = sb(ea, "gq", [128, 512], F32); bgq = Buf("gq")
            bcast_load(gattn, bgattn, attn_norm, "gattn")
            bcast_load(gkv, bgkv, kv_a_norm, "gkv")
            bcast_load(gq, bgq, q_a_norm, "gq")
            nbf = sb(ea, "nbf", [8, 1], F32); bnbf = Buf("nbf")
            P.dma("sp", lambda e: e.dma_start(out=nbf[:], in_=b_forget.rearrange("(h o) -> h o", o=1)), "nbf", writes=[bnbf])
            P.op("dve", lambda e: e.tensor_scalar(out=nbf[:], in0=nbf[:], scalar1=-1.0, scalar2=None, op0=ALU.mult), reads=[bnbf], writes=[bnbf])

            xin = [sb(ea, "xin%d" % i, [128, D], F32) for i in range(8)]
            bxin = [Buf("xin%d" % i) for i in range(8)]
            junk = sb(ea, "junk", [128, D], BF16); bjunk = Buf("junk")
            xs = [sb(ea, "xs%d" % i, [128, D], BF16) for i in range(4)]
            bxs = [Buf("xs%d" % i) for i in range(4)]
            ssq = [sb(ea, "ssq%d" % i, [128, 1], F32) for i in range(6)]
            bssq = [Buf("ssq%d" % i) for i in range(6)]
            rstd = [sb(ea, "rstd%d" % i, [128, 1], F32) for i in range(6)]
            brstd = [Buf("rstd%d" % i) for i in range(6)]
            xT = [sb(ea, "xT%d" % i, [128, 8, 512], BF16) for i in range(2)]
            bxT = [[Buf("xT%d_%d" % (i, t)) for t in range(4)] for i in range(2)]
            st = {"xin": 0, "xs": 0, "grp": 0}

            def issue_loads(srcs):
                res = []
                for (src, rows) in srcs:
                    i3 = st["xin"] % 8
                    st["xin"] += 1
                    if rows < 128:
                        P.op("pool", lambda e: e.memset(xin[i3][:], 0.0), writes=[bxin[i3]])
                    P.dma("sp", lambda e: e.dma_start(out=xin[i3][0:rows, :], in_=src), "xin%d" % i3, writes=[bxin[i3]])
                    res.append(i3)
                return res

            def norm_stage(loaded):
                for t, i3 in enumerate(loaded):
                    P.op("act", lambda e: e.activation(out=junk[:], in_=xin[i3][:], func=AF.Square, accum_out=ssq[t][:]), reads=[bxin[i3]], writes=[bjunk, bssq[t]])
                    P.load["act"] += 1.2
                    rstd_from_ssq(ssq[t][:], bssq[t], rstd[t][:], brstd[t], D)
                for t, i3 in enumerate(loaded):
                    P.op("dve", lambda e: e.scalar_tensor_tensor(out=xs[t][:], in0=xin[i3][:], scalar=rstd[t][:, 0:1], in1=gattn[:], op0=ALU.mult, op1=ALU.mult), reads=[bxin[i3], brstd[t], bgattn], writes=[bxs[t]])
                    P.load["dve"] += 1.2

            def tr_stage(nt_, slot):
                for t in range(nt_):
                    pt_, bpt_ = next_bf()
                    for c in range(8):
                        P.op("pe", lambda e: e.transpose(out=pt_[:, c * 128:(c + 1) * 128], in_=xs[t][:, c * 128:(c + 1) * 128], identity=identb[:]), reads=[bxs[t], bidb], writes=[bpt_], inc=(c == 7))
                    copy(xT[slot][:, :, t * 128:(t + 1) * 128], pt_[:, :].rearrange("p (c k) -> p c k", c=8), [bpt_], [bxT[slot][t]], 1024)

            with contextlib.ExitStack() as e1:
                wckv = sb(e1, "wckv", [128, 8, 256], BF16); bwckv = Buf("wckv")
                wkr = sb(e1, "wkr", [128, 8, 2, 96], BF16); bwkr = Buf("wkr")
                wfk = sb(e1, "wfk", [128, 8, 512], BF16); bwfk = Buf("wfk")
                wfv = sb(e1, "wfv", [128, 8, 512], BF16); bwfv = Buf("wfv")
                wkvk = sb(e1, "wkvk", [128, 2, 8, 64], BF16); bwkvk = Buf("wkvk")
                wkvv = sb(e1, "wkvv", [128, 2, 8, 64], BF16); bwkvv = Buf("wkvv")
                w_in_c = w_in.rearrange("(c p) n -> p c n", p=128)
                load_w(wckv[:], w_in_c[:, :, O_CKV:O_CKV + 256], "wckv", bwckv)
                P.op("pool", lambda e: e.memset(wkr[:], 0.0), writes=[bwkr])
                load_w(wkr[:, :, 0, 64:96], w_in_c[:, :, O_KR:O_KR + 32], "wkr", bwkr)
                load_w(wkr[:, :, 1, 64:80], w_in_c[:, :, O_KR + 16:O_KR + 32], "wkr", bwkr)
                load_w(wkr[:, :, 1, 80:96], w_in_c[:, :, O_KR:O_KR + 16], "wkr", bwkr)
                load_w(wfk[:], w_in_c[:, :, O_FK:O_FK + 512], "wfk", bwfk)
                load_w(wfv[:], w_in_c[:, :, O_FV:O_FV + 512], "wfv", bwfv)
                load_w(wkr[:, :, 0, 0:8], w_in_c[:, :, O_FL:O_FL + 8], "wkr", bwkr)
                wkv_c = w_kv_up.rearrange("(c p) (h d) -> p c h d", p=128, d=128)
                for c in range(2):
                    load_w(wkvk[:, c, :, :], wkv_c[:, c, :, 0:64], "wkvk", bwkvk)
                    load_w(wkvv[:, c, :, :], wkv_c[:, c, :, 64:128], "wkvv", bwkvv)

                ckvn = [sb(e1, "ckvn%d" % i, [128, 256], BF16) for i in range(2)]; bckvn = [Buf("ckvn%d" % i) for i in range(2)]
                ckvnT = [sb(e1, "ckvnT%d" % i, [128, 2, 512], BF16) for i in range(2)]
                bckvnT = [[Buf("ckvnT%d_%d" % (i, t)) for t in range(4)] for i in range(2)]
                vf = [sb(e1, "vf%d" % i, [128, 8, 4, 65], BF16) for i in range(2)]
                bvf = [Buf("vf%d" % i) for i in range(2)]
                vm = [sb(e1, "vm%d" % i, [128, 8, 4, 65], BF16) for i in range(2)]
                bvm = [Buf("vm%d" % i) for i in range(2)]
                for i in range(2):
                    P.op("pool", lambda e, i=i: e.memset(vf[i][:], 1.0), writes=[bvf[i]])
                    P.op("pool", lambda e, i=i: e.memset(vm[i][:], 1.0), writes=[bvm[i]])
                kf = [sb(e1, "kf%d" % i, [128, 4, 512], BF16) for i in range(2)]
                bkf = [Buf("kf%d" % i) for i in range(2)]
                km = [sb(e1, "km%d" % i, [128, 4, 512], BF16) for i in range(2)]
                bkm = [Buf("km%d" % i) for i in range(2)]
                krT = [sb(e1, "krT%d" % i, [96, 512], BF16) for i in range(2)]
                bkrT = [Buf("krT%d" % i) for i in range(2)]
                ktab = [sb(e1, "ktab%d" % i, [96, 2, 512], F32) for i in range(2)]
                bktab = [Buf("ktab%d" % i) for i in range(2)]
                rt1 = sb(e1, "rt1", [96, 512], F32); brt1 = Buf("rt1")
                rt2 = sb(e1, "rt2", [96, 512], F32); brt2 = Buf("rt2")
                lf = sb(e1, "lf", [8, 512], F32); blf = Buf("lf")
                negc = [sb(e1, "negc%d" % i, [8, 512], F32) for i in range(2)]
                bnegc = [Buf("negc%d" % i) for i in range(2)]
                pcs = [sb(e1, "pcs%d" % i, [8, 3, 512], BF16) for i in range(2)]
                bpcs = [Buf("pcs%d" % i) for i in range(2)]
                r1 = sb(e1, "r1", [8, 512], F32); br1 = Buf("r1")
                r2 = sb(e1, "r2", [8, 512], F32); br2 = Buf("r2")
                carry = {"ap": None, "buf": None}

                def kside(loaded, col0, ncol, vt0):
                    g = st["grp"]
                    st["grp"] += 1
                    s = g % 2
                    nt = len(loaded)
                    ntok = nt * 128
                    tr_stage(nt, s)
                    rd_xT = [bxT[s][t] for t in range(nt)]
                    P.dma("sp", lambda e: e.dma_start(out=ktab[s][64:96, 0, 0:ncol], in_=cosk[:, col0:col0 + ncol]), "ktab%d" % s, writes=[bktab[s]])
                    P.dma("sp", lambda e: e.dma_start(out=ktab[s][64:96, 1, 0:ncol], in_=sink[:, col0:col0 + ncol]), "ktab%d" % s, writes=[bktab[s]])
                    def tm_front(t):
                        tc_ = slice(t * 128, (t + 1) * 128)
                        pa, bpa = next_f()
                        for c in range(8):
                            P.op("pe", lambda e: e.matmul(pa[:, 0:256], lhsT=xT[s][:, c, tc_], rhs=wckv[:, c, :], start=(c == 0), stop=(c == 7)), reads=[bxT[s][t], bwckv], writes=[bpa], inc=(c == 7))
                        i2 = 4 + t % 2
                        P.op("act", lambda e: e.activation(out=junk[:, 0:256], in_=pa[:, 0:256], func=AF.Square, accum_out=ssq[i2][:]), reads=[bpa], writes=[bjunk, bssq[i2]])
                        P.load["act"] += 0.5
                        rstd_from_ssq(ssq[i2][:], bssq[i2], rstd[i2][:], brstd[i2], 256)
                        P.op("dve", lambda e: e.scalar_tensor_tensor(out=ckvn[t % 2][:], in0=pa[:, 0:256], scalar=rstd[i2][:, 0:1], in1=gkv[:], op0=ALU.mult, op1=ALU.mult), reads=[bpa, brstd[i2], bgkv], writes=[bckvn[t % 2]])
                        P.load["dve"] += 0.5
                        pb, bpb = next_f()
                        for c in range(8):
                            P.op("pe", lambda e: e.matmul(pb[:, :], lhsT=xT[s][:, c, tc_], rhs=wfv[:, c, :], start=(c == 0), stop=(c == 7)), reads=[bxT[s][t], bwfv], writes=[bpb], inc=(c == 7))
                        copy(vf[s][:, :, t, 0:64], pb[:, :].rearrange("p (h d) -> p h d", h=8), [bpb], [bvf[s]], 512)

                    def tm_back(t):
                        tc_ = slice(t * 128, (t + 1) * 128)
                        pt_, bpt_ = next_bf()
                        for c in range(2):
                            P.op("pe", lambda e: e.transpose(out=pt_[:, c * 128:(c + 1) * 128], in_=ckvn[t % 2][:, c * 128:(c + 1) * 128], identity=identb[:]), reads=[bckvn[t % 2], bidb], writes=[bpt_], inc=(c == 1))
                        copy(ckvnT[s][:, :, tc_], pt_[:, 0:256].rearrange("p (c k) -> p c k", c=2), [bpt_], [bckvnT[s][t]], 256)
                        pc, bpc = next_f()
                        for c in range(2):
                            P.op("pe", lambda e: e.matmul(pc[:, :], lhsT=ckvnT[s][:, c, tc_], rhs=wkvv[:, c, :, :], start=(c == 0), stop=(c == 1)), reads=[bckvnT[s][t], bwkvv], writes=[bpc], inc=(c == 1))
                        copy(vm[s][:, :, t, 0:64], pc[:, :].rearrange("p (h d) -> p h d", h=8), [bpc], [bvm[s]], 512)

                    tm_front(0)
                    for t in range(nt):
                        if t + 1 < nt:
                            tm_front(t + 1)
                        tm_back(t)
                    for h in range(8):
                        P.dma("sp", lambda e: e.dma_start(out=VF[h, :, vt0:vt0 + nt, :], in_=vf[s][:, h, 0:nt, :]), "vf%d" % s, reads=[bvf[s]], writes=[bVF])
                        P.dma("sp", lambda e: e.dma_start(out=VM[h, :, vt0:vt0 + nt, :], in_=vm[s][:, h, 0:nt, :]), "vm%d" % s, reads=[bvm[s]], writes=[bVM])
                    rd_ck = [bckvnT[s][t] for t in range(nt)]
                    yield
                    for pr in range(4):
                        pa, bpa = next_f()
                        for c in range(8):
                            P.op("pe", lambda e, c=c, pa=pa, pr=pr: e.matmul(pa[:, 0:ntok], lhsT=wfk[:, c, pr * 128:(pr + 1) * 128], rhs=xT[s][:, c, 0:ntok], start=(c == 0), stop=(c == 7)), reads=rd_xT + [bwfk], writes=[bpa], inc=(c == 7))
                        copy(kf[s][:, pr, 0:ntok], pa[:, 0:ntok], [bpa], [bkf[s]], ntok)
                    for two in range(2):
                        P.dma("sp", lambda e, two=two: e.dma_start(out=KF[:, 0:64, col0:col0 + ncol].rearrange("(p two) r c -> two r p c", two=2)[two], in_=kf[s][two * 64:(two + 1) * 64, :, 0:ncol]), "kf%d" % s, reads=[bkf[s]], writes=[bKF])
                    for pr in range(4):
                        pa, bpa = next_f()
                        for c in range(2):
                            P.op("pe", lambda e, c=c, pa=pa, pr=pr: e.matmul(pa[:, 0:ntok], lhsT=wkvk[:, c, 2 * pr:2 * pr + 2, :], rhs=ckvnT[s][:, c, 0:ntok], start=(c == 0), stop=(c == 1)), reads=rd_ck + [bwkvk], writes=[bpa], inc=(c == 1))
                        copy(km[s][:, pr, 0:ntok], pa[:, 0:ntok], [bpa], [bkm[s]], ntok)
                    for two in range(2):
                        P.dma("sp", lambda e, two=two: e.dma_start(out=KM[:, 0:64, col0:col0 + ncol].rearrange("(p two) r c -> two r p c", two=2)[two], in_=km[s][two * 64:(two + 1) * 64, :, 0:ncol]), "km%d" % s, reads=[bkm[s]], writes=[bKM])
                    pa, bpa = next_f()
                    pb, bpb = next_f()
                    pkr, bpkr = pa, bpa
                    for v, (pp, bpp) in enumerate(((pa, bpa), (pb, bpb))):
                        for c in range(8):
                            P.op("pe", lambda e, c=c, pp=pp, v=v: e.matmul(pp[0:96, 0:ntok], lhsT=wkr[:, c, v, :], rhs=xT[s][:, c, 0:ntok], start=(c == 0), stop=(c == 7)), reads=rd_xT + [bwkr], writes=[bpp], inc=(c == 7))
                    P.op("dve", lambda e, pa=pa: e.tensor_tensor(out=rt1[64:96, 0:ncol], in0=pa[64:96, 0:ncol], in1=ktab[s][64:96, 0, 0:ncol], op=ALU.mult), reads=[bpa, bktab[s]], writes=[brt1])
                    P.op("dve", lambda e, pb=pb: e.tensor_tensor(out=rt2[64:96, 0:ncol], in0=pb[64:96, 0:ncol], in1=ktab[s][64:96, 1, 0:ncol], op=ALU.mult), reads=[bpb, bktab[s]], writes=[brt2])
                    P.op("pool", lambda e: e.tensor_tensor(out=krT[s][64:96, 0:ncol], in0=rt1[64:96, 0:ncol], in1=rt2[64:96, 0:ncol], op=ALU.add), reads=[brt1, brt2], writes=[bkrT[s]])
                    P.load["dve"] += 1.0
                    for h in range(8):
                        P.dma("sp", lambda e, h=h: e.dma_start(out=KM[h, 64:96, col0:col0 + ncol], in_=krT[s][64:96, 0:ncol]), "krT%d" % s, reads=[bkrT[s]], writes=[bKM])
                    pa, bpa = pkr, bpkr
                    P.op("act", lambda e, pa=pa: e.activation(out=lf[:, 0:ncol], in_=pa[0:8, 0:ncol], func=AF.Exp, bias=nbf[:, 0:1], scale=-1.0), reads=[bpa, bnbf], writes=[blf])
                    P.op("act", lambda e: e.activation(out=lf[:, 0:ncol], in_=lf[:, 0:ncol], func=AF.Ln, bias=1.0, scale=1.0), reads=[blf], writes=[blf])
                    init = carry["ap"] if carry["ap"] is not None else 0.0
                    rds = [blf, bz8] + ([carry["buf"]] if carry["buf"] is not None else [])
                    P.op("dve", lambda e, init=init: e.tensor_tensor_scan(out=negc[s][:, 0:ncol], data0=lf[:, 0:ncol], data1=zeros8[:, 0:ncol], initial=init, op0=ALU.add, op1=ALU.add), reads=rds, writes=[bnegc[s]])
                    carry["ap"] = negc[s][:, ncol - 1:ncol]
                    carry["buf"] = bnegc[s]
                    P.dma("sp", lambda e: e.dma_start(out=CT[:, col0:col0 + ncol], in_=negc[s][:, 0:ncol]), "negc%d" % s, reads=[bnegc[s]], writes=[bCT])
                    P.op("dve", lambda e: e.tensor_copy(out=pcs[s][:, 0, 0:ncol], in_=negc[s][:, 0:ncol]), reads=[bnegc[s]], writes=[bpcs[s]])
                    P.op("dve", lambda e: e.tensor_tensor(out=r1[:, 0:ncol], in0=negc[s][:, 0:ncol], in1=pcs[s][:, 0, 0:ncol], op=ALU.subtract), reads=[bnegc[s], bpcs[s]], writes=[br1])
                    P.op("dve", lambda e: e.tensor_copy(out=pcs[s][:, 1, 0:ncol], in_=r1[:, 0:ncol]), reads=[br1, bpcs[s]], writes=[bpcs[s]])
                    P.op("dve", lambda e: e.tensor_tensor(out=r2[:, 0:ncol], in0=r1[:, 0:ncol], in1=pcs[s][:, 1, 0:ncol], op=ALU.subtract), reads=[br1, bpcs[s]], writes=[br2])
                    P.op("dve", lambda e: e.tensor_copy(out=pcs[s][:, 2, 0:ncol], in_=r2[:, 0:ncol]), reads=[br2, bpcs[s]], writes=[bpcs[s]])
                    P.load["dve"] += 1.5
                    P.dma("sp", lambda e: e.dma_start(out=KF[:, 65:68, col0:col0 + ncol], in_=pcs[s][:, :, 0:ncol]), "pcs%d" % s, reads=[bpcs[s]], writes=[bKF])

                groups = [([(meta, NMETA)], 0, NMETA, 0)]
                for g in range(16):
                    groups.append(([(x_all[g * 512 + t * 128:g * 512 + (t + 1) * 128, :], 128) for t in range(4)], NMETA + g * 512, 512, 1 + 4 * g))
                pending = issue_loads(groups[0][0])
                norm_stage(pending)
                for gi, (srcs_, col0_, ncol_, vt0_) in enumerate(groups):
                    cur = pending
                    pending = issue_loads(groups[gi + 1][0]) if gi + 1 < len(groups) else None
                    if gi == 6:
                        for h in range(8):
                            for c0 in range(0, NK, 4104):
                                P.dma("sp", lambda e: e.dma_start(out=KF[h, 64:65, c0:c0 + 4104].rearrange("a (k c) -> (a k) c", k=9), in_=ones_b[0:9, 0:456]), "ones_b", reads=[bones], writes=[bKF])
                            P.dma("sp", lambda e: e.dma_start(out=QF[h, 65:68, :].rearrange("r (k c) -> (r k) c", k=4), in_=ones_b[0:12, 0:512]), "ones_b", reads=[bones], writes=[bQF])
                    gen = kside(cur, col0_, ncol_, vt0_)
                    next(gen)
                    if pending is not None:
                        norm_stage(pending)
                    for _ in gen:
                        pass
            P.barrier()

            with contextlib.ExitStack() as e2:
                wcq = sb(e2, "wcq", [128, 8, 512], BF16); bwcq = Buf("wcq")
                wfq = sb(e2, "wfq", [128, 8, 512], BF16); bwfq = Buf("wfq")
                wg = sb(e2, "wg", [128, 8, 2048], BF16); bwg = Buf("wg")
                wq = sb(e2, "wq", [128, 4, 768], BF16); bwq = Buf("wq")
                wqs = sb(e2, "wqs", [128, 4, 768], BF16); bwqs = Buf("wqs")
                w_in_c = w_in.rearrange("(c p) n -> p c n", p=128)
                load_w(wcq[:], w_in_c[:, :, O_CQ:O_CQ + 512], "wcq", bwcq)
                load_w(wfq[:], w_in_c[:, :, O_FQ:O_FQ + 512], "wfq", bwfq)
                for c in range(8):
                    load_w(wg[:, c, :], w_in_c[:, c, O_GA:O_GA + 2048], "wg", bwg)
                wq_c = w_q_up.rearrange("(c p) n -> p c n", p=128)
                load_w(wq[:], wq_c, "wq", bwq)
                load_w(wqs[:], wq_c, "wqs", bwqs)
                wq_h = w_q_up.rearrange("(c p) (h d) -> p c h d", p=128, d=96)
                wqs_h = wqs[:, :, :].rearrange("p c (h d) -> p c h d", d=96)
                for c in range(4):
                    load_w(wqs_h[:, c, :, 64:80], wq_h[:, c, :, 80:96], "wqs", bwqs)
                    load_w(wqs_h[:, c, :, 80:96], wq_h[:, c, :, 64:80], "wqs", bwqs)
                qtab = sb(e2, "qtab", [96, 2, NQ], F32); bqtab = Buf("qtab")
                P.dma("sp", lambda e: e.dma_start(out=qtab[64:96, 0, :], in_=cosq), "qtab", writes=[bqtab])
                P.dma("sp", lambda e: e.dma_start(out=qtab[64:96, 1, :], in_=sinq), "qtab", writes=[bqtab])
                cqn = [sb(e2, "cqn%d" % i, [128, 512], BF16) for i in range(2)]; bcqn = [Buf("cqn%d" % i) for i in range(2)]
                cqnT = sb(e2, "cqnT", [128, 4, 512], BF16)
                bcqnT = [Buf("cqnT%d" % t) for t in range(4)]
                qm = [sb(e2, "qm%d" % i, [96, 8, 512], BF16) for i in range(2)]
                bqm = [Buf("qm%d" % i) for i in range(2)]
                qf = [sb(e2, "qf%d" % i, [128, 4, 512], BF16) for i in range(2)]
                bqf = [Buf("qf%d" % i) for i in range(2)]
                qt1 = sb(e2, "qt1", [96, 512], F32); bqt1 = Buf("qt1")
                qt2 = sb(e2, "qt2", [96, 512], F32); bqt2 = Buf("qt2")
                cq32 = sb(e2, "cq32", [8, 512], F32); bcq32 = Buf("cq32")
                cqb = sb(e2, "cqb", [8, 512], BF16); bcqb = Buf("cqb")
                sg = [sb(e2, "sg%d" % i, [128, 512], BF16) for i in range(3)]
                bsg = [Buf("sg%d" % i) for i in range(3)]
                st["grp"] = 0
                qsrc = [[(xq[m * 512 + t * 128:m * 512 + (t + 1) * 128, :], 128) for t in range(4)] for m in range(4)]
                pending = issue_loads(qsrc[0])
                norm_stage(pending)
                for m in range(4):
                    s = m % 2
                    mc = slice(m * 512, (m + 1) * 512)
                    cur = pending
                    pending = issue_loads(qsrc[m + 1]) if m + 1 < 4 else None
                    tr_stage(4, s)
                    rd_xT = [bxT[s][t] for t in range(4)]
                    def cq_front(t):
                        tc_ = slice(t * 128, (t + 1) * 128)
                        pa, bpa = next_f()
                        for c in range(8):
                            P.op("pe", lambda e: e.matmul(pa[:, :], lhsT=xT[s][:, c, tc_], rhs=wcq[:, c, :], start=(c == 0), stop=(c == 7)), reads=[bxT[s][t], bwcq], writes=[bpa], inc=(c == 7))
                        i2 = 4 + t % 2
                        P.op("act", lambda e: e.activation(out=junk[:, 0:512], in_=pa[:, :], func=AF.Square, accum_out=ssq[i2][:]), reads=[bpa], writes=[bjunk, bssq[i2]])
                        rstd_from_ssq(ssq[i2][:], bssq[i2], rstd[i2][:], brstd[i2], 512)
                        P.op("dve", lambda e: e.scalar_tensor_tensor(out=cqn[t % 2][:], in0=pa[:, :], scalar=rstd[i2][:, 0:1], in1=gq[:], op0=ALU.mult, op1=ALU.mult), reads=[bpa, brstd[i2], bgq], writes=[bcqn[t % 2]])

                    def cq_back(t):
                        tc_ = slice(t * 128, (t + 1) * 128)
                        pt_, bpt_ = next_bf()
                        for c in range(4):
                            P.op("pe", lambda e: e.transpose(out=pt_[:, c * 128:(c + 1) * 128], in_=cqn[t % 2][:, c * 128:(c + 1) * 128], identity=identb[:]), reads=[bcqn[t % 2], bidb], writes=[bpt_], inc=(c == 3))
                        copy(cqnT[:, :, tc_], pt_[:, 0:512].rearrange("p (c k) -> p c k", c=4), [bpt_], [bcqnT[t]], 512)

                    def gate(jg):
                        pa, bpa = next_f()
                        for c in range(8):
                            P.op("pe", lambda e: e.matmul(pa[:, :], lhsT=wg[:, c, jg * 128:(jg + 1) * 128], rhs=xT[s][:, c, :], start=(c == 0), stop=(c == 7)), reads=rd_xT + [bwg], writes=[bpa], inc=(c == 7))
                        k3 = jg % 3
                        P.op("act", lambda e: e.activation(out=sg[k3][:], in_=pa[:, :], func=AF.Sigmoid), reads=[bpa], writes=[bsg[k3]])
                        P.load["act"] += 0.7
                        P.dma("sp", lambda e: e.dma_start(out=GS[jg, :, mc], in_=sg[k3][:]), "sg%d" % k3, reads=[bsg[k3]], writes=[bGS])

                    cq_front(0)
                    for t in range(4):
                        if t + 1 < 4:
                            cq_front(t + 1)
                        for jg in range(4 * t, 4 * t + 4):
                            gate(jg)
                        cq_back(t)
                    for h in range(8):
                        pa, bpa = next_f()
                        pb, bpb = next_f()
                        for (pp, bpp, ww, bww) in ((pa, bpa, wq, bwq), (pb, bpb, wqs, bwqs)):
                            for c in range(4):
                                P.op("pe", lambda e, c=c, pp=pp, ww=ww, h=h: e.matmul(pp[0:96, :], lhsT=ww[:, c, h * 96:(h + 1) * 96], rhs=cqnT[:, c, :], start=(c == 0), stop=(c == 3)), reads=bcqnT + [bww], writes=[bpp], inc=(c == 3))
                        copy(qm[s][0:64, h, :], pa[0:64, :], [bpa], [bqm[s]], 512, scale=SC_MLA)
                        P.op("dve", lambda e, pa=pa: e.tensor_tensor(out=qt1[64:96, :], in0=pa[64:96, :], in1=qtab[64:96, 0, mc], op=ALU.mult), reads=[bpa, bqtab], writes=[bqt1])
                        P.op("dve", lambda e, pb=pb: e.tensor_tensor(out=qt2[64:96, :], in0=pb[64:96, :], in1=qtab[64:96, 1, mc], op=ALU.mult), reads=[bpb, bqtab], writes=[bqt2])
                        P.op("pool", lambda e, h=h: e.tensor_tensor(out=qm[s][64:96, h, :], in0=qt1[64:96, :], in1=qt2[64:96, :], op=ALU.add), reads=[bqt1, bqt2], writes=[bqm[s]])
                        P.load["dve"] += 0.6
                    P.dma("sp", lambda e: e.dma_start(out=QM[:, :, mc].rearrange("h r c -> r h c"), in_=qm[s][:, :, :]), "qm%d" % s, reads=[bqm[s]], writes=[bQM])
                    if pending is not None:
                        norm_stage(pending)
                    for pr in range(4):
                        pa, bpa = next_f()
                        for c in range(8):
                            P.op("pe", lambda e, c=c, pa=pa, pr=pr: e.matmul(pa[:, :], lhsT=wfq[:, c, pr * 128:(pr + 1) * 128], rhs=xT[s][:, c, :], start=(c == 0), stop=(c == 7)), reads=rd_xT + [bwfq], writes=[bpa], inc=(c == 7))
                        copy(qf[s][:, pr, :], pa[:, :], [bpa], [bqf[s]], 512, scale=SC_FOX)
                    for two in range(2):
                        P.dma("sp", lambda e, two=two: e.dma_start(out=QF[:, 0:64, mc].rearrange("(p two) r c -> two r p c", two=2)[two], in_=qf[s][two * 64:(two + 1) * 64, :, :]), "qf%d" % s, reads=[bqf[s]], writes=[bQF])
                    for t in range(4):
                        def gat(e, t=t, m=m):
                            j = nc.partition_id() % 4
                            return e.dma_start(out=cq32[:, t * 128:(t + 1) * 128], in_=CT[:, bass.ds(j * 128 + NMETA + 512 * (4 * m + t), 128)])
                        P.dma("sp", gat, "cq32", reads=[bCT], writes=[bcq32], deferred=True)
                    P.op("dve", lambda e: e.tensor_scalar(out=cqb[:], in0=cq32[:], scalar1=-1.0, scalar2=None, op0=ALU.mult), reads=[bcq32], writes=[bcqb])
                    P.dma("sp", lambda e: e.dma_start(out=QF[:, 64, mc], in_=cqb[:]), "cqb", reads=[bcqb], writes=[bQF])
            P.barrier()

        acc = sb(es, "acc", [128, NT, D], F32)
        bacc = [[Buf("acc%d_%d" % (t, hf)) for hf in range(2)] for t in range(NT)]
        ey = contextlib.ExitStack()
        ybuf = sb(ey, "ybuf", [128, NT, D], BF16)
        bybuf = [Buf("ybuf%d" % t) for t in range(NT)]
        with contextlib.ExitStack() as eb:
            ksb = [sb(eb, "ksb%d" % i, [96, NK], BF16) for i in range(2)]
            bksb = [Buf("ksb%d" % i) for i in range(2)]
            vsb = [sb(eb, "vsb%d" % i, [128, NKT, 65], BF16) for i in range(2)]
            bvsb = [Buf("vsb%d" % i) for i in range(2)]
            qsb = [sb(eb, "qsb%d" % i, [96, NQ], BF16) for i in range(2)]
            bqsb = [Buf("qsb%d" % i) for i in range(2)]
            mk = sb(eb, "mk", [128, 4, 128], BF16); bmk = Buf("mk")
            P.dma("pool", lambda e: e.dma_start(out=mk[:], in_=masks.rearrange("d p c -> p d c")), "mk", writes=[bmk])
            ptb = [sb(eb, "ptb%d" % i, [128, 2, 512], BF16) for i in range(3)]
            bptb = [Buf("ptb%d" % i) for i in range(3)]
            sbank = [pS[i // 2][:, (i % 2) * 512:(i % 2 + 1) * 512] for i in range(6)]
            bsbank = [bpf[0], bpf[1], bpf[2], bpf[3], bpbf[0], bpbf[1]]
            fillv = pbf[0][:, :].bitcast(F32)
            rden = sb(eb, "rden", [128, 4], F32); brden = Buf("rden")
            rounds = []
            for hh in range(16):
                for m in range(4):
                    k_ = len(rounds) % 2
                    rounds.append(dict(hh=hh, m=m, mla=hh < 8, h=hh % 8, dk=96 if hh < 8 else 68, slot=hh % 2, po=pf[4 + k_], bpo=bpf[4 + k_]))
            flat = []
            for r in rounds:
                nkt = 16 * r["m"] + 17
                us = [(0,)] + [(k, k + 1) for k in range(1, nkt, 2)]
                for ui, u in enumerate(us):
                    flat.append((r, u, ui == 0, ui == len(us) - 1))

            def loads(hh):
                mla = hh < 8
                h = hh % 8
                dk = 96 if mla else 68
                slot = hh % 2
                Ks, Vs, Qs = (KM, VM, QM) if mla else (KF, VF, QF)
                bKs, bVs, bQs = (bKM, bVM, bQM) if mla else (bKF, bVF, bQF)
                for c0 in range(0, NK, 2052):
                    P.dma("sp", lambda e: e.dma_start(out=ksb[slot][0:dk, c0:c0 + 2052], in_=Ks[h, :, c0:c0 + 2052]), "ksb%d" % slot, reads=[bKs], writes=[bksb[slot]])
                P.dma("sp", lambda e: e.dma_start(out=vsb[slot][:], in_=Vs[h]), "vsb%d" % slot, reads=[bVs], writes=[bvsb[slot]])
                P.dma("sp", lambda e: e.dma_start(out=qsb[slot][0:dk, :], in_=Qs[h]), "qsb%d" % slot, reads=[bQs], writes=[bqsb[slot]])

            def geom(m, kt):
                i0 = max(0, -(-(kt - 16 * m - 4) // 4))
                kp = NMETA if kt == 0 else 128
                kc0 = 0 if kt == 0 else NMETA + 128 * (kt - 1)
                return i0, kp, kc0

            def emit_S(r, kt, ps_, bps_):
                m, slot, dk = r["m"], r["slot"], r["dk"]
                i0, kp, kc0 = geom(m, kt)
                msk = [(i, kt - (16 * m + 4 * i + 1)) for i in range(i0, 4) if 0 <= kt - (16 * m + 4 * i + 1) <= 3]
                P.op("pe", lambda e: e.matmul(ps_[0:kp, i0 * 128:512], lhsT=ksb[slot][0:dk, kc0:kc0 + kp], rhs=qsb[slot][0:dk, m * 512 + i0 * 128:(m + 1) * 512], start=True, stop=(len(msk) == 0), skip_group_check=True), reads=[bksb[slot], bqsb[slot]], writes=[bps_], inc=(len(msk) == 0))
                for n_, (i, d) in enumerate(msk):
                    P.op("pe", lambda e: e.matmul(ps_[0:128, i * 128:(i + 1) * 128], lhsT=identb[:, :], rhs=mk[:, d, :], start=False, stop=(n_ == len(msk) - 1), skip_group_check=True), reads=[bidb, bmk], writes=[bps_], inc=(n_ == len(msk) - 1))

            def emit_unit_S(fi):
                r, u, _, _ = flat[fi]
                sl3 = fi % 3
                for jj, kt in enumerate(u):
                    emit_S(r, kt, sbank[2 * sl3 + jj], bsbank[2 * sl3 + jj])

            loads(0)
            emit_unit_S(0)
            emit_unit_S(1)
            for fi, (r, unit, first, last) in enumerate(flat):
                m, slot, po, bpo = r["m"], r["slot"], r["po"], r["bpo"]
                if first and m == 0 and r["hh"] + 1 < 16:
                    loads(r["hh"] + 1)
                if fi + 2 < len(flat):
                    emit_unit_S(fi + 2)
                sl3 = fi % 3
                i0, kp, _ = geom(m, unit[0])
                assert all(geom(m, kt)[0] == i0 for kt in unit)
                pt_, bpt_ = ptb[sl3], bptb[sl3]
                rdS = [bsbank[2 * sl3 + jj] for jj in range(len(unit))]
                if len(unit) == 1:
                    P.op("act", lambda e: e.activation(out=pt_[0:kp, 0, i0 * 128:512], in_=sbank[2 * sl3][0:kp, i0 * 128:512], func=AF.Exp), reads=rdS, writes=[bpt_])
                else:
                    P.op("act", lambda e: e.activation(out=pt_[:, :, i0 * 128:512], in_=pS[sl3][:, :].rearrange("p (b c) -> p b c", b=2)[:, :, i0 * 128:512], func=AF.Exp), reads=rdS, writes=[bpt_])
                for jj, kt in enumerate(unit):
                    if FILLER and kt % FILLER == 0 and kt > 0:
                        P.op("pe", lambda e: e.matmul(po[:, 260:512], lhsT=identb[:, :], rhs=ones_b[:, 0:252], start=False, stop=False, skip_group_check=True), inc=False)
                    for i in range(i0, 4):
                        P.op("pe", lambda e: e.matmul(po[:, i * 65:(i + 1) * 65], lhsT=pt_[0:kp, jj, i * 128:(i + 1) * 128], rhs=vsb[slot][0:kp, kt, :], start=(kt == 0 and i == 0), stop=(kt == 16 * m + 4 * i + 4), skip_group_check=True), reads=[bpt_, bvsb[slot]], writes=[bpo], inc=(i == 3))
                if last:
                    pov = po[:, 0:260].rearrange("p (i d) -> p i d", i=4)
                    P.op("dve", lambda e: e.reciprocal(out=rden[:], in_=pov[:, :, 64]), reads=[bpo], writes=[brden])
                    col = (0 if r["mla"] else 512) + r["h"] * 64
                    for i in range(4):
                        P.op("dve", lambda e: e.tensor_scalar(out=ybuf[:, 4 * m + i, col:col + 64], in0=pov[:, i, 0:64], scalar1=rden[:, i:i + 1], scalar2=None, op0=ALU.mult), reads=[bpo, brden], writes=[bybuf[4 * m + i]])
            P.barrier()
        if debug:
            bYD = Buf("YD", acc=True)
            for t in range(NT):
                P.dma("sp", lambda e, t=t: e.dma_start(out=YD[t * 128:(t + 1) * 128, :], in_=ybuf[:, t, :]), "ybuf", reads=[bybuf[t]], writes=[bYD])

        with contextlib.ExitStack() as ec:
            wmo = sb(ec, "wmo", [128, 4, D], BF16); bwmo = Buf("wmo")
            wfo = sb(ec, "wfo", [128, 4, D], BF16); bwfo = Buf("wfo")
            wo = sb(ec, "wo", [128, 8, D], BF16); bwo = Buf("wo")
            load_w(wmo[:], w_mla_out.rearrange("(c p) n -> p c n", p=128), "wmo", bwmo)
            load_w(wfo[:], w_fox_out.rearrange("(c p) n -> p c n", p=128), "wfo", bwfo)
            for c in range(8):
                load_w(wo[:, c, :], w_out[c * 128:(c + 1) * 128, :], "wo", bwo)
            yT = sb(ec, "yT", [128, 8, 512], BF16)
            byT = [Buf("yT%d" % c) for c in range(8)]
            ga = [sb(ec, "ga%d" % i, [128, 16, 512], BF16) for i in range(1)]
            bga = [Buf("ga%d" % i) for i in range(1)]
            mT = sb(ec, "mT", [128, 8, 512], BF16)
            bmT = [Buf("mT%d" % c) for c in range(8)]
            mt1 = sb(ec, "mt1", [128, 512], F32); bmt1 = Buf("mt1")
            mt2 = sb(ec, "mt2", [128, 512], F32); bmt2 = Buf("mt2")
            xr = [sb(ec, "xr%d" % i, [128, D], F32) for i in range(2)]
            bxr = [Buf("xr%d" % i) for i in range(2)]
            for m in range(4):
                mc = slice(m * 512, (m + 1) * 512)
                P.dma("sp", lambda e, mc=mc: e.dma_start(out=ga[0][:], in_=GS[:, :, mc].rearrange("j p c -> p j c")), "ga0", reads=[bGS], writes=[bga[0]])
                for c in range(8):
                    pt_, bpt_ = next_bf()
                    for t in range(4):
                        P.op("pe", lambda e, c=c, t=t, pt_=pt_: e.transpose(out=pt_[:, t * 128:(t + 1) * 128], in_=ybuf[:, 4 * m + t, c * 128:(c + 1) * 128], identity=identb[:]), reads=[bybuf[4 * m + t], bidb], writes=[bpt_], inc=(t == 3))
                    copy(yT[:, c, :], pt_[:, 0:512], [bpt_], [byT[c]], 512)
                for jc in range(8):
                    pa, bpa = next_f()
                    pb, bpb = next_f()
                    for c in range(4):
                        P.op("pe", lambda e, c=c, pa=pa, jc=jc: e.matmul(pa[:, :], lhsT=wmo[:, c, jc * 128:(jc + 1) * 128], rhs=yT[:, c, :], start=(c == 0), stop=(c == 3)), reads=byT[0:4] + [bwmo], writes=[bpa], inc=(c == 3))
                    for c in range(4):
                        P.op("pe", lambda e, c=c, pb=pb, jc=jc: e.matmul(pb[:, :], lhsT=wfo[:, c, jc * 128:(jc + 1) * 128], rhs=yT[:, 4 + c, :], start=(c == 0), stop=(c == 3)), reads=byT[4:8] + [bwfo], writes=[bpb], inc=(c == 3))
                    P.op("dve", lambda e, pa=pa, jc=jc: e.tensor_tensor(out=mt1[:], in0=pa[:, :], in1=ga[0][:, jc, :], op=ALU.mult), reads=[bpa, bga[0]], writes=[bmt1])
                    P.op("dve", lambda e, pb=pb, jc=jc: e.tensor_tensor(out=mt2[:], in0=pb[:, :], in1=ga[0][:, 8 + jc, :], op=ALU.mult), reads=[bpb, bga[0]], writes=[bmt2])
                    P.op("pool", lambda e, jc=jc: e.tensor_tensor(out=mT[:, jc, :], in0=mt1[:], in1=mt2[:], op=ALU.add), reads=[bmt1, bmt2], writes=[bmT[jc]])
                for t in range(4):
                    tt = 4 * m + t
                    k2 = tt % 2
                    P.dma("sp", lambda e, tt=tt, k2=k2: e.dma_start(out=xr[k2][:], in_=xq[tt * 128:(tt + 1) * 128, :]), "xr%d" % k2, writes=[bxr[k2]])
                    for hf in range(2):
                        pa, bpa = next_f()
                        for c in range(8):
                            P.op("pe", lambda e, c=c, pa=pa, t=t, hf=hf: e.matmul(pa[:, :], lhsT=mT[:, c, t * 128:(t + 1) * 128], rhs=wo[:, c, hf * 512:(hf + 1) * 512], start=(c == 0), stop=(c == 7)), reads=bmT + [bwo], writes=[bpa], inc=(c == 7))
                        P.op("dve", lambda e, pa=pa, tt=tt, hf=hf, k2=k2: e.tensor_tensor(out=acc[:, tt, hf * 512:(hf + 1) * 512], in0=pa[:, :], in1=xr[k2][:, hf * 512:(hf + 1) * 512], op=ALU.add), reads=[bpa, bxr[k2]], writes=[bacc[tt][hf]])
            P.barrier()

        ey.close()
        with contextlib.ExitStack() as em:
            er = contextlib.ExitStack()
            gffn = sb(em, "gffn", [128, D], F32); bgffn = Buf("gffn")
            gfin = sb(em, "gfin", [128, D], F32); bgfin = Buf("gfin")
            bcast_load(gffn, bgffn, ffn_norm, "gffn")
            bcast_load(gfin, bgfin, final_norm, "gfin")
            sel = sb(em, "sel", [32, 32, 128], BF16); bsel = Buf("sel")
            for e_ in range(32):
                P.op("pool", lambda e, e_=e_: e.tensor_copy(out=sel[:, e_, :], in_=ident32[0:32, e_:e_ + 1].to_broadcast([32, 128])), reads=[bid32], writes=[bsel])
            hT = sb(em, "hT", [128, 8, NQ], BF16)
            bhT = [Buf("hT%d" % t) for t in range(NT)]
            combT = sb(em, "combT", [32, NQ], BF16)
            bcombT = [Buf("combT%d" % t) for t in range(NT)]
            ER = E_PER_ROUND
            ew0 = contextlib.ExitStack()
            wgt = [sb(ew0, "wgt%d" % i, [128, ER, 8, 256], BF16) for i in range(2)]
            bwgt = [Buf("wgt%d" % i) for i in range(2)]
            wup = [sb(ew0, "wup%d" % i, [128, ER, 8, 256], BF16) for i in range(2)]
            bwup = [Buf("wup%d" % i) for i in range(2)]
            wdn = [sb(ew0, "wdn%d" % i, [128, ER, 2, D], BF16) for i in range(2)]
            bwdn = [Buf("wdn%d" % i) for i in range(2)]
            nround = 32 // ER

            def load_round(r):
                sl = r % 2
                for x_ in range(ER):
                    e_ = r * ER + x_
                    P.dma("pool", lambda e, e_=e_, x_=x_: e.dma_start(out=wgt[sl][:, x_, :, :], in_=w_gate[e_].rearrange("(c p) f -> p c f", p=128)), "wgt%d" % sl, writes=[bwgt[sl]])
                    P.dma("pool", lambda e, e_=e_, x_=x_: e.dma_start(out=wup[sl][:, x_, :, :], in_=w_up[e_].rearrange("(c p) f -> p c f", p=128)), "wup%d" % sl, writes=[bwup[sl]])
                    P.dma("pool", lambda e, e_=e_, x_=x_: e.dma_start(out=wdn[sl][:, x_, :, :], in_=w_down[e_].rearrange("(c p) d -> p c d", p=128)), "wdn%d" % sl, writes=[bwdn[sl]])

            load_round(0)
            wr = sb(er, "wr", [128, 8, 36], BF16); bwr = Buf("wr")
            load_w(wr[:, :, 0:4], w_grt.rearrange("(c p) n -> p c n", p=128), "wr", bwr)
            load_w(wr[:, :, 4:36], w_ert.rearrange("(c p) n -> p c n", p=128), "wr", bwr)
            rb = sb(er, "rb", [128, 36], F32); brb = Buf("rb")
            P.dma("sp", lambda e: e.dma_start(out=rb[:, 0:4], in_=b_grt.partition_broadcast(128)), "rb", writes=[brb])
            P.dma("sp", lambda e: e.dma_start(out=rb[:, 4:36], in_=b_ert.partition_broadcast(128)), "rb", writes=[brb])
            junk2 = sb(er, "junk2", [128, D], BF16); bjunk2 = Buf("junk2")
            hs = [sb(er, "hs%d" % i, [128, D], BF16) for i in range(4)]
            bhs = [Buf("hs%d" % i) for i in range(4)]
            ssq2 = [sb(er, "ssq2_%d" % i, [128, 1], F32) for i in range(4)]
            bssq2 = [Buf("ssq2_%d" % i) for i in range(4)]
            rstd2 = [sb(er, "rstd2_%d" % i, [128, 1], F32) for i in range(4)]
            brstd2 = [Buf("rstd2_%d" % i) for i in range(4)]
            lgA = sb(er, "lgA", [128, NT, 36], F32); blgA = [Buf("lgA%d" % t) for t in range(NT)]
            smA = sb(er, "smA", [128, NT, 8], F32); bsmA = [Buf("smA%d" % t) for t in range(NT)]
            ohA = sb(er, "ohA", [128, NT, 4], F32); bohA = [Buf("ohA%d" % t) for t in range(NT)]
            mlA = sb(er, "mlA", [128, NT, 32], F32); bmlA = [Buf("mlA%d" % t) for t in range(NT)]
            t8A = sb(er, "t8A", [128, NT, 8], F32); bt8A = [Buf("t8A%d" % t) for t in range(NT)]
            c1A = sb(er, "c1A", [128, NT, 32], F32); bc1A = [Buf("c1A%d" % t) for t in range(NT)]
            c2A = sb(er, "c2A", [128, NT, 32], F32); bc2A = [Buf("c2A%d" % t) for t in range(NT)]
            for t0_ in range(0, NT, 4):
                tl = list(range(t0_, t0_ + 4))
                for t in tl:
                    k4 = t % 4
                    P.op("act", lambda e: e.activation(out=junk2[:], in_=acc[:, t, :], func=AF.Square, accum_out=ssq2[k4][:]), reads=bacc[t], writes=[bjunk2, bssq2[k4]])
                    rstd_from_ssq(ssq2[k4][:], bssq2[k4], rstd2[k4][:], brstd2[k4], D)
                for t in tl:
                    k4 = t % 4
                    P.op("dve", lambda e: e.scalar_tensor_tensor(out=hs[k4][:], in0=acc[:, t, :], scalar=rstd2[k4][:, 0:1], in1=gffn[:], op0=ALU.mult, op1=ALU.mult), reads=bacc[t] + [brstd2[k4], bgffn], writes=[bhs[k4]])
                for t in tl:
                    k4 = t % 4
                    tc_ = slice(t * 128, (t + 1) * 128)
                    pt_, bpt_ = next_bf()
                    for c in range(8):
                        P.op("pe", lambda e: e.transpose(out=pt_[:, c * 128:(c + 1) * 128], in_=hs[k4][:, c * 128:(c + 1) * 128], identity=identb[:]), reads=[bhs[k4], bidb], writes=[bpt_], inc=(c == 7))
                    copy(hT[:, :, tc_], pt_[:, :].rearrange("p (c k) -> p c k", c=8), [bpt_], [bhT[t]], 1024)
                for t in tl:
                    tc_ = slice(t * 128, (t + 1) * 128)
                    pa, bpa = next_f()
                    for c in range(8):
                        P.op("pe", lambda e: e.matmul(pa[:, 0:36], lhsT=hT[:, c, tc_], rhs=wr[:, c, :], start=(c == 0), stop=(c == 7)), reads=[bhT[t], bwr], writes=[bpa], inc=(c == 7))
                    P.op("dve", lambda e: e.tensor_tensor(out=lgA[:, t, :], in0=pa[:, 0:36], in1=rb[:], op=ALU.add), reads=[bpa, brb], writes=[blgA[t]])

            def stage(fn):
                for t in range(NT):
                    fn(t)
            stage(lambda t: P.op("dve", lambda e: e.reduce_max(out=smA[:, t, 0:1], in_=lgA[:, t, 0:4], axis=AX.X), reads=[blgA[t]], writes=[bsmA[t]]))
            stage(lambda t: P.op("dve", lambda e: e.tensor_scalar(out=smA[:, t, 1:2], in0=smA[:, t, 0:1], scalar1=-1.0, scalar2=None, op0=ALU.mult), reads=[bsmA[t]], writes=[bsmA[t]]))
            stage(lambda t: P.op("act", lambda e: e.activation(out=ohA[:, t, :], in_=lgA[:, t, 0:4], func=AF.Exp, bias=smA[:, t, 1:2], scale=1.0, accum_out=smA[:, t, 2:3]), reads=[blgA[t], bsmA[t]], writes=[bohA[t], bsmA[t]]))
            stage(lambda t: P.op("dve", lambda e: e.reciprocal(out=smA[:, t, 3:4], in_=smA[:, t, 2:3]), reads=[bsmA[t]], writes=[bsmA[t]]))
            stage(lambda t: P.op("dve", lambda e: e.tensor_scalar(out=ohA[:, t, :], in0=lgA[:, t, 0:4], scalar1=smA[:, t, 0:1], scalar2=1e30, op0=ALU.is_lt, op1=ALU.mult), reads=[blgA[t], bsmA[t], bohA[t]], writes=[bohA[t]]))
            for g_ in range(4):
                stage(lambda t: P.op("dve", lambda e: e.tensor_scalar(out=mlA[:, t, g_ * 8:(g_ + 1) * 8], in0=lgA[:, t, 4 + g_ * 8:4 + (g_ + 1) * 8], scalar1=ohA[:, t, g_:g_ + 1], scalar2=None, op0=ALU.subtract), reads=[blgA[t], bohA[t], bmlA[t]], writes=[bmlA[t]]))
            stage(lambda t: P.op("dve", lambda e: e.max(out=t8A[:, t, :], in_=mlA[:, t, :]), reads=[bmlA[t]], writes=[bt8A[t]]))
            stage(lambda t: P.op("dve", lambda e: e.tensor_tensor(out=smA[:, t, 4:5], in0=t8A[:, t, 0:1], in1=t8A[:, t, 1:2], op=ALU.subtract), reads=[bt8A[t], bsmA[t]], writes=[bsmA[t]]))
            stage(lambda t: P.op("act", lambda e: e.activation(out=smA[:, t, 5:6], in_=smA[:, t, 4:5], func=AF.Sigmoid), reads=[bsmA[t]], writes=[bsmA[t]]))
            stage(lambda t: P.op("dve", lambda e: e.tensor_tensor(out=smA[:, t, 6:7], in0=smA[:, t, 5:6], in1=smA[:, t, 3:4], op=ALU.mult), reads=[bsmA[t]], writes=[bsmA[t]]))
            stage(lambda t: P.op("dve", lambda e: e.tensor_tensor(out=smA[:, t, 7:8], in0=smA[:, t, 3:4], in1=smA[:, t, 6:7], op=ALU.subtract), reads=[bsmA[t]], writes=[bsmA[t]]))
            stage(lambda t: P.op("dve", lambda e: e.tensor_scalar(out=c1A[:, t, :], in0=mlA[:, t, :], scalar1=t8A[:, t, 0:1], scalar2=smA[:, t, 6:7], op0=ALU.is_equal, op1=ALU.mult), reads=[bmlA[t], bt8A[t], bsmA[t]], writes=[bc1A[t]]))
            stage(lambda t: P.op("dve", lambda e: e.tensor_scalar(out=c2A[:, t, :], in0=mlA[:, t, :], scalar1=t8A[:, t, 1:2], scalar2=smA[:, t, 7:8], op0=ALU.is_equal, op1=ALU.mult), reads=[bmlA[t], bt8A[t], bsmA[t]], writes=[bc2A[t]]))
            stage(lambda t: P.op("dve", lambda e: e.tensor_tensor(out=c1A[:, t, :], in0=c1A[:, t, :], in1=c2A[:, t, :], op=ALU.add), reads=[bc1A[t], bc2A[t]], writes=[bc1A[t]]))

            def to_combT(t):
                pb, bpb = next_f()
                P.op("pe", lambda e: e.matmul(pb[0:32, 0:128], lhsT=c1A[:, t, :], rhs=ident32[:], start=True, stop=True), reads=[bc1A[t], bid32], writes=[bpb])
                copy(combT[:, t * 128:(t + 1) * 128], pb[0:32, 0:128], [bpb], [bcombT[t]], 128)
            stage(to_combT)

            P.barrier()
            er.close()
            ew = contextlib.ExitStack()
            ER = E_PER_ROUND
            cbs = [sb(ew, "cbs%d" % i, [128, 512], BF16) for i in range(2)]
            bcbs = [Buf("cbs%d" % i) for i in range(2)]
            sa = [sb(ew, "sa%d" % i, [128, 512], F32) for i in range(2)]
            bsa = [Buf("sa%d" % i) for i in range(2)]
            tm = [sb(ew, "tm%d" % i, [128, 512], F32) for i in range(2)]
            btm = [Buf("tm%d" % i) for i in range(2)]
            hm = [sb(ew, "hm%d" % i, [128, ER, 2, 512], BF16) for i in range(2)]
            bhm = [Buf("hm%d" % i) for i in range(2)]
            pdv = [pbf[0][:, :].bitcast(F32), pbf[1][:, :].bitcast(F32)]
            itc = {"it": 0, "pd": 0}

            def front(r, m):
                sl = r % 2
                mc = slice(m * 512, (m + 1) * 512)
                hsl = (r * 4 + m) % 2
                for x_ in range(ER):
                    e_ = r * ER + x_
                    pc, bpc = pf[4], bpf[4]
                    P.op("pe", lambda e: e.matmul(pc[:, :], lhsT=sel[:, e_, :], rhs=combT[:, mc], start=True, stop=True), reads=bcombT[4 * m:4 * m + 4] + [bsel], writes=[bpc])
                    k2 = (itc["it"] // 2) % 2
                    P.op("act", lambda e: e.copy(out=cbs[k2][:], in_=pc[:, :]), reads=[bpc], writes=[bcbs[k2]])
                    for fc in range(2):
                        kk = itc["it"] % 2
                        itc["it"] += 1
                        pa, bpa = pf[0 + kk], bpf[0 + kk]
                        pb, bpb = pf[2 + kk], bpf[2 + kk]
                        for c in range(8):
                            P.op("pe", lambda e: e.matmul(pa[:, :], lhsT=wgt[sl][:, x_, c, fc * 128:(fc + 1) * 128], rhs=hT[:, c, mc], start=(c == 0), stop=(c == 7)), reads=bhT[4 * m:4 * m + 4] + [bwgt[sl]], writes=[bpa], inc=(c == 7))
                        for c in range(8):
                            P.op("pe", lambda e: e.matmul(pb[:, :], lhsT=wup[sl][:, x_, c, fc * 128:(fc + 1) * 128], rhs=hT[:, c, mc], start=(c == 0), stop=(c == 7)), reads=bhT[4 * m:4 * m + 4] + [bwup[sl]], writes=[bpb], inc=(c == 7))
                        P.op("act", lambda e: e.activation(out=sa[kk][:], in_=pa[:, :], func=AF.Silu), reads=[bpa], writes=[bsa[kk]])
                        P.op("dve", lambda e: e.tensor_tensor(out=tm[kk][:], in0=pb[:, :], in1=sa[kk][:], op=ALU.mult), reads=[bpb, bsa[kk]], writes=[btm[kk]])
                        P.op("pool", lambda e: e.tensor_tensor(out=hm[hsl][:, x_, fc, :], in0=tm[kk][:], in1=cbs[k2][:], op=ALU.mult), reads=[btm[kk], bcbs[k2]], writes=[bhm[hsl]])

            def back(r, m):
                sl = r % 2
                hsl = (r * 4 + m) % 2
                for t in range(4):
                    tt = 4 * m + t
                    for hf in range(2):
                        kd = itc["pd"] % 2
                        itc["pd"] += 1
                        pd, bpd = pdv[kd], bpbf[kd]
                        n_ = 0
                        for x_ in range(ER):
                            for fc in range(2):
                                P.op("pe", lambda e: e.matmul(pd, lhsT=hm[hsl][:, x_, fc, t * 128:(t + 1) * 128], rhs=wdn[sl][:, x_, fc, hf * 512:(hf + 1) * 512], start=(n_ == 0), stop=(n_ == 2 * ER - 1)), reads=[bhm[hsl], bwdn[sl]], writes=[bpd], inc=(n_ == 2 * ER - 1))
                                n_ += 1
                        P.op("dve", lambda e: e.tensor_tensor(out=acc[:, tt, hf * 512:(hf + 1) * 512], in0=pd, in1=acc[:, tt, hf * 512:(hf + 1) * 512], op=ALU.add), reads=[bpd, bacc[tt][hf]], writes=[bacc[tt][hf]])

            units = [(r, m) for r in range(nround) for m in range(4)]
            for ui, (r, m) in enumerate(units):
                front(r, m)
                if ui > 0:
                    back(*units[ui - 1])
                if m == 0 and r + 1 < nround:
                    load_round(r + 1)
            back(*units[-1])

            P.barrier()
            ew.close()
            ew0.close()
            junk2 = sb(em, "junk2b", [128, D], BF16); bjunk2 = Buf("junk2b")
            ssq2 = [sb(em, "ssq3_%d" % i, [128, 1], F32) for i in range(4)]
            bssq2 = [Buf("ssq3_%d" % i) for i in range(4)]
            rstd2 = [sb(em, "rstd3_%d" % i, [128, 1], F32) for i in range(4)]
            brstd2 = [Buf("rstd3_%d" % i) for i in range(4)]
            ot = [sb(em, "ot%d" % i, [128, D], F32) for i in range(4)]
            bot = [Buf("ot%d" % i) for i in range(4)]
            for t0_ in range(0, NT, 4):
                tl = list(range(t0_, t0_ + 4))
                for t in tl:
                    k4 = t % 4
                    P.op("act", lambda e: e.activation(out=junk2[:], in_=acc[:, t, :], func=AF.Square, accum_out=ssq2[k4][:]), reads=bacc[t], writes=[bjunk2, bssq2[k4]])
                    rstd_from_ssq(ssq2[k4][:], bssq2[k4], rstd2[k4][:], brstd2[k4], D)
                for t in tl:
                    k4 = t % 4
                    P.op("dve", lambda e: e.scalar_tensor_tensor(out=ot[k4][:], in0=acc[:, t, :], scalar=rstd2[k4][:, 0:1], in1=gfin[:], op0=ALU.mult, op1=ALU.mult), reads=bacc[t] + [brstd2[k4], bgfin], writes=[bot[k4]])
                    P.dma("sp", lambda e: e.dma_start(out=out[t * 128:(t + 1) * 128, :], in_=ot[k4][:]), "ot%d" % k4, reads=[bot[k4]], writes=[bOUT])
            P.barrier()

        P.emit(nc, es)
    return nc


def _host_consts(j):
    inv = (np.float32(10000.0) ** (-np.arange(0, 32, 2, dtype=np.float32) / np.float32(32))).astype(np.float32)
    pos = np.arange(NK, dtype=np.float32)
    ang = (pos[:, None] * inv[None, :]).astype(np.float32)
    cos = np.cos(ang).astype(np.float32).T
    sin = np.sin(ang).astype(np.float32).T
    cosk = np.concatenate([cos, cos], 0)
    sink = np.concatenate([-sin, sin], 0)
    qpos = np.concatenate([NMETA + 128 * (j + 4 * i) + np.arange(128) for i in range(NT)])
    cosq = (cosk[:, qpos] * np.float32(SC_MLA)).astype(np.float32)
    sinq = (sink[:, qpos] * np.float32(SC_MLA)).astype(np.float32)
    r = np.arange(128)
    tri = (r[None, :] >= r[:, None]).astype(np.float32)
    masks = np.full((4, 128, 128), -30000.0, np.float32)
    for d in range(4):
        if d < j:
            masks[d] = 0.0
        elif d == j:
            masks[d] = (tri - 1.0) * 30000.0
    return dict(cosk=np.ascontiguousarray(cosk), sink=np.ascontiguousarray(sink), cosq=np.ascontiguousarray(cosq),
                sinq=np.ascontiguousarray(sinq), masks=masks, ident=np.eye(128, dtype=np.float32))


def make_in_maps(inputs):
    f = lambda a: np.ascontiguousarray(np.asarray(a, dtype=np.float32))
    x = f(inputs["x"])
    shared = {
        "meta": f(inputs["meta"]), "attn_norm": f(inputs["attn_norm"]).reshape(D),
        "w_in": f(inputs["w_in"]).reshape(D, 4392), "b_forget": f(inputs["b_forget"]).reshape(8),
        "q_a_norm": f(inputs["q_a_norm"]).reshape(512), "w_q_up": f(inputs["w_q_up"]).reshape(512, 768),
        "kv_a_norm": f(inputs["kv_a_norm"]).reshape(256), "w_kv_up": f(inputs["w_kv_up"]).reshape(256, 1024),
        "w_mla_out": f(inputs["w_mla_out"]).reshape(512, D), "w_fox_out": f(inputs["w_fox_out"]).reshape(512, D),
        "w_out": f(inputs["w_out"]).reshape(D, D), "ffn_norm": f(inputs["ffn_norm"]).reshape(D),
        "w_group_router": f(inputs["w_group_router"]).reshape(D, 4), "b_group_router": f(inputs["b_group_router"]).reshape(4),
        "w_expert_router": f(inputs["w_expert_router"]).reshape(D, 32), "b_expert_router": f(inputs["b_expert_router"]).reshape(32),
        "w_gate": f(inputs["w_gate"]).reshape(32, D, 256), "w_up": f(inputs["w_up"]).reshape(32, D, 256),
        "w_down": f(inputs["w_down"]).reshape(32, 256, D), "final_norm": f(inputs["final_norm"]).reshape(D),
    }
    maps = []
    for c in range(8):
        b, j = c // 4, c % 4
        m = dict(shared)
        m["x_all"] = x[b]
        xb = x[b].reshape(64, 128, D)
        m["xq"] = np.ascontiguousarray(xb[j::4].reshape(NQ, D))
        m.update(_host_consts(j))
        maps.append(m)
    return maps


def kernel(**inputs):
    maps = make_in_maps(inputs)
    nc = build_program()
    res = run_bass_kernel_spmd(nc, maps, core_ids=list(range(8)))
    outp = np.zeros((2, 64, 128, D), np.float32)
    for c in range(8):
        b, j = c // 4, c % 4
        outp[b, j::4] = np.asarray(res.results[c]["out"], dtype=np.float32).reshape(NT, 128, D)
    return outp.reshape(2, SEQ, D)
```
